# Optimizing a Trainium2 kernel written in Bass

```python
import math, functools
import jax, jax.numpy as jnp
from jax import lax
import numpy as np

D_MODEL = 2048
BATCH = 4
SEQ = 2048
DEPTH = 4

N_MIXERS = 3
N_RGLRU_LAYERS = (DEPTH + 2) // 3
N_GDN_LAYERS = (DEPTH + 1) // 3
N_GLA_LAYERS = DEPTH // 3

DEEPNORM_ALPHA = float((2 * DEPTH) ** 0.25)
DEEPNORM_BETA = float((8 * DEPTH) ** -0.25)
LN_EPS = 1e-5
RMS_EPS = 1e-6
D_FF = 4 * D_MODEL
N_MOD = 6
CONV_WIDTH = 4

RNN_WIDTH = (5 * D_MODEL) // 4
RNN_BLOCKS = 16
RNN_BLOCK_DIM = RNN_WIDTH // RNN_BLOCKS
LRU_C = 8.0

GDN_HEAD_DIM = 128
GDN_QK_HEADS = D_MODEL // GDN_HEAD_DIM
GDN_V_HEADS = 2 * GDN_QK_HEADS
GDN_KEY_DIM = GDN_QK_HEADS * GDN_HEAD_DIM
GDN_VALUE_DIM = GDN_V_HEADS * GDN_HEAD_DIM
GDN_CONV_DIM = 2 * GDN_KEY_DIM + GDN_VALUE_DIM
GDN_IN_DIM = GDN_CONV_DIM + GDN_VALUE_DIM + 2 * GDN_V_HEADS
GDN_CHUNK = 64

GLA_HEADS = 4
GLA_KEY_DIM = D_MODEL // 2
GLA_VALUE_DIM = D_MODEL
GLA_HEAD_K = GLA_KEY_DIM // GLA_HEADS
GLA_HEAD_V = GLA_VALUE_DIM // GLA_HEADS
GLA_GATE_RANK = 16
GLA_GATE_NORMALIZER = 16.0
GLA_IN_DIM = 2 * GLA_KEY_DIM + 2 * GLA_VALUE_DIM + GLA_GATE_RANK
GLA_CHUNK = 64

kernel_name = 'hybrid_rglru_gdn_gla_deepnorm_adaln'


def _layer_norm(x, g, b):
    xf = x.astype(jnp.float32)
    mu = jnp.mean(xf, axis=-1, keepdims=True)
    var = jnp.mean(jnp.square(xf - mu), axis=-1, keepdims=True)
    return ((xf - mu) * lax.rsqrt(var + LN_EPS) * g.astype(jnp.float32) + b.astype(jnp.float32)).astype(x.dtype)


def _rms_norm_gated(o, w, z):
    of = o.astype(jnp.float32)
    n = of * lax.rsqrt(jnp.mean(of * of, axis=-1, keepdims=True) + RMS_EPS)
    return n * w.astype(jnp.float32) * jax.nn.silu(z.astype(jnp.float32))


def _l2norm(t):
    return t * lax.rsqrt(jnp.sum(t * t, axis=-1, keepdims=True) + RMS_EPS)


def _causal_depthwise_conv(x, w):
    k = w.shape[0]
    return lax.conv_general_dilated(
        x, w[:, None, :].astype(x.dtype), window_strides=(1,), padding=((k - 1, 0),),
        dimension_numbers=('NWC', 'WIO', 'NWC'), feature_group_count=x.shape[-1])


def _linear_scan(a, b):
    def combine(left, right):
        a_l, b_l = left
        a_r, b_r = right
        return a_l * a_r, a_r * b_l + b_r
    return lax.associative_scan(combine, (a, b), axis=1)[1]


def rglru_mixer(h, w_in, conv_w, conv_b, w_rgate, b_rgate, w_igate, b_igate, lam, w_out):
    bsz, seq, _ = h.shape
    gate_branch, rec_branch = jnp.split(h @ w_in, 2, axis=-1)
    xr = _causal_depthwise_conv(rec_branch, conv_w) + conv_b
    xb = xr.reshape(bsz, seq, RNN_BLOCKS, RNN_BLOCK_DIM)
    r = jax.nn.sigmoid((jnp.einsum('bsnd,nde->bsne', xb, w_rgate) + b_rgate).astype(jnp.float32))
    i = jax.nn.sigmoid((jnp.einsum('bsnd,nde->bsne', xb, w_igate) + b_igate).astype(jnp.float32))
    log_a = -LRU_C * r * jax.nn.softplus(-lam.astype(jnp.float32)).reshape(RNN_BLOCKS, RNN_BLOCK_DIM)
    a = jnp.exp(log_a)
    b_t = jnp.sqrt(-jnp.expm1(2.0 * log_a)) * (i * xb.astype(jnp.float32))
    hs = _linear_scan(a, b_t).reshape(bsz, seq, RNN_WIDTH)
    y = jax.nn.gelu(gate_branch, approximate=True) * hs.astype(h.dtype)
    return y @ w_out


def _chunk_gated_delta_rule(q, k, v, g, beta):
    bsz, seq, nh, dk = q.shape
    dv = v.shape[-1]
    cs = GDN_CHUNK
    n = seq // cs
    q = q.reshape(bsz, n, cs, nh, dk)
    k = k.reshape(bsz, n, cs, nh, dk)
    v = v.reshape(bsz, n, cs, nh, dv)
    beta = beta.reshape(bsz, n, cs, nh)
    gc = jnp.cumsum(g.reshape(bsz, n, cs, nh), axis=2)
    gc_t = jnp.swapaxes(gc, 2, 3)
    beta_t = jnp.swapaxes(beta, 2, 3)
    causal = jnp.tril(jnp.ones((cs, cs), dtype=bool))
    strict = jnp.tril(jnp.ones((cs, cs), dtype=bool), k=-1)
    decay = jnp.exp(jnp.where(causal, gc_t[..., :, None] - gc_t[..., None, :], -jnp.inf))
    a_kk = jnp.where(strict, jnp.einsum('bnihd,bnjhd->bnhij', k, k) * decay, 0.0) * beta_t[..., :, None]
    eye = jnp.eye(cs, dtype=jnp.float32)
    t_mat = lax.linalg.triangular_solve(eye + a_kk, jnp.broadcast_to(eye, a_kk.shape),
                                        left_side=True, lower=True, unit_diagonal=True)
    w = jnp.einsum('bnhij,bnjhd->nbhid', t_mat, k * (beta * jnp.exp(gc))[..., None])
    u = jnp.einsum('bnhij,bnjhd->nbhid', t_mat, v * beta[..., None])
    q_dec = jnp.transpose(q * jnp.exp(gc)[..., None], (1, 0, 3, 2, 4))
    a_qk = jnp.moveaxis(jnp.where(causal, jnp.einsum('bnihd,bnjhd->bnhij', q, k) * decay, 0.0), 1, 0)
    k_tail = jnp.transpose(k * jnp.exp(gc[:, :, -1:, :] - gc)[..., None], (1, 0, 3, 2, 4))
    chunk_decay = jnp.moveaxis(jnp.exp(gc[:, :, -1, :]), 1, 0)

    def step(state, xs):
        w_c, u_c, q_c, a_c, k_c, dec_c = xs
        v_new = u_c - jnp.einsum('bhcd,bhde->bhce', w_c, state)
        o_c = jnp.einsum('bhcd,bhde->bhce', q_c, state) + jnp.einsum('bhij,bhje->bhie', a_c, v_new)
        state = state * dec_c[..., None, None] + jnp.einsum('bhcd,bhce->bhde', k_c, v_new)
        return state, o_c

    state0 = jnp.zeros((bsz, nh, dk, dv), jnp.float32)
    _, o = lax.scan(step, state0, (w, u, q_dec, a_qk, k_tail, chunk_decay))
    return jnp.transpose(o, (1, 0, 3, 2, 4)).reshape(bsz, seq, nh, dv)


def gated_deltanet_mixer(h, w_in, conv_w, a_log, dt_bias, norm_w, w_out):
    bsz, seq, _ = h.shape
    qkv, z, b_raw, a_raw = jnp.split(
        h @ w_in, [GDN_CONV_DIM, GDN_CONV_DIM + GDN_VALUE_DIM, GDN_CONV_DIM + GDN_VALUE_DIM + GDN_V_HEADS], axis=-1)
    qkv = jax.nn.silu(_causal_depthwise_conv(qkv, conv_w)).astype(jnp.float32)
    q, k, v = jnp.split(qkv, [GDN_KEY_DIM, 2 * GDN_KEY_DIM], axis=-1)
    rep = GDN_V_HEADS // GDN_QK_HEADS
    q = jnp.repeat(_l2norm(q.reshape(bsz, seq, GDN_QK_HEADS, GDN_HEAD_DIM)) * GDN_HEAD_DIM ** -0.5, rep, axis=2)
    k = jnp.repeat(_l2norm(k.reshape(bsz, seq, GDN_QK_HEADS, GDN_HEAD_DIM)), rep, axis=2)
    v = v.reshape(bsz, seq, GDN_V_HEADS, GDN_HEAD_DIM)
    beta = jax.nn.sigmoid(b_raw.astype(jnp.float32))
    g = -jnp.exp(a_log.astype(jnp.float32)) * jax.nn.softplus(a_raw.astype(jnp.float32) + dt_bias.astype(jnp.float32))
    o = _chunk_gated_delta_rule(q, k, v, g, beta)
    o = _rms_norm_gated(o, norm_w, z.reshape(bsz, seq, GDN_V_HEADS, GDN_HEAD_DIM))
    return o.reshape(bsz, seq, GDN_VALUE_DIM).astype(h.dtype) @ w_out


def _chunk_gla(q, k, v, log_alpha):
    bsz, seq, nh, dk = q.shape
    dv = v.shape[-1]
    cs = GLA_CHUNK
    n = seq // cs
    q = q.reshape(bsz, n, cs, nh, dk)
    k = k.reshape(bsz, n, cs, nh, dk)
    v = v.reshape(bsz, n, cs, nh, dv)
    bcum = jnp.cumsum(log_alpha.reshape(bsz, n, cs, nh, dk), axis=2)
    q_dec = q * jnp.exp(bcum)
    k_inv = k * jnp.exp(-bcum)
    causal = jnp.tril(jnp.ones((cs, cs), dtype=bool))
    a_qk = jnp.where(causal, jnp.einsum('bnihd,bnjhd->bnhij', q_dec, k_inv), 0.0)
    intra = jnp.einsum('bnhij,bnjhe->bnihe', a_qk, v)
    k_tail = k * jnp.exp(bcum[:, :, -1:] - bcum)
    chunk_decay = jnp.exp(bcum[:, :, -1])

    def step(state, xs):
        q_c, k_c, v_c, dec_c = xs
        o_c = jnp.einsum('bchd,bhde->bche', q_c, state)
        state = state * dec_c[..., None] + jnp.einsum('bchd,bche->bhde', k_c, v_c)
        return state, o_c

    state0 = jnp.zeros((bsz, nh, dk, dv), jnp.float32)
    xs = (jnp.moveaxis(q_dec, 1, 0), jnp.moveaxis(k_tail, 1, 0), jnp.moveaxis(v, 1, 0), jnp.moveaxis(chunk_decay, 1, 0))
    _, inter = lax.scan(step, state0, xs)
    return (jnp.moveaxis(inter, 0, 1) + intra).reshape(bsz, seq, nh, dv)


def gla_mixer(h, w_in, w_alpha_up, b_alpha, norm_w, w_out):
    bsz, seq, _ = h.shape
    q, k, v, gate, a_low = jnp.split(
        h @ w_in, [GLA_KEY_DIM, 2 * GLA_KEY_DIM, 2 * GLA_KEY_DIM + GLA_VALUE_DIM, 2 * GLA_KEY_DIM + 2 * GLA_VALUE_DIM], axis=-1)
    log_alpha = jax.nn.log_sigmoid((a_low @ w_alpha_up + b_alpha).astype(jnp.float32)) / GLA_GATE_NORMALIZER
    q = q.astype(jnp.float32).reshape(bsz, seq, GLA_HEADS, GLA_HEAD_K) * GLA_HEAD_K ** -0.5
    k = k.astype(jnp.float32).reshape(bsz, seq, GLA_HEADS, GLA_HEAD_K)
    v = v.astype(jnp.float32).reshape(bsz, seq, GLA_HEADS, GLA_HEAD_V)
    o = _chunk_gla(q, k, v, log_alpha.reshape(bsz, seq, GLA_HEADS, GLA_HEAD_K))
    o = _rms_norm_gated(o, norm_w, gate.reshape(bsz, seq, GLA_HEADS, GLA_HEAD_V))
    return o.reshape(bsz, seq, GLA_VALUE_DIM).astype(h.dtype) @ w_out


def sq_relu_mlp(h, w1, w2):
    return jnp.square(jax.nn.relu(h @ w1)) @ w2


def _residual(x, shift, scale, gate, ln_g, ln_b, sublayer):
    h = x * (1.0 + scale[:, None, :]) + shift[:, None, :]
    return _layer_norm(DEEPNORM_ALPHA * x + (1.0 + gate[:, None, :]) * sublayer(h), ln_g, ln_b)


def setup_inputs(seed: int = 0) -> dict:
    key = jax.random.key(seed)
    keys = iter(jax.random.split(key, 40))
    f32 = jnp.float32

    def nrm(shape, scale):
        return jax.random.normal(next(keys), shape, f32) * scale

    def unif(shape, lo, hi):
        return jax.random.uniform(next(keys), shape, f32, minval=lo, maxval=hi)

    x = nrm((BATCH, SEQ, D_MODEL), 1.0)
    c = nrm((BATCH, D_MODEL), 1.0)
    ln_g = 1.0 + nrm((DEPTH, 2, D_MODEL), 0.02)
    ln_b = nrm((DEPTH, 2, D_MODEL), 0.02)
    w_mod = nrm((DEPTH, D_MODEL, N_MOD * D_MODEL), 0.25 * D_MODEL ** -0.5)
    b_mod = nrm((DEPTH, N_MOD * D_MODEL), 0.02)
    w_ff1 = nrm((DEPTH, D_MODEL, D_FF), D_MODEL ** -0.5)
    w_ff2 = nrm((DEPTH, D_FF, D_MODEL), DEEPNORM_BETA * D_FF ** -0.5)
    na = N_RGLRU_LAYERS
    rglru_w_in = nrm((na, D_MODEL, 2 * RNN_WIDTH), D_MODEL ** -0.5)
    rglru_conv_w = nrm((na, CONV_WIDTH, RNN_WIDTH), CONV_WIDTH ** -0.5)
    rglru_conv_b = nrm((na, RNN_WIDTH), 0.02)
    rglru_w_rgate = nrm((na, RNN_BLOCKS, RNN_BLOCK_DIM, RNN_BLOCK_DIM), RNN_BLOCK_DIM ** -0.5)
    rglru_b_rgate = nrm((na, RNN_BLOCKS, RNN_BLOCK_DIM), 0.02)
    rglru_w_igate = nrm((na, RNN_BLOCKS, RNN_BLOCK_DIM, RNN_BLOCK_DIM), RNN_BLOCK_DIM ** -0.5)
    rglru_b_igate = nrm((na, RNN_BLOCKS, RNN_BLOCK_DIM), 0.02)
    p = unif((na, RNN_WIDTH), 0.9, 0.999) ** (1.0 / LRU_C)
    rglru_lambda = jnp.log(p) - jnp.log1p(-p)
    rglru_w_out = nrm((na, RNN_WIDTH, D_MODEL), DEEPNORM_BETA * RNN_WIDTH ** -0.5)
    nb = N_GDN_LAYERS
    gdn_w_in = nrm((nb, D_MODEL, GDN_IN_DIM), D_MODEL ** -0.5)
    gdn_conv_w = nrm((nb, CONV_WIDTH, GDN_CONV_DIM), CONV_WIDTH ** -0.5)
    gdn_a_log = jnp.log(unif((nb, GDN_V_HEADS), 1.0, 16.0))
    dt = jnp.exp(unif((nb, GDN_V_HEADS), math.log(1e-3), math.log(1e-1)))
    gdn_dt_bias = dt + jnp.log(-jnp.expm1(-dt))
    gdn_norm_w = 1.0 + nrm((nb, GDN_HEAD_DIM), 0.02)
    gdn_w_out = nrm((nb, GDN_VALUE_DIM, D_MODEL), DEEPNORM_BETA * GDN_VALUE_DIM ** -0.5)
    nc = N_GLA_LAYERS
    gla_w_in = nrm((nc, D_MODEL, GLA_IN_DIM), D_MODEL ** -0.5)
    gla_w_alpha_up = nrm((nc, GLA_GATE_RANK, GLA_KEY_DIM), GLA_GATE_RANK ** -0.5)
    gla_b_alpha = nrm((nc, GLA_KEY_DIM), 0.1)
    gla_norm_w = 1.0 + nrm((nc, GLA_HEAD_V), 0.02)
    gla_w_out = nrm((nc, GLA_VALUE_DIM, D_MODEL), DEEPNORM_BETA * GLA_VALUE_DIM ** -0.5)
    return {
        'x': x, 'c': c, 'ln_g': ln_g, 'ln_b': ln_b, 'w_mod': w_mod, 'b_mod': b_mod,
        'w_ff1': w_ff1, 'w_ff2': w_ff2,
        'rglru_w_in': rglru_w_in, 'rglru_conv_w': rglru_conv_w, 'rglru_conv_b': rglru_conv_b,
        'rglru_w_rgate': rglru_w_rgate, 'rglru_b_rgate': rglru_b_rgate,
        'rglru_w_igate': rglru_w_igate, 'rglru_b_igate': rglru_b_igate,
        'rglru_lambda': rglru_lambda, 'rglru_w_out': rglru_w_out,
        'gdn_w_in': gdn_w_in, 'gdn_conv_w': gdn_conv_w, 'gdn_a_log': gdn_a_log,
        'gdn_dt_bias': gdn_dt_bias, 'gdn_norm_w': gdn_norm_w, 'gdn_w_out': gdn_w_out,
        'gla_w_in': gla_w_in, 'gla_w_alpha_up': gla_w_alpha_up, 'gla_b_alpha': gla_b_alpha,
        'gla_norm_w': gla_norm_w, 'gla_w_out': gla_w_out,
    }


def reference(x, c, ln_g, ln_b, w_mod, b_mod, w_ff1, w_ff2,
              rglru_w_in, rglru_conv_w, rglru_conv_b, rglru_w_rgate, rglru_b_rgate,
              rglru_w_igate, rglru_b_igate, rglru_lambda, rglru_w_out,
              gdn_w_in, gdn_conv_w, gdn_a_log, gdn_dt_bias, gdn_norm_w, gdn_w_out,
              gla_w_in, gla_w_alpha_up, gla_b_alpha, gla_norm_w, gla_w_out):
    c_act = jax.nn.silu(c)
    for i in range(DEPTH):
        mod = c_act @ w_mod[i] + b_mod[i]
        sh_m, sc_m, gt_m, sh_f, sc_f, gt_f = jnp.split(mod, N_MOD, axis=-1)
        kind, slot = i % N_MIXERS, i // N_MIXERS
        if kind == 0:
            mixer = functools.partial(
                rglru_mixer, w_in=rglru_w_in[slot], conv_w=rglru_conv_w[slot], conv_b=rglru_conv_b[slot],
                w_rgate=rglru_w_rgate[slot], b_rgate=rglru_b_rgate[slot], w_igate=rglru_w_igate[slot],
                b_igate=rglru_b_igate[slot], lam=rglru_lambda[slot], w_out=rglru_w_out[slot])
        elif kind == 1:
            mixer = functools.partial(
                gated_deltanet_mixer, w_in=gdn_w_in[slot], conv_w=gdn_conv_w[slot], a_log=gdn_a_log[slot],
                dt_bias=gdn_dt_bias[slot], norm_w=gdn_norm_w[slot], w_out=gdn_w_out[slot])
        else:
            mixer = functools.partial(
                gla_mixer, w_in=gla_w_in[slot], w_alpha_up=gla_w_alpha_up[slot], b_alpha=gla_b_alpha[slot],
                norm_w=gla_norm_w[slot], w_out=gla_w_out[slot])
        x = _residual(x, sh_m, sc_m, gt_m, ln_g[i, 0], ln_b[i, 0], mixer)
        x = _residual(x, sh_f, sc_f, gt_f, ln_g[i, 1], ln_b[i, 1],
                      functools.partial(sq_relu_mlp, w1=w_ff1[i], w2=w_ff2[i]))
    return x
```

```python
import numpy as np
import concourse.bass as bass
import concourse.mybir as mybir
from concourse.bass_utils import run_bass_kernel_spmd

F32 = mybir.dt.float32
BF16 = mybir.dt.bfloat16
ALU = mybir.AluOpType
AF = mybir.ActivationFunctionType

ENGS = ("pe", "act", "dve", "pool", "sp")

D = 2048
KD = 16
SEQ = 2048
TP = 512
DEPTH = 4
ALPHA = float((2 * DEPTH) ** 0.25)
DFF = 8192
RW = 2560
RCH = 32
NWB = 2


class Prog:
    def __init__(self, nc):
        self.nc = nc
        self.q = {e: [] for e in ENGS}
        self.cnt = {e: 0 for e in ENGS}
        self.sem = {e: nc.alloc_semaphore(name=f"c_{e}") for e in ENGS if e != "sp"}
        self.last_w = {}
        self.readers = {}
        self.seen = {e: {} for e in ENGS}
        self.slots = {}

    def _deps(self, eng, reads, writes):
        deps = []
        for k in reads:
            w = self.last_w.get(k)
            if w is not None:
                deps.append(w)
            if isinstance(k, tuple) and k[0] == "ps":
                deps.extend(t for t in self.readers.get(k, ()) if t[0] != eng)
        for k in writes:
            w = self.last_w.get(k)
            if w is not None:
                deps.append(w)
            deps.extend(self.readers.get(k, ()))
        waits = {}
        for (src, val) in deps:
            if src == "pe" and eng == "pe":
                continue
            if val > waits.get(src, 0):
                waits[src] = val
        out = []
        for src, val in waits.items():
            if self.seen[eng].get(src, 0) >= val:
                continue
            self.seen[eng][src] = val
            out.append((src, val))
        return out

    def _commit(self, tok, reads, writes):
        for k in writes:
            self.last_w[k] = tok
            self.readers[k] = []
        for k in reads:
            self.readers.setdefault(k, []).append(tok)

    def op(self, eng, fn, reads=(), writes=()):
        waits = self._deps(eng, reads, writes)
        self.cnt[eng] += 1
        tok = (eng, self.cnt[eng])
        self.q[eng].append((waits, fn, eng))
        self._commit(tok, reads, writes)

    def dma(self, eng, slot, fn, reads=(), writes=()):
        if slot not in self.slots:
            self.slots[slot] = [self.nc.alloc_semaphore(name=f"d_{slot}"), 0]
        waits = self._deps(eng, reads, writes)
        s = self.slots[slot]
        s[1] += 16
        tok = (("slot", slot), s[1])
        self.q[eng].append((waits, fn, ("slot", slot)))
        self._commit(tok, reads, writes)

    def wait_all(self, eng, keys):
        waits = self._deps(eng, keys, ())
        self.q[eng].append((waits, None, None))

    def _semof(self, src):
        if isinstance(src, tuple):
            return self.slots[src[1]][0]
        return self.sem[src]

    def emit(self):
        nc = self.nc
        prog = self
        emap = {"pe": "tensor", "act": "scalar", "dve": "vector", "pool": "gpsimd", "sp": "sync"}
        with nc.Block() as block:
            for e in ENGS:
                def body(engobj, e=e):
                    for waits, fn, kind in prog.q[e]:
                        for src, val in waits:
                            engobj.wait_ge(prog._semof(src), val)
                        if fn is None:
                            continue
                        ins = fn(engobj)
                        if isinstance(kind, tuple):
                            ins.then_inc(prog.slots[kind[1]][0], 16)
                        else:
                            ins.then_inc(prog.sem[e], 1)
                getattr(block, emap[e])(body)


class MK:
    def __init__(self, layers=tuple(range(DEPTH)), n_pass=SEQ // TP):
        self.layers = tuple(layers)
        self.n_layers = len(self.layers)
        self.n_pass = n_pass
        nc = self.nc = bass.Bass("TRN2", target_bir_lowering=False)
        self.P = Prog(nc)
        self.din = {}
        self.wi = 0
        self.pi = 0
        self.sqi = 0

    def inp(self, name, shape):
        if name not in self.din:
            self.din[name] = self.nc.dram_tensor(name, list(shape), F32, kind="ExternalInput").ap()
        return self.din[name]

    def T(self, name, shape, dt=F32):
        return self.nc.alloc_sbuf_tensor(name, list(shape), dt)

    def ACT(self, out, in_, func, r, w, scale=None, bias=None):
        kw = {}
        if scale is not None:
            kw["scale"] = scale
        if bias is not None:
            kw["bias"] = bias
        self.P.op("act", lambda e: e.activation(out=out, in_=in_, func=func, **kw), reads=r, writes=w)

    def TS(self, out, in0, s1, s2, op0, op1, r, w, eng="dve"):
        if op1 is None:
            self.P.op(eng, lambda e: e.tensor_scalar(out=out, in0=in0, scalar1=s1, scalar2=None, op0=op0), reads=r, writes=w)
        else:
            self.P.op(eng, lambda e: e.tensor_scalar(out=out, in0=in0, scalar1=s1, scalar2=s2, op0=op0, op1=op1), reads=r, writes=w)

    def TT(self, out, in0, in1, op, r, w, eng="dve"):
        self.P.op(eng, lambda e: e.tensor_tensor(out=out, in0=in0, in1=in1, op=op), reads=r, writes=w)

    def STT(self, out, in0, scalar, in1, op0, op1, r, w):
        self.P.op("dve", lambda e: e.scalar_tensor_tensor(out=out, in0=in0, scalar=scalar, in1=in1, op0=op0, op1=op1), reads=r, writes=w)

    def CP(self, eng, out, in_, r, w):
        if eng == "act":
            self.P.op("act", lambda e: e.copy(out=out, in_=in_), reads=r, writes=w)
        else:
            self.P.op(eng, lambda e: e.tensor_copy(out=out, in_=in_), reads=r, writes=w)

    def MM(self, out, lhsT, rhs, start, stop, r, w):
        self.P.op("pe", lambda e: e.matmul(out, lhsT=lhsT, rhs=rhs, start=start, stop=stop), reads=r, writes=w)

    def DMA(self, eng, slot, out, in_, r, w):
        if slot in ("c0", "c1"):
            self.cslot = getattr(self, "cslot", 0) + 1
            slot = f"{slot}_{self.cslot}"
        self.P.dma(eng, slot, lambda e: e.dma_start(out=out, in_=in_), reads=r, writes=w)

    def psum(self):
        i = self.pi % 8
        self.pi += 1
        return self.ps[i], ("ps", i)

    def sq(self):
        i = self.sqi % 2
        self.sqi += 1
        return self.sqb[i], ("sq", i)

    def wtile(self, src, kp, kc, ncols):
        i = self.wi % NWB
        self.wi += 1
        t = self.wb[i]
        key = ("wb", i)
        self.DMA("pool", f"wb{i}", t[:kp, :kc, :ncols], src, [], [key])
        return t, key

    def linear(self, W, kp, KC, col0, ncols, ocw, act, on_block, blk0=0, cpt=4):
        tcols = ocw * cpt
        assert ncols % tcols == 0, (ncols, tcols)
        nkg = (KC + 15) // 16
        for og in range(ncols // tcols):
            banks = [self.psum() for _ in range(cpt)]
            for kg in range(nkg):
                kc = min(16, KC - kg * 16)
                c0 = col0 + og * tcols
                src = W[kg * 16 * kp:(kg * 16 + kc) * kp, c0:c0 + tcols].rearrange("(k p) n -> p k n", p=kp)
                t, wkey = self.wtile(src, kp, kc, tcols)
                for j in range(cpt):
                    pt, pk = banks[j]
                    for k in range(kc):
                        a, akey = act(kg * 16 + k)
                        self.MM(pt[:ocw, :], t[:kp, k, j * ocw:(j + 1) * ocw], a,
                                (kg == 0 and k == 0), (kg == nkg - 1 and k == kc - 1), [wkey, akey], [pk])
            for j in range(cpt):
                on_block(blk0 + og * cpt + j, banks[j][0], banks[j][1])

    def build(self):
        nc, P = self.nc, self.P
        L = self.n_layers
        xT = self.inp("xT", [D, SEQ])
        cT = self.inp("cT", [128, KD])
        lng = self.inp("lng", [128, DEPTH * 2 * KD])
        lnb = self.inp("lnb", [128, DEPTH * 2 * KD])
        bmod = self.inp("bmod", [128, DEPTH * 96])
        w_mod = self.inp("w_mod", [DEPTH, D, 6 * D])
        w_ff1 = self.inp("w_ff1", [DEPTH, D, DFF])
        w_ff2 = self.inp("w_ff2", [DEPTH, DFF, D])
        if any(l % 3 == 0 for l in self.layers):
            self.r_w_in = self.inp("rglru_w_in", [2, D, 2 * RW])
            self.r_w_out = self.inp("rglru_w_out", [2, RW, D])
            self.r_wrg = self.inp("rglru_w_rgate", [2, 16, 160, 160])
            self.r_wig = self.inp("rglru_w_igate", [2, 16, 160, 160])
        rvec = self.inp("rvec", [80, 2 * 8 * RCH])
        self.yT = nc.dram_tensor("yT", [D, SEQ], F32, kind="ExternalOutput").ap()

        self.xres = self.T("xres", [128, KD, TP])
        self.h = self.T("h", [128, KD, TP], BF16)
        self.U = self.T("U", [128, 64, TP], BF16)
        self.wb = [self.T(f"wb{i}", [128, 16, 512], BF16) for i in range(NWB)]
        self.ps = [nc.alloc_psum_tensor(f"ps{i}", [128, 512], F32) for i in range(8)]
        self.sqb = [self.T(f"sq{i}", [128, TP]) for i in range(2)]
        self.ones = self.T("ones", [128, 128])
        self.mean = self.T("mean", [128, TP])
        self.rstd = self.T("rstd", [128, TP])
        self.tmpv = self.T("tmpv", [128, TP])
        self.cact = self.T("cact", [128, KD], BF16)
        self.cf = self.T("cf", [128, KD])
        self.mod = self.T("mod", [128, DEPTH, 96])
        self.bm = self.T("bm", [128, DEPTH * 96])
        self.g_sb = self.T("g_sb", [128, DEPTH * 2 * KD])
        self.b_sb = self.T("b_sb", [128, DEPTH * 2 * KD])
        self.SC = self.T("SC", [128, 7200])
        self.rv = self.T("rv", [80, 2, 8, RCH])
        self.cneg = self.T("cneg", [80, 2, 2, RCH])
        self.hstate = self.T("hstate", [80, 2, RCH])
        self.tails = self.T("tails", [80, 2, RCH, 3])
        self.gw = [self.T(f"gw{i}", [80, 2, 2, 160], BF16) for i in range(2)]
        self.gwi = 0
        self.xrb = self.T("xrb", [80, 2, TP], BF16)

        P.op("dve", lambda e: e.memset(self.ones[:], 1.0), writes=["ones"])
        P.op("dve", lambda e: e.memset(self.hstate[:], 0.0), writes=["hstate"])
        P.op("dve", lambda e: e.memset(self.tails[:], 0.0), writes=["tails"])
        self.DMA("sp", "c0", self.cf[:], cT, [], ["cf"])
        self.DMA("sp", "c0", self.bm[:], bmod, [], ["bm"])
        self.DMA("sp", "c0", self.g_sb[:], lng, [], ["g_sb"])
        self.DMA("sp", "c0", self.b_sb[:], lnb, [], ["b_sb"])
        self.DMA("sp", "c0", self.rv[:].rearrange("p a b c -> p (a b c)"), rvec, [], ["rv"])
        self.ACT(self.cact[:], self.cf[:], AF.Silu, ["cf"], ["cact"])
        for s in range(2):
            self.ACT(self.cneg[:, s, 0, :], self.rv[:, s, 7, :], AF.Exp, ["rv"], ["cneg"], scale=-1.0)
            self.ACT(self.cneg[:, s, 0, :], self.cneg[:, s, 0, :], AF.Ln, ["cneg"], ["cneg"], bias=1.0)
            self.TS(self.cneg[:, s, 1, :], self.cneg[:, s, 0, :], -16.0, None, ALU.mult, None, ["cneg"], ["cneg"])
            self.TS(self.cneg[:, s, 0, :], self.cneg[:, s, 0, :], -8.0, None, ALU.mult, None, ["cneg"], ["cneg"])

        for l in self.layers:
            pm, pmk = self.psum()

            def act_c(k):
                return self.cact[:, k:k + 1], "cact"

            for og in range(24):
                t, wkey = self.wtile(w_mod[l][:, og * 512:(og + 1) * 512].rearrange("(k p) n -> p k n", p=128), 128, 16, 512)
                for j in range(4):
                    col = og * 4 + j
                    for k in range(16):
                        self.MM(pm[:, col:col + 1], t[:, k, j * 128:(j + 1) * 128], self.cact[:, k:k + 1], k == 0, k == 15, [wkey, "cact"], [pmk])
            self.TT(self.mod[:, l, :], pm[:, 0:96], self.bm[:, l * 96:(l + 1) * 96], ALU.add, [pmk, "bm"], ["mod"])
            for a in (16, 32, 64, 80):
                self.TS(self.mod[:, l, a:a + 16], self.mod[:, l, a:a + 16], 1.0, None, ALU.add, None, ["mod"], ["mod"])

        for p in range(self.n_pass):
            t0 = p * TP
            self.DMA("sp", "xin", self.xres[:], xT[:, t0:t0 + TP].rearrange("(k p) t -> p k t", p=128),
                     [], [("x", k) for k in range(KD)])
            for l in self.layers:
                kind, slot = l % 3, l // 3
                self.sublayer(l, 0, kind, slot, p)
                self.sublayer(l, 1, 3, l, p)
            self.DMA("sp", "xout", self.yT[:, t0:t0 + TP].rearrange("(k p) t -> p k t", p=128), self.xres[:],
                     [("x", k) for k in range(KD)], ["yT"])
        P.wait_all("sp", ["yT"])
        P.emit()
        return nc

    def sublayer(self, l, which, kind, slot, p):
        mb = 48 * which
        sh = lambda k: self.mod[:, l, mb + k:mb + k + 1]
        sc1 = lambda k: self.mod[:, l, mb + 16 + k:mb + 17 + k]
        gt1 = lambda k: self.mod[:, l, mb + 32 + k:mb + 33 + k]
        for k in range(KD):
            self.ACT(self.h[:, k, :], self.xres[:, k, :], AF.Identity, [("x", k), "mod"], [("h", k)], scale=sc1(k), bias=sh(k))
        for k in range(KD):
            self.TS(self.xres[:, k, :], self.xres[:, k, :], ALPHA, None, ALU.mult, None, [("x", k), "stg"], [("x", k)])

        def resid(n, pt, pk):
            self.STT(self.xres[:, n, :], pt[:, :], gt1(n), self.xres[:, n, :], ALU.mult, ALU.add, [pk, ("x", n), "mod"], [("x", n)])

        import os as _os2
        if kind == 3:
            if not _os2.environ.get("MK_SKIP_FFN"):
                self.ffn(l, resid)
        elif kind == 0:
            self.rglru(slot, resid, p)
        elif kind == 2:
            self.gla(resid, p)
        else:
            self.gdn(resid, p)
        if not _os2.environ.get("MK_SKIP_LN"):
            self.layernorm(l, which)

    def hact(self, k):
        return self.h[:, k, :], ("h", k)

    def ffn(self, l, resid):
        w1 = self.din["w_ff1"][l]
        w2 = self.din["w_ff2"][l]

        def relu2(n, pt, pk):
            s, sk = self.sq()
            self.ACT(s[:], pt[:, :], AF.Square, [pk], [sk])
            self.STT(self.U[:, n, :], pt[:, :], 0.0, s[:], ALU.is_gt, ALU.mult, [pk, sk], [("U", n)])

        self.linear(w1, 128, KD, 0, DFF, 128, self.hact, relu2)
        self.linear(w2, 128, 64, 0, D, 128, lambda k: (self.U[:, k, :], ("U", k)), resid)

    def layernorm(self, l, which):
        s1, s1k = self.psum()
        s2, s2k = self.psum()
        for k in range(KD):
            s, sk = self.sq()
            self.ACT(s[:], self.xres[:, k, :], AF.Square, [("x", k)], [sk])
            self.MM(s1[:, :], self.ones[:], self.xres[:, k, :], k == 0, k == KD - 1, ["ones", ("x", k)], [s1k])
            self.MM(s2[:, :], self.ones[:], s[:], k == 0, k == KD - 1, ["ones", sk], [s2k])
        self.TS(self.mean[:], s1[:, :], 1.0 / D, None, ALU.mult, None, [s1k], ["mean"])
        self.TT(self.tmpv[:], self.mean[:], self.mean[:], ALU.mult, ["mean"], ["tmpv"])
        self.STT(self.tmpv[:], s2[:, :], 1.0 / D, self.tmpv[:], ALU.mult, ALU.subtract, [s2k, "tmpv"], ["tmpv"])
        self.TS(self.tmpv[:], self.tmpv[:], 1e-5, None, ALU.add, None, ["tmpv"], ["tmpv"])
        self.ACT(self.tmpv[:], self.tmpv[:], AF.Sqrt, ["tmpv"], ["tmpv"])
        self.P.op("dve", lambda e: e.reciprocal(out=self.rstd[:], in_=self.tmpv[:]), reads=["tmpv"], writes=["rstd"])
        gi = (l * 2 + which) * KD
        for k in range(KD):
            self.TT(self.xres[:, k, :], self.xres[:, k, :], self.mean[:], ALU.subtract, [("x", k), "mean"], [("x", k)])
            self.TT(self.xres[:, k, :], self.xres[:, k, :], self.rstd[:], ALU.mult, [("x", k), "rstd"], [("x", k)])
            self.ACT(self.xres[:, k, :], self.xres[:, k, :], AF.Identity, [("x", k), "g_sb", "b_sb"], [("x", k)],
                     scale=self.g_sb[:, gi + k:gi + k + 1], bias=self.b_sb[:, gi + k:gi + k + 1])

    def rglru(self, s, resid, p):
        w_in = self.r_w_in[s]
        w_out = self.r_w_out[s]
        U = self.U
        SC = self.SC
        def sc(i, w=2 * TP):
            return SC[:80, i * 1024:i * 1024 + w]
        recb = SC[:80, 6144:6144 + 2 * 515].rearrange("p (i t) -> p i t", i=2)
        xs = sc(0).rearrange("p (i t) -> p i t", i=2)
        xr = sc(1).rearrange("p (i t) -> p i t", i=2)
        rg = sc(2).rearrange("p (i t) -> p i t", i=2)
        ig = sc(3).rearrange("p (i t) -> p i t", i=2)
        aa = sc(4).rearrange("p (i t) -> p i t", i=2)
        bb = sc(5).rearrange("p (i t) -> p i t", i=2)
        xrb = self.xrb
        cw = lambda j, c: self.rv[:, s, j, c:c + 1]

        def gate_blk(c, pt, pk):
            x = xs[:, 0, :]
            self.ACT(x, pt[:80, :], AF.Copy, [pk], ["xs"])
            self.ACT(xr[:, 0, :], pt[:80, :], AF.Square, [pk], ["xr"])
            self.TS(xr[:, 0, :], xr[:, 0, :], 0.044715, 1.0, ALU.mult, ALU.add, ["xr"], ["xr"])
            self.TT(xr[:, 0, :], xr[:, 0, :], x, ALU.mult, ["xr", "xs"], ["xr"])
            self.ACT(xr[:, 0, :], xr[:, 0, :], AF.Sigmoid, ["xr"], ["xr"], scale=1.5957691216057308)
            self.TT(U[:80, c, :], xr[:, 0, :], x, ALU.mult, ["xr", "xs"], [("U", c)])

        self.linear(w_in, 128, KD, 0, RW, 80, self.hact, gate_blk)

        def rec_blk(c, pt, pk):
            i = c % 2
            n = c // 2
            self.ACT(recb[:, i, 3:515], pt[:80, :], AF.Copy, [pk], ["recb"])
            self.CP("dve", recb[:, i, 0:3], self.tails[:, s, c, :], ["tails"], ["recb"])
            self.CP("dve", self.tails[:, s, c, :], recb[:, i, 512:515], ["recb"], ["tails"])
            self.TS(xr[:, i, :], recb[:, i, 0:512], cw(0, c), cw(4, c), ALU.mult, ALU.add, ["recb", "rv"], ["xr"])
            for j in (1, 2, 3):
                self.STT(xr[:, i, :], recb[:, i, j:j + 512], cw(j, c), xr[:, i, :], ALU.mult, ALU.add, ["recb", "rv", "xr"], ["xr"])
            if i == 0:
                return
            self.CP("dve", xrb[:, :, :], xr[:, :, :], ["xr"], ["xrb"])
            gwt = self.gw[self.gwi % 2]
            gk = ("gw", self.gwi % 2)
            self.gwi += 1
            self.DMA("pool", f"gw{gk[1]}a", gwt[:, 0, :, :], self.r_wrg[s, n].rearrange("(i p) e -> p i e", p=80), [], [gk])
            self.DMA("pool", f"gw{gk[1]}b", gwt[:, 1, :, :], self.r_wig[s, n].rearrange("(i p) e -> p i e", p=80), [], [gk])
            for g, dst, bj in ((0, rg, 5), (1, ig, 6)):
                for j in range(2):
                    gp, gpk = self.psum()
                    for ii in range(2):
                        self.MM(gp[:80, :], gwt[:, g, ii, j * 80:(j + 1) * 80], xrb[:, ii, :], ii == 0, ii == 1, [gk, "xrb"], [gpk])
                    cc = 2 * n + j
                    self.ACT(dst[:, j, :], gp[:80, :], AF.Sigmoid, [gpk, "rv"], ["rg" if g == 0 else "ig"], bias=self.rv[:, s, bj, cc:cc + 1])
            for j in range(2):
                cc = 2 * n + j
                self.ACT(aa[:, j, :], rg[:, j, :], AF.Exp, ["rg", "cneg"], ["aa"], scale=self.cneg[:, s, 0, cc:cc + 1])
                self.ACT(bb[:, j, :], rg[:, j, :], AF.Exp, ["rg", "cneg"], ["bb"], scale=self.cneg[:, s, 1, cc:cc + 1])
            self.TS(bb[:, :, :], bb[:, :, :], -1.0, 1.0, ALU.mult, ALU.add, ["bb"], ["bb"])
            self.TS(bb[:, :, :], bb[:, :, :], 0.0, None, ALU.max, None, ["bb"], ["bb"])
            self.ACT(bb[:, :, :], bb[:, :, :], AF.Sqrt, ["bb"], ["bb"])
            self.TT(bb[:, :, :], bb[:, :, :], ig[:, :, :], ALU.mult, ["bb", "ig"], ["bb"])
            self.TT(bb[:, :, :], bb[:, :, :], xr[:, :, :], ALU.mult, ["bb", "xr"], ["bb"])
            for j in range(2):
                cc = 2 * n + j
                self.P.op("dve", lambda e, j=j, cc=cc: e.tensor_tensor_scan(out=rg[:, j, :], data0=aa[:, j, :], data1=bb[:, j, :],
                                                                            initial=self.hstate[:, s, cc:cc + 1], op0=ALU.mult, op1=ALU.add),
                          reads=["aa", "bb", "hstate"], writes=["rg"])
                self.CP("dve", self.hstate[:, s, cc:cc + 1], rg[:, j, TP - 1:TP], ["rg"], ["hstate"])
            self.TT(U[:80, 2 * n:2 * n + 2, :], U[:80, 2 * n:2 * n + 2, :], rg[:, :, :], ALU.mult,
                    [("U", 2 * n), ("U", 2 * n + 1), "rg"], [("U", 2 * n), ("U", 2 * n + 1)])

        self.linear(w_in, 128, KD, RW, RW, 80, self.hact, rec_blk)
        self.linear(w_out, 80, RCH, 0, D, 128, lambda k: (U[:80, k, :], ("U", k)), resid)


    def mix_setup(self):
        if hasattr(self, "UF"):
            return
        nc = self.nc
        self.UB = self.U[:].rearrange("p a b -> p (a b)")
        self.UF = self.UB.bitcast(F32)
        self.SCB = self.SC[:].bitcast(BF16)
        self.ST = self.SC[:, 0:4096]
        cst = self.inp("cst", [128, 1024])
        self.cst = self.T("cst_sb", [128, 1024])
        self.identb = self.T("identb", [128, 128], BF16)
        self.DMA("sp", "c1", self.cst[:], cst, [], ["cst"])
        self.CP("dve", self.identb[:], self.cst[:, 0:128], ["cst"], ["identb"])
        self.cstb = self.T("cstb", [128, 1024], BF16)
        self.CP("dve", self.cstb[:], self.cst[:], ["cst"], ["cst"])
        self.identf = self.cst[:, 0:128]
        self.tri_ge = self.cst[:, 128:192]
        self.tri_gt = self.cst[:, 192:256]
        self.cmask = self.cst[:, 256:768]
        self.ones64 = self.cst[:, 768:896]

    def ufs(self, off, n):
        return self.UF[:, off:off + n]

    def ubs(self, off, n):
        return self.UB[:, 2 * off:2 * off + n]

    def gla(self, resid, p):
        self.mix_setup()
        nc = self.nc
        Win = self.inp("gla_w_in", [1, D, 6160])[0]
        Wout = self.inp("gla_w_out", [1, D, D])[0]
        if not hasattr(self, "wup"):
            wupd = self.inp("gla_w_alpha_up", [1, 16, 1024])[0]
            glav = self.inp("glav", [128, 12])
            self.wup = self.T("wup_sb", [16, 1024], BF16)
            self.glv = self.T("glv_sb", [128, 12])
            self.st_gla = nc.dram_tensor("st_gla", [128, 4096], F32).ap()
            self.DMA("pool", "wupd", self.wup[:], wupd, [], ["wup"])
            self.DMA("sp", "c1", self.glv[:], glav, [], ["glv"])
            self.TS(self.glv[:, 0:8], self.glv[:, 0:8], -1.0, None, ALU.mult, None, ["glv"], ["glv"])
        nba = lambda z: self.glv[:, z:z + 1]
        nw = lambda e: self.glv[:, 8 + e:9 + e]
        ST = self.ST
        if p == 0:
            self.P.op("dve", lambda e: e.memset(ST, 0.0), writes=["ST"])
        else:
            self.DMA("sp", "stl_gla", ST, self.st_gla, ["st_gla", ("x", 0)], ["ST"])
        v3 = lambda ap_: ap_.rearrange("p (d t) -> p d t", d=2)
        bc = v3(self.ufs(4096, 1024)); Ep = v3(self.ufs(5120, 1024)); Em = v3(self.ufs(6144, 1024))
        qd = v3(self.ubs(7168, 1024)); ki = v3(self.ubs(7680, 1024)); kt = v3(self.ubs(8192, 1024))
        ktok = self.ubs(8704, 2048)[:64].rearrange("p (c d e) -> p c d e", c=8, d=2)
        vtok = self.ubs(9728, 4096)[:64].rearrange("p (c e) -> p c e", c=8)
        sg = self.ufs(11776, 2048).rearrange("p (a t) -> p a t", a=4)
        t1 = self.ufs(14336, 512); rinv = self.ufs(14848, 512)
        Sbf = v3(self.ubs(15360, 1024))
        AT = self.ubs(15872, 64)[:64]
        alow = self.ubs(15904, 512)[:16]
        Eend = self.ufs(16160, 16).rearrange("p (d c) -> p d c", d=2)

        def alow_blk(n, pt, pk):
            self.CP("act", alow, pt[:16, :], [pk], ["alow"])
        self.linear(Win, 128, KD, 6144, 16, 16, self.hact, alow_blk, cpt=1)
        for hd in range(4):
            Sv = v3(ST[:, hd * 1024:(hd + 1) * 1024])
            for dc in range(2):
                zc = hd * 2 + dc
                zp, zk = self.psum()
                self.MM(zp[:, :], self.wup[:16, zc * 128:(zc + 1) * 128], alow, True, True, ["wup", "alow"], [zk])
                self.ACT(Ep[:, dc, :], zp[:, :], AF.Exp, [zk, "glv"], ["Ep"], scale=-1.0, bias=nba(zc))
                self.ACT(Ep[:, dc, :], Ep[:, dc, :], AF.Ln, ["Ep"], ["Ep"], bias=1.0)
                self.TS(Ep[:, dc, :], Ep[:, dc, :], -1.0 / 16.0, None, ALU.mult, None, ["Ep"], ["Ep"])
                self.P.op("dve", lambda e, dc=dc: e.tensor_tensor_scan(out=bc[:, dc, :], data0=self.cmask, data1=Ep[:, dc, :], initial=0.0,
                                                                       op0=ALU.mult, op1=ALU.add), reads=["Ep", "cst"], writes=["bc"])
            self.ACT(Eend[:, :, :], bc.rearrange("p d (c t) -> p d c t", t=64)[:, :, :, 63], AF.Exp, ["bc"], ["Eend"])
            self.ACT(Ep[:, :, :], bc[:, :, :], AF.Exp, ["bc", "Ep"], ["Ep"])
            self.ACT(Em[:, :, :], bc[:, :, :], AF.Exp, ["bc"], ["Em"], scale=-1.0)

            def q_blk(n, pt, pk):
                self.STT(qd[:, n, :], pt[:, :], 1.0 / 16.0, Ep[:, n, :], ALU.mult, ALU.mult, [pk, "Ep"], ["qd"])
            self.linear(Win, 128, KD, hd * 256, 256, 128, self.hact, q_blk, cpt=2)

            def k_blk(n, pt, pk):
                self.TT(ki[:, n, :], pt[:, :], Em[:, n, :], ALU.mult, [pk, "Em"], ["ki"])
                self.TT(kt[:, n, :].rearrange("p (c t) -> p c t", t=64), ki[:, n, :].rearrange("p (c t) -> p c t", t=64),
                        Eend[:, n, :, None].broadcast_to([128, 8, 64]), ALU.mult, ["ki", "Eend"], ["kt"])
            self.linear(Win, 128, KD, 1024 + hd * 256, 256, 128, self.hact, k_blk, cpt=2)
            for c in range(8):
                for dc in range(2):
                    tp, tk = self.psum()
                    self.MM(tp[:64, 0:128], kt[:, dc, c * 64:(c + 1) * 64], self.identb[:, :], True, True, ["kt", "identb"], [tk])
                    self.CP("act", ktok[:, c, dc, :], tp[:64, 0:128], [tk], ["ktok"])
            wt, wk = self.wtile(Win[:, 2048 + hd * 512:2048 + (hd + 1) * 512].rearrange("(k p) n -> p k n", p=128), 128, 16, 512)
            for c in range(8):
                vp, vk = self.psum()
                for k in range(KD):
                    self.MM(vp[:64, :], self.h[:, k, c * 64:(c + 1) * 64], wt[:, k, :], k == 0, k == KD - 1, [wk, ("h", k)], [vk])
                self.CP("dve", vtok[:, c, :], vp[:64, :], [vk], ["vtok"])

            def g_blk(n, pt, pk):
                self.ACT(sg[:, n, :], pt[:, :], AF.Silu, [pk], ["sg"])
            self.linear(Win, 128, KD, 4096 + hd * 512, 512, 128, self.hact, g_blk, cpt=4)
            self.CP("dve", Sbf[:, :, :], Sv[:, :, :], ["ST"], ["Sbf"])
            ob = [(self.ps[i], ("ps", i)) for i in range(4)]
            for c in range(8):
                cs = slice(c * 64, (c + 1) * 64)
                a_p, a_k = self.ps[6], ("ps", 6)
                for dc in range(2):
                    self.MM(a_p[:64, 0:64], ki[:, dc, cs], qd[:, dc, cs], dc == 0, dc == 1, ["ki", "qd"], [a_k])
                self.TT(AT, a_p[:64, 0:64], self.tri_ge[:64, :], ALU.mult, [a_k, "cst"], ["AT"])
                for ec in range(4):
                    op_, ok_ = ob[ec]
                    for dc in range(2):
                        self.MM(op_[:, cs], Sbf[:, dc, ec * 128:(ec + 1) * 128], qd[:, dc, cs], dc == 0, False, ["Sbf", "qd"], [ok_])
                    self.MM(op_[:, cs], vtok[:, c, ec * 128:(ec + 1) * 128], AT, False, True, ["vtok", "AT"], [ok_])
                for dc in range(2):
                    s_p, s_k = self.ps[4 + dc], ("ps", 4 + dc)
                    self.MM(s_p[:, :], ktok[:, c, dc, :], vtok[:, c, :], True, True, ["ktok", "vtok"], [s_k])
                    self.STT(Sv[:, dc, :], Sv[:, dc, :], Eend[:, dc, c:c + 1], s_p[:, :], ALU.mult, ALU.add, ["ST", "Eend", s_k], ["ST"])
                self.CP("act", Sbf[:, :, :], Sv[:, :, :], ["ST"], ["Sbf"])
            r_p, r_k = self.ps[7], ("ps", 7)
            for ec in range(4):
                s_, sk_ = self.sq()
                self.ACT(s_[:], ob[ec][0][:, :], AF.Square, [ob[ec][1]], [sk_])
                self.MM(r_p[:, :], self.ones[:], s_[:], ec == 0, ec == 3, ["ones", sk_], [r_k])
            self.TS(rinv, r_p[:, :], 1.0 / 512.0, 1e-6, ALU.mult, ALU.add, [r_k], ["rinv"])
            self.ACT(rinv, rinv, AF.Sqrt, ["rinv"], ["rinv"])
            self.P.op("dve", lambda e: e.reciprocal(out=rinv, in_=rinv), reads=["rinv"], writes=["rinv"])
            for ec in range(4):
                self.TT(t1, ob[ec][0][:, :], rinv, ALU.mult, [ob[ec][1], "rinv"], ["t1"])
                self.STT(self.U[:, hd * 4 + ec, :], sg[:, ec, :], nw(ec), t1, ALU.mult, ALU.mult, ["sg", "glv", "t1"], [("U", hd * 4 + ec)])
        self.DMA("sp", "sts_gla", self.st_gla, ST, ["ST"], ["st_gla", "stg"])
        self.linear(Wout, 128, KD, 0, D, 128, lambda k: (self.U[:, k, :], ("U", k)), resid)

    def gdn(self, resid, p):
        self.mix_setup()
        nc = self.nc
        Win = self.inp("gdn_w_in", [1, D, 12352])[0]
        Wout = self.inp("gdn_w_out", [1, 4096, D])[0]
        if not hasattr(self, "gdv"):
            gdnv = self.inp("gdnv", [128, 4 * 64 + 1 + 64])
            self.gdv = self.T("gdv_sb", [128, 4 * 64 + 1 + 64])
            self.gtail = self.T("gtail", [128, 64, 3])
            self.st_gdn = nc.dram_tensor("st_gdn", [128, 4096], F32).ap()
            self.DMA("sp", "c1", self.gdv[:], gdnv, [], ["gdv"])
            self.P.op("dve", lambda e: e.memset(self.gtail[:], 0.0), writes=["gtail"])
            self.ACT(self.gdv[:64, 257:289], self.gdv[:64, 257:289], AF.Exp, ["gdv"], ["gdv"])
            self.TS(self.gdv[:64, 257:289], self.gdv[:64, 257:289], -1.0, None, ALU.mult, None, ["gdv"], ["gdv"])
        cw = lambda j, ch: self.gdv[:, j * 64 + ch:j * 64 + ch + 1]
        normw = self.gdv[:, 256:257]
        negA = self.gdv[:64, 257:289]
        dtb = self.gdv[:64, 289:321]
        ST = self.ST
        if p == 0:
            self.P.op("dve", lambda e: e.memset(ST, 0.0), writes=["ST"])
        else:
            self.DMA("sp", "stl_gdn", ST, self.st_gdn, ["st_gdn", ("x", 0)], ["ST"])
        o = [8192]

        def F(n, parts=128):
            a = self.ufs(o[0], n)[:parts]
            o[0] += (n + 7) // 8 * 8
            return a

        def B(n, parts=128):
            a = self.ubs(o[0], n)[:parts]
            o[0] += ((n + 1) // 2 + 7) // 8 * 8
            return a
        ba = F(512, 64).rearrange("p (c x) -> p c x", c=8)
        beta = F(256, 64).rearrange("p (c x) -> p c x", c=8)
        nbeta = F(256, 64).rearrange("p (c x) -> p c x", c=8)
        gg = F(256, 64).rearrange("p (c x) -> p c x", c=8)
        gc = F(256, 64).rearrange("p (c x) -> p c x", c=8)
        bg = F(256, 64).rearrange("p (c x) -> p c x", c=8)
        tailf = F(256, 64).rearrange("p (c x) -> p c x", c=8)
        dend = F(256).rearrange("p (c x) -> p c x", c=8)
        cbuf = F(515)
        qn = B(512); kn = B(512); vT = B(512)
        acc = F(512); szT = F(512); rn = F(512); t1 = F(512)
        Gs = F(512, 64).rearrange("p (c x) -> p c x", c=8)
        QK = F(512, 64).rearrange("p (c x) -> p c x", c=8)
        ktk = self.SC[:64, 4096:5120].rearrange("p (c x) -> p c x", c=8)
        vbt = self.SC[:64, 5120:5632].bitcast(BF16).rearrange("p (c x) -> p c x", c=8)
        dg = F(64, 64); tmpa = F(64, 64); Dm = F(64, 64); DT = F(64, 64)
        dgh = B(64, 64); dgl = B(64, 64); dgt = F(64, 64)
        Nn = B(64, 64); Mm = B(64, 64); Uu = B(64, 64); N2 = B(64, 64); M2 = B(64, 64)
        kb = B(128, 64); usb = F(128, 64)
        wT = B(64); vn = B(128, 64); Aq = B(64, 64); qdc = B(64); ktt = B(128, 64); Sbf = B(128)
        eq = F(64)
        assert o[0] <= 16384, o[0]

        wt, wk = self.wtile(Win[:, 12288:12352].rearrange("(k p) n -> p k n", p=128), 128, 16, 64)
        for c in range(8):
            bp, bk = self.psum()
            for k in range(KD):
                self.MM(bp[:64, 0:64], self.h[:, k, c * 64:(c + 1) * 64], wt[:, k, 0:64], k == 0, k == KD - 1, [wk, ("h", k)], [bk])
            self.CP("act", ba[:, c, :], bp[:64, 0:64], [bk], ["ba"])
        self.ACT(beta[:, :, :], ba[:, :, 0:32], AF.Sigmoid, ["ba"], ["beta"])
        self.TS(nbeta[:, :, :], beta[:, :, :], -1.0, None, ALU.mult, None, ["beta"], ["nbeta"])
        self.TT(gg[:, :, :], ba[:, :, 32:64], dtb[:, None, :].broadcast_to([64, 8, 32]), ALU.add, ["ba", "gdv"], ["gg"])
        self.ACT(gg[:, :, :], gg[:, :, :], AF.Exp, ["gg"], ["gg"])
        self.ACT(gg[:, :, :], gg[:, :, :], AF.Ln, ["gg"], ["gg"], bias=1.0)
        self.TT(gg[:, :, :], gg[:, :, :], negA[:, None, :].broadcast_to([64, 8, 32]), ALU.mult, ["gg", "gdv"], ["gg"])
        ghi = self.SC[:64, 6144:6272].bitcast(BF16).rearrange("p (c x) -> p c x", c=8)
        glo = self.SC[:64, 6272:6400].bitcast(BF16).rearrange("p (c x) -> p c x", c=8)
        gtmp = self.SC[:64, 6400:6656].rearrange("p (c x) -> p c x", c=8)
        trib = self.cstb[:64, 128:192]
        onesb = self.cstb[:64, 768:896]
        self.CP("dve", ghi, gg[:, :, :], ["gg"], ["ghi"])
        self.TT(gtmp, gg[:, :, :], ghi, ALU.subtract, ["gg", "ghi"], ["gtmp"])
        self.CP("dve", glo, gtmp, ["gtmp"], ["glo"])
        for c in range(8):
            cp_, ck_ = self.psum()
            self.MM(cp_[:64, 0:32], trib, ghi[:, c, :], True, False, ["cst", "ghi"], [ck_])
            self.MM(cp_[:64, 0:32], trib, glo[:, c, :], False, True, ["cst", "glo"], [ck_])
            self.CP("act", gc[:, c, :], cp_[:64, 0:32], [ck_], ["gc"])
            ep_, ek_ = self.psum()
            self.MM(ep_[:, 0:32], onesb, ghi[:, c, :], True, False, ["cst", "ghi"], [ek_])
            self.MM(ep_[:, 0:32], onesb, glo[:, c, :], False, True, ["cst", "glo"], [ek_])
            self.ACT(dend[:, c, :], ep_[:, 0:32], AF.Exp, [ek_], ["dend"])
            self.TT(tailf[:, c, :], ep_[:64, 0:32], gc[:, c, :], ALU.subtract, [ek_, "gc"], ["tailf"])
        self.ACT(tailf[:, :, :], tailf[:, :, :], AF.Exp, ["tailf"], ["tailf"])
        self.ACT(bg[:, :, :], gc[:, :, :], AF.Exp, ["gc"], ["bg"])
        self.TT(bg[:, :, :], bg[:, :, :], beta[:, :, :], ALU.mult, ["bg", "beta"], ["bg"])

        import os as _os3
        _cs = int(_os3.environ.get("GDN_CS", "9"))

        def conv_silu(ch, pt, pk, dst, dkey, l2=None):
            self.ACT(cbuf[:, 3:515], pt[:, :], AF.Copy, [pk], ["cbuf"])
            if _cs <= 1:
                return
            self.CP("dve", cbuf[:, 0:3], self.gtail[:, ch, :], ["gtail"], ["cbuf"])
            self.CP("dve", self.gtail[:, ch, :], cbuf[:, 512:515], ["cbuf"], ["gtail"])
            self.TS(acc, cbuf[:, 0:512], cw(0, ch), None, ALU.mult, None, ["cbuf", "gdv"], ["acc"])
            for j in (1, 2, 3):
                self.STT(acc, cbuf[:, j:j + 512], cw(j, ch), acc, ALU.mult, ALU.add, ["cbuf", "gdv", "acc"], ["acc"])
            if _cs <= 2:
                return
            if l2 is None:
                self.ACT(dst, acc, AF.Silu, ["acc"], [dkey])
                return
            self.ACT(acc, acc, AF.Silu, ["acc"], ["acc"])
            if _cs <= 3:
                return
            s_, sk_ = self.sq()
            self.ACT(s_[:], acc, AF.Square, ["acc"], [sk_])
            lp, lk = self.psum()
            self.MM(lp[:, :], self.ones[:], s_[:], True, True, ["ones", sk_], [lk])
            if _cs <= 4:
                return
            self.TS(rn, lp[:, :], 1e-6, None, ALU.add, None, [lk], ["rn"])
            if _cs <= 5:
                return
            self.ACT(rn, rn, AF.Sqrt, ["rn"], ["rn"])
            if _cs <= 6:
                return
            self.P.op("dve", lambda e: e.reciprocal(out=rn, in_=rn), reads=["rn"], writes=["rn"])
            if _cs <= 7:
                return
            self.STT(dst, acc, l2, rn, ALU.mult, ALU.mult, ["acc", "rn"], [dkey])

        import os as _os
        _dbg = int(_os.environ.get("GDN_DBG", "0"))
        for hq in range(16 if _dbg != 3 else 0):
            self.linear(Win, 128, KD, hq * 128, 128, 128, self.hact,
                        lambda n, pt, pk: conv_silu(hq, pt, pk, qn, "qn", l2=128.0 ** -0.5), cpt=1)
            self.linear(Win, 128, KD, 2048 + hq * 128, 128, 128, self.hact,
                        lambda n, pt, pk: conv_silu(16 + hq, pt, pk, kn, "kn", l2=1.0), cpt=1)
            for c in range(8 if _dbg != 5 else 0):
                cs = slice(c * 64, (c + 1) * 64)
                g_p, g_k = self.psum()
                if _dbg not in (6, 7, 8):
                    self.MM(g_p[:64, 0:64], kn[:, cs], kn[:, cs], True, True, ["kn"], [g_k])
                if _dbg != 8:
                    self.MM(g_p[:64, 64:128], kn[:, cs], qn[:, cs], True, True, ["kn", "qn"], [g_k])
                if _dbg != 7:
                    self.MM(g_p[:64, 128:256], kn[:, cs], self.identb[:, :], True, True, ["kn", "identb"], [g_k])
                if _dbg not in (6, 7, 8):
                    self.CP("act", Gs[:, c, :], g_p[:64, 0:64], [g_k], ["Gs"])
                if _dbg != 8:
                    self.CP("act", QK[:, c, :], g_p[:64, 64:128], [g_k], ["QK"])
                if _dbg != 7:
                    self.CP("act", ktk[:, c, :], g_p[:64, 128:256], [g_k], ["ktk"])
            for h in ((2 * hq, 2 * hq + 1) if _dbg not in (4, 5, 6, 7, 8) else ()):
                self.linear(Win, 128, KD, 4096 + h * 128, 128, 128, self.hact,
                            lambda n, pt, pk: conv_silu(32 + h, pt, pk, vT, "vT"), cpt=1)
                self.linear(Win, 128, KD, 8192 + h * 128, 128, 128, self.hact,
                            lambda n, pt, pk: self.ACT(szT, pt[:, :], AF.Silu, [pk], ["szT"]), cpt=1)
                for c in range(8):
                    v_p, v_k = self.psum()
                    self.MM(v_p[:64, 0:128], vT[:, c * 64:(c + 1) * 64], self.identb[:, :], True, True, ["vT", "identb"], [v_k])
                    self.TS(vbt[:, c, :], v_p[:64, 0:128], beta[:, c, h:h + 1], None, ALU.mult, None, [v_k, "beta"], ["vbt"])
                Sv = ST[:, h * 128:(h + 1) * 128]
                self.CP("dve", Sbf, Sv, ["ST"], ["Sbf"])
                o_p, o_k = self.ps[0], ("ps", 0)
                for c in range(8 if _dbg != 1 else 0):
                    cs = slice(c * 64, (c + 1) * 64)
                    gci = gc[:, c, h:h + 1]
                    P1, K1 = self.ps[1], ("ps", 1)
                    P2, K2 = self.ps[2], ("ps", 2)
                    P3, K3 = self.ps[3], ("ps", 3)
                    self.TS(dg, self.identf[:64, 0:64], gci, None, ALU.mult, None, ["cst", "gc"], ["dg"])
                    self.CP("dve", dgh, dg, ["dg"], ["dgh"])
                    self.TT(dgt, dg, dgh, ALU.subtract, ["dg", "dgh"], ["dgt"])
                    self.CP("dve", dgl, dgt, ["dgt"], ["dgl"])
                    self.MM(P1[:, 0:64], onesb, dgh, True, False, ["cst", "dgh"], [K1])
                    self.MM(P1[:, 0:64], onesb, dgl, False, True, ["cst", "dgl"], [K1])
                    self.TS(tmpa, P1[:64, 0:64], gci, 0.0, ALU.subtract, ALU.max, [K1, "gc"], ["tmpa"])
                    self.ACT(Dm, tmpa, AF.Exp, ["tmpa"], ["Dm"], scale=-1.0)
                    self.TS(tmpa, P1[:64, 0:64], gci, 0.0, ALU.subtract, ALU.min, [K1, "gc"], ["tmpa"])
                    self.ACT(DT, tmpa, AF.Exp, ["tmpa"], ["DT"])
                    self.ACT(eq, P1[:, 0:64], AF.Exp, [K1], ["eq"])
                    self.TT(qdc, qn[:, cs], eq, ALU.mult, ["qn", "eq"], ["qdc"])
                    self.TT(Nn, Gs[:, c, :], Dm, ALU.mult, ["Gs", "Dm"], ["Nn"])
                    self.STT(Nn, Nn, nbeta[:, c, h:h + 1], self.tri_gt[:64, :], ALU.mult, ALU.mult, ["Nn", "nbeta", "cst"], ["Nn"])
                    self.MM(P2[:64, 0:64], Nn, self.identb[:64, 0:64], True, True, ["Nn", "identb"], [K2])
                    self.CP("act", Mm, P2[:64, 0:64], [K2], ["Mm"])
                    self.TT(Uu, Mm, self.identf[:64, 0:64], ALU.add, ["Mm", "cst"], ["Uu"])
                    for lvl in range(5):
                        self.MM(P2[:64, 64:128], Mm, Nn, True, True, ["Mm", "Nn"], [K2])
                        if lvl < 4:
                            self.MM(P2[:64, 128:192], Nn, Mm, True, True, ["Mm", "Nn"], [K2])
                        self.CP("act", N2, P2[:64, 64:128], [K2], ["N2"])
                        if lvl < 4:
                            self.CP("act", M2, P2[:64, 128:192], [K2], ["M2"])
                        self.MM(P3[:64, 0:64], N2, Uu, True, True, ["N2", "Uu"], [K3])
                        self.TT(Uu, Uu, P3[:64, 0:64], ALU.add, ["Uu", K3], ["Uu"])
                        self.CP("act", Nn, N2, ["N2"], ["Nn"])
                        if lvl < 4:
                            self.CP("dve", Mm, M2, ["M2"], ["Mm"])
                    if _dbg == 2:
                        continue
                    self.TS(kb, ktk[:, c, :], bg[:, c, h:h + 1], None, ALU.mult, None, ["ktk", "bg"], ["kb"])
                    self.MM(P2[:, 256:320], kb, Uu, True, True, ["kb", "Uu"], [K2])
                    self.CP("act", wT, P2[:, 256:320], [K2], ["wT"])
                    self.MM(P3[:64, 128:256], Uu, vbt[:, c, :], True, True, ["Uu", "vbt"], [K3])
                    self.CP("act", usb, P3[:64, 128:256], [K3], ["usb"])
                    self.MM(P3[:64, 256:384], wT, Sbf, True, True, ["wT", "Sbf"], [K3])
                    self.TT(vn, usb, P3[:64, 256:384], ALU.subtract, ["usb", K3], ["vn"])
                    self.TT(tmpa, QK[:, c, :], DT, ALU.mult, ["QK", "DT"], ["tmpa"])
                    self.TT(Aq, tmpa, self.tri_ge[:64, :], ALU.mult, ["tmpa", "cst"], ["Aq"])
                    self.MM(o_p[:, cs], Sbf, qdc, True, False, ["Sbf", "qdc"], [o_k])
                    self.MM(o_p[:, cs], vn, Aq, False, True, ["vn", "Aq"], [o_k])
                    self.TS(ktt, ktk[:, c, :], tailf[:, c, h:h + 1], None, ALU.mult, None, ["ktk", "tailf"], ["ktt"])
                    self.MM(P2[:, 384:512], ktt, vn, True, True, ["ktt", "vn"], [K2])
                    self.STT(Sv, Sv, dend[:, c, h:h + 1], P2[:, 384:512], ALU.mult, ALU.add, ["ST", "dend", K2], ["ST"])
                    self.CP("act", Sbf, Sv, ["ST"], ["Sbf"])
                s_, sk_ = self.sq()
                self.ACT(s_[:], o_p[:, :], AF.Square, [o_k], [sk_])
                r_p, r_k = self.ps[7], ("ps", 7)
                self.MM(r_p[:, :], self.ones[:], s_[:], True, True, ["ones", sk_], [r_k])
                self.TS(rn, r_p[:, :], 1.0 / 128.0, 1e-6, ALU.mult, ALU.add, [r_k], ["rn"])
                self.ACT(rn, rn, AF.Sqrt, ["rn"], ["rn"])
                self.P.op("dve", lambda e: e.reciprocal(out=rn, in_=rn), reads=["rn"], writes=["rn"])
                self.TT(t1, o_p[:, :], rn, ALU.mult, [o_k, "rn"], ["t1"])
                self.STT(self.U[:, h, :], szT, normw, t1, ALU.mult, ALU.mult, ["szT", "gdv", "t1"], [("U", h)])
        self.DMA("sp", "sts_gdn", self.st_gdn, ST, ["ST"], ["st_gdn", "stg"])
        self.linear(Wout, 128, 32, 0, D, 128, lambda k: (self.U[:, k, :], ("U", k)), resid)


_CACHE = {}


def _vecP(v, p=128):
    v = np.asarray(v, np.float32)
    lead = v.shape[:-1]
    c = v.shape[-1] // p
    v = v.reshape(*lead, c, p)
    v = np.moveaxis(v, -1, 0)
    return np.ascontiguousarray(v.reshape(p, -1))


def make_inputs(inputs, b):
    f = lambda a: np.ascontiguousarray(np.asarray(a, np.float32))
    m = {}
    m["xT"] = np.ascontiguousarray(np.asarray(inputs["x"][b], np.float32).T)
    m["cT"] = _vecP(inputs["c"][b])
    m["lng"] = _vecP(inputs["ln_g"])
    m["lnb"] = _vecP(inputs["ln_b"])
    m["bmod"] = _vecP(inputs["b_mod"])
    for k in ("w_mod", "w_ff1", "w_ff2", "rglru_w_in", "rglru_w_out", "rglru_w_rgate", "rglru_w_igate",
              "gla_w_in", "gla_w_out", "gla_w_alpha_up", "gdn_w_in", "gdn_w_out"):
        m[k] = f(inputs[k])
    cst = np.zeros((128, 1024), np.float32)
    cst[:, 0:128] = np.eye(128, dtype=np.float32)
    pp = np.arange(128)[:, None]
    ff = np.arange(64)[None, :]
    cst[:, 128:192] = (ff >= pp).astype(np.float32)
    cst[:, 192:256] = (ff < pp).astype(np.float32)
    cst[:, 256:768] = ((np.arange(512) % 64) != 0).astype(np.float32)[None, :]
    cst[:, 768:896] = 1.0
    m["cst"] = cst
    m["glav"] = np.concatenate([_vecP(inputs["gla_b_alpha"][0]), _vecP(inputs["gla_norm_w"][0])], axis=1)
    gd = np.zeros((128, 321), np.float32)
    gd[:, 0:256] = _vecP(inputs["gdn_conv_w"][0])
    gd[:, 256] = np.asarray(inputs["gdn_norm_w"][0], np.float32)
    gd[:, 257:289] = np.asarray(inputs["gdn_a_log"][0], np.float32)[None, :]
    gd[:, 289:321] = np.asarray(inputs["gdn_dt_bias"][0], np.float32)[None, :]
    m["gdnv"] = gd
    cwv = np.asarray(inputs["rglru_conv_w"], np.float32)
    parts = [cwv[:, 0], cwv[:, 1], cwv[:, 2], cwv[:, 3], np.asarray(inputs["rglru_conv_b"], np.float32),
             np.asarray(inputs["rglru_b_rgate"], np.float32).reshape(2, RW), np.asarray(inputs["rglru_b_igate"], np.float32).reshape(2, RW),
             np.asarray(inputs["rglru_lambda"], np.float32)]
    rv = np.stack(parts, axis=1)
    m["rvec"] = _vecP(rv, 80)
    return m


def kernel(**inputs):
    layers = tuple(inputs.pop("_layers", range(DEPTH)))
    n_pass = int(inputs.pop("_n_pass", SEQ // TP))
    ncores = int(inputs.pop("_cores", 8))
    key = (layers, n_pass)
    if key not in _CACHE:
        mk = MK(layers, n_pass)
        _CACHE[key] = (mk.build(), set(mk.din.keys()))
    nc, names = _CACHE[key]
    full = [make_inputs(inputs, b) for b in range(min(4, ncores))]
    in_maps = [{k: v for k, v in full[i % len(full)].items() if k in names} for i in range(ncores)]
    res = run_bass_kernel_spmd(nc, in_maps, core_ids=list(range(ncores)))
    nb = min(4, ncores)
    out = np.stack([np.ascontiguousarray(res.results[b]["yT"].T) for b in range(nb)], axis=0)
    return out.astype(np.float32)
```

```python
import numpy as np
import concourse.bass as bass
import concourse.mybir as mybir
from concourse.bass_utils import run_bass_kernel_spmd

F32 = mybir.dt.float32
BF16 = mybir.dt.bfloat16
ALU = mybir.AluOpType
AF = mybir.ActivationFunctionType

ENGS = ("pe", "act", "dve", "pool", "sp")

D = 2048
KD = 16
SEQ = 2048
TP = 512
DEPTH = 4
ALPHA = float((2 * DEPTH) ** 0.25)
DFF = 8192
RW = 2560
RCH = 32
NWB = 2


class Prog:
    def __init__(self, nc):
        self.nc = nc
        self.q = {e: [] for e in ENGS}
        self.cnt = {e: 0 for e in ENGS}
        self.sem = {e: nc.alloc_semaphore(name=f"c_{e}") for e in ENGS if e != "sp"}
        self.last_w = {}
        self.readers = {}
        self.seen = {e: {} for e in ENGS}
        self.slots = {}

    def _deps(self, eng, reads, writes):
        deps = []
        for k in reads:
            w = self.last_w.get(k)
            if w is not None:
                deps.append(w)
            if isinstance(k, tuple) and k[0] == "ps":
                deps.extend(t for t in self.readers.get(k, ()) if t[0] != eng)
        for k in writes:
            w = self.last_w.get(k)
            if w is not None:
                deps.append(w)
            deps.extend(self.readers.get(k, ()))
        waits = {}
        for (src, val) in deps:
            if src == "pe" and eng == "pe":
                continue
            if val > waits.get(src, 0):
                waits[src] = val
        out = []
        for src, val in waits.items():
            if self.seen[eng].get(src, 0) >= val:
                continue
            self.seen[eng][src] = val
            out.append((src, val))
        return out

    def _commit(self, tok, reads, writes):
        for k in writes:
            self.last_w[k] = tok
            self.readers[k] = []
        for k in reads:
            self.readers.setdefault(k, []).append(tok)

    def op(self, eng, fn, reads=(), writes=()):
        waits = self._deps(eng, reads, writes)
        self.cnt[eng] += 1
        tok = (eng, self.cnt[eng])
        self.q[eng].append((waits, fn, eng))
        self._commit(tok, reads, writes)

    def dma(self, eng, slot, fn, reads=(), writes=()):
        if slot not in self.slots:
            self.slots[slot] = [self.nc.alloc_semaphore(name=f"d_{slot}"), 0]
        waits = self._deps(eng, reads, writes)
        s = self.slots[slot]
        s[1] += 16
        tok = (("slot", slot), s[1])
        self.q[eng].append((waits, fn, ("slot", slot)))
        self._commit(tok, reads, writes)

    def wait_all(self, eng, keys):
        waits = self._deps(eng, keys, ())
        self.q[eng].append((waits, None, None))

    def _semof(self, src):
        if isinstance(src, tuple):
            return self.slots[src[1]][0]
        return self.sem[src]

    def emit(self):
        nc = self.nc
        prog = self
        emap = {"pe": "tensor", "act": "scalar", "dve": "vector", "pool": "gpsimd", "sp": "sync"}
        with nc.Block() as block:
            for e in ENGS:
                def body(engobj, e=e):
                    for waits, fn, kind in prog.q[e]:
                        for src, val in waits:
                            engobj.wait_ge(prog._semof(src), val)
                        if fn is None:
                            continue
                        ins = fn(engobj)
                        if isinstance(kind, tuple):
                            ins.then_inc(prog.slots[kind[1]][0], 16)
                        else:
                            ins.then_inc(prog.sem[e], 1)
                getattr(block, emap[e])(body)


class MK:
    def __init__(self, layers=tuple(range(DEPTH)), n_pass=SEQ // TP):
        self.layers = tuple(layers)
        self.n_layers = len(self.layers)
        self.n_pass = n_pass
        nc = self.nc = bass.Bass("TRN2", target_bir_lowering=False)
        self.P = Prog(nc)
        self.din = {}
        self.wi = 0
        self.pi = 0
        self.sqi = 0

    def inp(self, name, shape):
        if name not in self.din:
            self.din[name] = self.nc.dram_tensor(name, list(shape), F32, kind="ExternalInput").ap()
        return self.din[name]

    def T(self, name, shape, dt=F32):
        return self.nc.alloc_sbuf_tensor(name, list(shape), dt)

    def ACT(self, out, in_, func, r, w, scale=None, bias=None):
        kw = {}
        if scale is not None:
            kw["scale"] = scale
        if bias is not None:
            kw["bias"] = bias
        self.P.op("act", lambda e: e.activation(out=out, in_=in_, func=func, **kw), reads=r, writes=w)

    def TS(self, out, in0, s1, s2, op0, op1, r, w, eng="dve"):
        if op1 is None:
            self.P.op(eng, lambda e: e.tensor_scalar(out=out, in0=in0, scalar1=s1, scalar2=None, op0=op0), reads=r, writes=w)
        else:
            self.P.op(eng, lambda e: e.tensor_scalar(out=out, in0=in0, scalar1=s1, scalar2=s2, op0=op0, op1=op1), reads=r, writes=w)

    def TT(self, out, in0, in1, op, r, w, eng="dve"):
        self.P.op(eng, lambda e: e.tensor_tensor(out=out, in0=in0, in1=in1, op=op), reads=r, writes=w)

    def STT(self, out, in0, scalar, in1, op0, op1, r, w):
        self.P.op("dve", lambda e: e.scalar_tensor_tensor(out=out, in0=in0, scalar=scalar, in1=in1, op0=op0, op1=op1), reads=r, writes=w)

    def CP(self, eng, out, in_, r, w):
        if eng == "act":
            self.P.op("act", lambda e: e.copy(out=out, in_=in_), reads=r, writes=w)
        else:
            self.P.op(eng, lambda e: e.tensor_copy(out=out, in_=in_), reads=r, writes=w)

    def MM(self, out, lhsT, rhs, start, stop, r, w):
        self.P.op("pe", lambda e: e.matmul(out, lhsT=lhsT, rhs=rhs, start=start, stop=stop), reads=r, writes=w)

    def DMA(self, eng, slot, out, in_, r, w):
        if slot in ("c0", "c1"):
            self.cslot = getattr(self, "cslot", 0) + 1
            slot = f"{slot}_{self.cslot}"
        self.P.dma(eng, slot, lambda e: e.dma_start(out=out, in_=in_), reads=r, writes=w)

    def psum(self):
        i = self.pi % 8
        self.pi += 1
        return self.ps[i], ("ps", i)

    def sq(self):
        i = self.sqi % 2
        self.sqi += 1
        return self.sqb[i], ("sq", i)

    def wtile(self, src, kp, kc, ncols):
        i = self.wi % NWB
        self.wi += 1
        t = self.wb[i]
        key = ("wb", i)
        self.DMA("pool", f"wb{i}", t[:kp, :kc, :ncols], src, [], [key])
        return t, key

    def linear(self, W, kp, KC, col0, ncols, ocw, act, on_block, blk0=0, cpt=4):
        tcols = ocw * cpt
        assert ncols % tcols == 0, (ncols, tcols)
        nkg = (KC + 15) // 16
        for og in range(ncols // tcols):
            banks = [self.psum() for _ in range(cpt)]
            for kg in range(nkg):
                kc = min(16, KC - kg * 16)
                c0 = col0 + og * tcols
                src = W[kg * 16 * kp:(kg * 16 + kc) * kp, c0:c0 + tcols].rearrange("(k p) n -> p k n", p=kp)
                t, wkey = self.wtile(src, kp, kc, tcols)
                for j in range(cpt):
                    pt, pk = banks[j]
                    for k in range(kc):
                        a, akey = act(kg * 16 + k)
                        self.MM(pt[:ocw, :], t[:kp, k, j * ocw:(j + 1) * ocw], a,
                                (kg == 0 and k == 0), (kg == nkg - 1 and k == kc - 1), [wkey, akey], [pk])
            for j in range(cpt):
                on_block(blk0 + og * cpt + j, banks[j][0], banks[j][1])

    def build(self):
        nc, P = self.nc, self.P
        L = self.n_layers
        xT = self.inp("xT", [D, SEQ])
        cT = self.inp("cT", [128, KD])
        lng = self.inp("lng", [128, DEPTH * 2 * KD])
        lnb = self.inp("lnb", [128, DEPTH * 2 * KD])
        bmod = self.inp("bmod", [128, DEPTH * 96])
        w_mod = self.inp("w_mod", [DEPTH, D, 6 * D])
        w_ff1 = self.inp("w_ff1", [DEPTH, D, DFF])
        w_ff2 = self.inp("w_ff2", [DEPTH, DFF, D])
        if any(l % 3 == 0 for l in self.layers):
            self.r_w_in = self.inp("rglru_w_in", [2, D, 2 * RW])
            self.r_w_out = self.inp("rglru_w_out", [2, RW, D])
            self.r_wrg = self.inp("rglru_w_rgate", [2, 16, 160, 160])
            self.r_wig = self.inp("rglru_w_igate", [2, 16, 160, 160])
        rvec = self.inp("rvec", [80, 2 * 8 * RCH])
        self.yT = nc.dram_tensor("yT", [D, SEQ], F32, kind="ExternalOutput").ap()

        self.xres = self.T("xres", [128, KD, TP])
        self.h = self.T("h", [128, KD, TP], BF16)
        self.U = self.T("U", [128, 64, TP], BF16)
        self.wb = [self.T(f"wb{i}", [128, 16, 512], BF16) for i in range(NWB)]
        self.ps = [nc.alloc_psum_tensor(f"ps{i}", [128, 512], F32) for i in range(8)]
        self.sqb = [self.T(f"sq{i}", [128, TP]) for i in range(2)]
        self.ones = self.T("ones", [128, 128])
        self.mean = self.T("mean", [128, TP])
        self.rstd = self.T("rstd", [128, TP])
        self.tmpv = self.T("tmpv", [128, TP])
        self.cact = self.T("cact", [128, KD], BF16)
        self.cf = self.T("cf", [128, KD])
        self.mod = self.T("mod", [128, DEPTH, 96])
        self.bm = self.T("bm", [128, DEPTH * 96])
        self.g_sb = self.T("g_sb", [128, DEPTH * 2 * KD])
        self.b_sb = self.T("b_sb", [128, DEPTH * 2 * KD])
        self.SC = self.T("SC", [128, 7200])
        self.rv = self.T("rv", [80, 2, 8, RCH])
        self.cneg = self.T("cneg", [80, 2, 2, RCH])
        self.hstate = self.T("hstate", [80, 2, RCH])
        self.tails = self.T("tails", [80, 2, RCH, 3])
        self.gw = [self.T(f"gw{i}", [80, 2, 2, 160], BF16) for i in range(2)]
        self.gwi = 0
        self.xrb = self.T("xrb", [80, 2, TP], BF16)

        P.op("dve", lambda e: e.memset(self.ones[:], 1.0), writes=["ones"])
        P.op("dve", lambda e: e.memset(self.hstate[:], 0.0), writes=["hstate"])
        P.op("dve", lambda e: e.memset(self.tails[:], 0.0), writes=["tails"])
        self.DMA("sp", "c0", self.cf[:], cT, [], ["cf"])
        self.DMA("sp", "c0", self.bm[:], bmod, [], ["bm"])
        self.DMA("sp", "c0", self.g_sb[:], lng, [], ["g_sb"])
        self.DMA("sp", "c0", self.b_sb[:], lnb, [], ["b_sb"])
        self.DMA("sp", "c0", self.rv[:].rearrange("p a b c -> p (a b c)"), rvec, [], ["rv"])
        self.ACT(self.cact[:], self.cf[:], AF.Silu, ["cf"], ["cact"])
        for s in range(2):
            self.ACT(self.cneg[:, s, 0, :], self.rv[:, s, 7, :], AF.Exp, ["rv"], ["cneg"], scale=-1.0)
            self.ACT(self.cneg[:, s, 0, :], self.cneg[:, s, 0, :], AF.Ln, ["cneg"], ["cneg"], bias=1.0)
            self.TS(self.cneg[:, s, 1, :], self.cneg[:, s, 0, :], -16.0, None, ALU.mult, None, ["cneg"], ["cneg"])
            self.TS(self.cneg[:, s, 0, :], self.cneg[:, s, 0, :], -8.0, None, ALU.mult, None, ["cneg"], ["cneg"])

        for l in self.layers:
            pm, pmk = self.psum()

            def act_c(k):
                return self.cact[:, k:k + 1], "cact"

            for og in range(24):
                t, wkey = self.wtile(w_mod[l][:, og * 512:(og + 1) * 512].rearrange("(k p) n -> p k n", p=128), 128, 16, 512)
                for j in range(4):
                    col = og * 4 + j
                    for k in range(16):
                        self.MM(pm[:, col:col + 1], t[:, k, j * 128:(j + 1) * 128], self.cact[:, k:k + 1], k == 0, k == 15, [wkey, "cact"], [pmk])
            self.TT(self.mod[:, l, :], pm[:, 0:96], self.bm[:, l * 96:(l + 1) * 96], ALU.add, [pmk, "bm"], ["mod"])
            for a in (16, 32, 64, 80):
                self.TS(self.mod[:, l, a:a + 16], self.mod[:, l, a:a + 16], 1.0, None, ALU.add, None, ["mod"], ["mod"])

        for p in range(self.n_pass):
            t0 = p * TP
            self.DMA("sp", "xin", self.xres[:], xT[:, t0:t0 + TP].rearrange("(k p) t -> p k t", p=128),
                     [], [("x", k) for k in range(KD)])
            for l in self.layers:
                kind, slot = l % 3, l // 3
                self.sublayer(l, 0, kind, slot, p)
                self.sublayer(l, 1, 3, l, p)
            self.DMA("sp", "xout", self.yT[:, t0:t0 + TP].rearrange("(k p) t -> p k t", p=128), self.xres[:],
                     [("x", k) for k in range(KD)], ["yT"])
        P.wait_all("sp", ["yT"])
        P.emit()
        return nc

    def sublayer(self, l, which, kind, slot, p):
        mb = 48 * which
        sh = lambda k: self.mod[:, l, mb + k:mb + k + 1]
        sc1 = lambda k: self.mod[:, l, mb + 16 + k:mb + 17 + k]
        gt1 = lambda k: self.mod[:, l, mb + 32 + k:mb + 33 + k]
        for k in range(KD):
            self.ACT(self.h[:, k, :], self.xres[:, k, :], AF.Identity, [("x", k), "mod"], [("h", k)], scale=sc1(k), bias=sh(k))
        for k in range(KD):
            self.TS(self.xres[:, k, :], self.xres[:, k, :], ALPHA, None, ALU.mult, None, [("x", k), "stg"], [("x", k)])

        def resid(n, pt, pk):
            self.STT(self.xres[:, n, :], pt[:, :], gt1(n), self.xres[:, n, :], ALU.mult, ALU.add, [pk, ("x", n), "mod"], [("x", n)])

        import os as _os2
        if kind == 3:
            if not _os2.environ.get("MK_SKIP_FFN"):
                self.ffn(l, resid)
        elif kind == 0:
            self.rglru(slot, resid, p)
        elif kind == 2:
            self.gla(resid, p)
        else:
            self.gdn(resid, p)
        if not _os2.environ.get("MK_SKIP_LN"):
            self.layernorm(l, which)

    def hact(self, k):
        return self.h[:, k, :], ("h", k)

    def ffn(self, l, resid):
        w1 = self.din["w_ff1"][l]
        w2 = self.din["w_ff2"][l]

        def relu2(n, pt, pk):
            s, sk = self.sq()
            self.ACT(s[:], pt[:, :], AF.Square, [pk], [sk])
            self.STT(self.U[:, n, :], pt[:, :], 0.0, s[:], ALU.is_gt, ALU.mult, [pk, sk], [("U", n)])

        self.linear(w1, 128, KD, 0, DFF, 128, self.hact, relu2)
        self.linear(w2, 128, 64, 0, D, 128, lambda k: (self.U[:, k, :], ("U", k)), resid)

    def layernorm(self, l, which):
        s1, s1k = self.psum()
        s2, s2k = self.psum()
        for k in range(KD):
            s, sk = self.sq()
            self.ACT(s[:], self.xres[:, k, :], AF.Square, [("x", k)], [sk])
            self.MM(s1[:, :], self.ones[:], self.xres[:, k, :], k == 0, k == KD - 1, ["ones", ("x", k)], [s1k])
            self.MM(s2[:, :], self.ones[:], s[:], k == 0, k == KD - 1, ["ones", sk], [s2k])
        self.TS(self.mean[:], s1[:, :], 1.0 / D, None, ALU.mult, None, [s1k], ["mean"])
        self.TT(self.tmpv[:], self.mean[:], self.mean[:], ALU.mult, ["mean"], ["tmpv"])
        self.STT(self.tmpv[:], s2[:, :], 1.0 / D, self.tmpv[:], ALU.mult, ALU.subtract, [s2k, "tmpv"], ["tmpv"])
        self.TS(self.tmpv[:], self.tmpv[:], 1e-5, None, ALU.add, None, ["tmpv"], ["tmpv"])
        self.ACT(self.tmpv[:], self.tmpv[:], AF.Sqrt, ["tmpv"], ["tmpv"])
        self.P.op("dve", lambda e: e.reciprocal(out=self.rstd[:], in_=self.tmpv[:]), reads=["tmpv"], writes=["rstd"])
        gi = (l * 2 + which) * KD
        for k in range(KD):
            self.TT(self.xres[:, k, :], self.xres[:, k, :], self.mean[:], ALU.subtract, [("x", k), "mean"], [("x", k)])
            self.TT(self.xres[:, k, :], self.xres[:, k, :], self.rstd[:], ALU.mult, [("x", k), "rstd"], [("x", k)])
            self.ACT(self.xres[:, k, :], self.xres[:, k, :], AF.Identity, [("x", k), "g_sb", "b_sb"], [("x", k)],
                     scale=self.g_sb[:, gi + k:gi + k + 1], bias=self.b_sb[:, gi + k:gi + k + 1])

    def rglru(self, s, resid, p):
        w_in = self.r_w_in[s]
        w_out = self.r_w_out[s]
        U = self.U
        SC = self.SC
        def sc(i, w=2 * TP):
            return SC[:80, i * 1024:i * 1024 + w]
        recb = SC[:80, 6144:6144 + 2 * 515].rearrange("p (i t) -> p i t", i=2)
        xs = sc(0).rearrange("p (i t) -> p i t", i=2)
        xr = sc(1).rearrange("p (i t) -> p i t", i=2)
        rg = sc(2).rearrange("p (i t) -> p i t", i=2)
        ig = sc(3).rearrange("p (i t) -> p i t", i=2)
        aa = sc(4).rearrange("p (i t) -> p i t", i=2)
        bb = sc(5).rearrange("p (i t) -> p i t", i=2)
        xrb = self.xrb
        cw = lambda j, c: self.rv[:, s, j, c:c + 1]

        def gate_blk(c, pt, pk):
            x = xs[:, 0, :]
            self.ACT(x, pt[:80, :], AF.Copy, [pk], ["xs"])
            self.ACT(xr[:, 0, :], pt[:80, :], AF.Square, [pk], ["xr"])
            self.TS(xr[:, 0, :], xr[:, 0, :], 0.044715, 1.0, ALU.mult, ALU.add, ["xr"], ["xr"])
            self.TT(xr[:, 0, :], xr[:, 0, :], x, ALU.mult, ["xr", "xs"], ["xr"])
            self.ACT(xr[:, 0, :], xr[:, 0, :], AF.Sigmoid, ["xr"], ["xr"], scale=1.5957691216057308)
            self.TT(U[:80, c, :], xr[:, 0, :], x, ALU.mult, ["xr", "xs"], [("U", c)])

        self.linear(w_in, 128, KD, 0, RW, 80, self.hact, gate_blk)

        def rec_blk(c, pt, pk):
            i = c % 2
            n = c // 2
            self.ACT(recb[:, i, 3:515], pt[:80, :], AF.Copy, [pk], ["recb"])
            self.CP("dve", recb[:, i, 0:3], self.tails[:, s, c, :], ["tails"], ["recb"])
            self.CP("dve", self.tails[:, s, c, :], recb[:, i, 512:515], ["recb"], ["tails"])
            self.TS(xr[:, i, :], recb[:, i, 0:512], cw(0, c), cw(4, c), ALU.mult, ALU.add, ["recb", "rv"], ["xr"])
            for j in (1, 2, 3):
                self.STT(xr[:, i, :], recb[:, i, j:j + 512], cw(j, c), xr[:, i, :], ALU.mult, ALU.add, ["recb", "rv", "xr"], ["xr"])
            if i == 0:
                return
            self.CP("dve", xrb[:, :, :], xr[:, :, :], ["xr"], ["xrb"])
            gwt = self.gw[self.gwi % 2]
            gk = ("gw", self.gwi % 2)
            self.gwi += 1
            self.DMA("pool", f"gw{gk[1]}a", gwt[:, 0, :, :], self.r_wrg[s, n].rearrange("(i p) e -> p i e", p=80), [], [gk])
            self.DMA("pool", f"gw{gk[1]}b", gwt[:, 1, :, :], self.r_wig[s, n].rearrange("(i p) e -> p i e", p=80), [], [gk])
            for g, dst, bj in ((0, rg, 5), (1, ig, 6)):
                for j in range(2):
                    gp, gpk = self.psum()
                    for ii in range(2):
                        self.MM(gp[:80, :], gwt[:, g, ii, j * 80:(j + 1) * 80], xrb[:, ii, :], ii == 0, ii == 1, [gk, "xrb"], [gpk])
                    cc = 2 * n + j
                    self.ACT(dst[:, j, :], gp[:80, :], AF.Sigmoid, [gpk, "rv"], ["rg" if g == 0 else "ig"], bias=self.rv[:, s, bj, cc:cc + 1])
            for j in range(2):
                cc = 2 * n + j
                self.ACT(aa[:, j, :], rg[:, j, :], AF.Exp, ["rg", "cneg"], ["aa"], scale=self.cneg[:, s, 0, cc:cc + 1])
                self.ACT(bb[:, j, :], rg[:, j, :], AF.Exp, ["rg", "cneg"], ["bb"], scale=self.cneg[:, s, 1, cc:cc + 1])
            self.TS(bb[:, :, :], bb[:, :, :], -1.0, 1.0, ALU.mult, ALU.add, ["bb"], ["bb"])
            self.TS(bb[:, :, :], bb[:, :, :], 0.0, None, ALU.max, None, ["bb"], ["bb"])
            self.ACT(bb[:, :, :], bb[:, :, :], AF.Sqrt, ["bb"], ["bb"])
            self.TT(bb[:, :, :], bb[:, :, :], ig[:, :, :], ALU.mult, ["bb", "ig"], ["bb"])
            self.TT(bb[:, :, :], bb[:, :, :], xr[:, :, :], ALU.mult, ["bb", "xr"], ["bb"])
            for j in range(2):
                cc = 2 * n + j
                self.P.op("dve", lambda e, j=j, cc=cc: e.tensor_tensor_scan(out=rg[:, j, :], data0=aa[:, j, :], data1=bb[:, j, :],
                                                                            initial=self.hstate[:, s, cc:cc + 1], op0=ALU.mult, op1=ALU.add),
                          reads=["aa", "bb", "hstate"], writes=["rg"])
                self.CP("dve", self.hstate[:, s, cc:cc + 1], rg[:, j, TP - 1:TP], ["rg"], ["hstate"])
            self.TT(U[:80, 2 * n:2 * n + 2, :], U[:80, 2 * n:2 * n + 2, :], rg[:, :, :], ALU.mult,
                    [("U", 2 * n), ("U", 2 * n + 1), "rg"], [("U", 2 * n), ("U", 2 * n + 1)])

        self.linear(w_in, 128, KD, RW, RW, 80, self.hact, rec_blk)
        self.linear(w_out, 80, RCH, 0, D, 128, lambda k: (U[:80, k, :], ("U", k)), resid)


    def mix_setup(self):
        if hasattr(self, "UF"):
            return
        nc = self.nc
        self.UB = self.U[:].rearrange("p a b -> p (a b)")
        self.UF = self.UB.bitcast(F32)
        self.SCB = self.SC[:].bitcast(BF16)
        self.ST = self.SC[:, 0:4096]
        cst = self.inp("cst", [128, 1024])
        self.cst = self.T("cst_sb", [128, 1024])
        self.identb = self.T("identb", [128, 128], BF16)
        self.DMA("sp", "c1", self.cst[:], cst, [], ["cst"])
        self.CP("dve", self.identb[:], self.cst[:, 0:128], ["cst"], ["identb"])
        self.cstb = self.T("cstb", [128, 1024], BF16)
        self.CP("dve", self.cstb[:], self.cst[:], ["cst"], ["cst"])
        self.identf = self.cst[:, 0:128]
        self.tri_ge = self.cst[:, 128:192]
        self.tri_gt = self.cst[:, 192:256]
        self.ntri_gt = self.cst[:, 896:960]
        self.cmask = self.cst[:, 256:768]
        self.ones64 = self.cst[:, 768:896]

    def ufs(self, off, n):
        return self.UF[:, off:off + n]

    def ubs(self, off, n):
        return self.UB[:, 2 * off:2 * off + n]

    def gla(self, resid, p):
        self.mix_setup()
        nc = self.nc
        Win = self.inp("gla_w_in", [1, D, 6160])[0]
        Wout = self.inp("gla_w_out", [1, D, D])[0]
        if not hasattr(self, "wup"):
            wupd = self.inp("gla_w_alpha_up", [1, 16, 1024])[0]
            glav = self.inp("glav", [128, 12])
            self.wup = self.T("wup_sb", [16, 1024], BF16)
            self.glv = self.T("glv_sb", [128, 12])
            self.st_gla = nc.dram_tensor("st_gla", [128, 4096], F32).ap()
            self.DMA("pool", "wupd", self.wup[:], wupd, [], ["wup"])
            self.DMA("sp", "c1", self.glv[:], glav, [], ["glv"])
            self.TS(self.glv[:, 0:8], self.glv[:, 0:8], -1.0, None, ALU.mult, None, ["glv"], ["glv"])
        nba = lambda z: self.glv[:, z:z + 1]
        nw = lambda e: self.glv[:, 8 + e:9 + e]
        ST = self.ST
        if p == 0:
            self.P.op("dve", lambda e: e.memset(ST, 0.0), writes=["ST"])
        else:
            self.DMA("sp", "stl_gla", ST, self.st_gla, ["st_gla", ("x", 0)], ["ST"])
        v3 = lambda ap_: ap_.rearrange("p (d t) -> p d t", d=2)
        bc = v3(self.ufs(4096, 1024)); Ep = v3(self.ufs(5120, 1024)); Em = v3(self.ufs(6144, 1024))
        qd = v3(self.ubs(7168, 1024)); ki = v3(self.ubs(7680, 1024)); kt = v3(self.ubs(8192, 1024))
        ktok = self.ubs(8704, 2048)[:64].rearrange("p (c d e) -> p c d e", c=8, d=2)
        vtok = self.ubs(9728, 4096)[:64].rearrange("p (c e) -> p c e", c=8)
        sg = self.ufs(11776, 2048).rearrange("p (a t) -> p a t", a=4)
        t1 = self.ufs(14336, 512); rinv = self.ufs(14848, 512)
        Sbf = v3(self.ubs(15360, 1024))
        AT = self.ubs(15872, 64)[:64]
        alow = self.ubs(15904, 512)[:16]
        Eend = self.ufs(16160, 16).rearrange("p (d c) -> p d c", d=2)

        def alow_blk(n, pt, pk):
            self.CP("act", alow, pt[:16, :], [pk], ["alow"])
        self.linear(Win, 128, KD, 6144, 16, 16, self.hact, alow_blk, cpt=1)
        for hd in range(4):
            Sv = v3(ST[:, hd * 1024:(hd + 1) * 1024])
            for dc in range(2):
                zc = hd * 2 + dc
                zp, zk = self.psum()
                self.MM(zp[:, :], self.wup[:16, zc * 128:(zc + 1) * 128], alow, True, True, ["wup", "alow"], [zk])
                self.ACT(Ep[:, dc, :], zp[:, :], AF.Exp, [zk, "glv"], ["Ep"], scale=-1.0, bias=nba(zc))
                self.ACT(Ep[:, dc, :], Ep[:, dc, :], AF.Ln, ["Ep"], ["Ep"], bias=1.0)
                self.TS(Ep[:, dc, :], Ep[:, dc, :], -1.0 / 16.0, None, ALU.mult, None, ["Ep"], ["Ep"])
                self.P.op("dve", lambda e, dc=dc: e.tensor_tensor_scan(out=bc[:, dc, :], data0=self.cmask, data1=Ep[:, dc, :], initial=0.0,
                                                                       op0=ALU.mult, op1=ALU.add), reads=["Ep", "cst"], writes=["bc"])
            self.ACT(Eend[:, :, :], bc.rearrange("p d (c t) -> p d c t", t=64)[:, :, :, 63], AF.Exp, ["bc"], ["Eend"])
            self.ACT(Ep[:, :, :], bc[:, :, :], AF.Exp, ["bc", "Ep"], ["Ep"])
            self.ACT(Em[:, :, :], bc[:, :, :], AF.Exp, ["bc"], ["Em"], scale=-1.0)

            def q_blk(n, pt, pk):
                self.STT(qd[:, n, :], pt[:, :], 1.0 / 16.0, Ep[:, n, :], ALU.mult, ALU.mult, [pk, "Ep"], ["qd"])
            self.linear(Win, 128, KD, hd * 256, 256, 128, self.hact, q_blk, cpt=2)

            def k_blk(n, pt, pk):
                self.TT(ki[:, n, :], pt[:, :], Em[:, n, :], ALU.mult, [pk, "Em"], ["ki"])
                self.TT(kt[:, n, :].rearrange("p (c t) -> p c t", t=64), ki[:, n, :].rearrange("p (c t) -> p c t", t=64),
                        Eend[:, n, :, None].broadcast_to([128, 8, 64]), ALU.mult, ["ki", "Eend"], ["kt"])
            self.linear(Win, 128, KD, 1024 + hd * 256, 256, 128, self.hact, k_blk, cpt=2)
            for c in range(8):
                for dc in range(2):
                    tp, tk = self.psum()
                    self.MM(tp[:64, 0:128], kt[:, dc, c * 64:(c + 1) * 64], self.identb[:, :], True, True, ["kt", "identb"], [tk])
                    self.CP("act", ktok[:, c, dc, :], tp[:64, 0:128], [tk], ["ktok"])
            wt, wk = self.wtile(Win[:, 2048 + hd * 512:2048 + (hd + 1) * 512].rearrange("(k p) n -> p k n", p=128), 128, 16, 512)
            for c in range(8):
                vp, vk = self.psum()
                for k in range(KD):
                    self.MM(vp[:64, :], self.h[:, k, c * 64:(c + 1) * 64], wt[:, k, :], k == 0, k == KD - 1, [wk, ("h", k)], [vk])
                self.CP("dve", vtok[:, c, :], vp[:64, :], [vk], ["vtok"])

            def g_blk(n, pt, pk):
                self.ACT(sg[:, n, :], pt[:, :], AF.Silu, [pk], ["sg"])
            self.linear(Win, 128, KD, 4096 + hd * 512, 512, 128, self.hact, g_blk, cpt=4)
            self.CP("dve", Sbf[:, :, :], Sv[:, :, :], ["ST"], ["Sbf"])
            ob = [(self.ps[i], ("ps", i)) for i in range(4)]
            for c in range(8):
                cs = slice(c * 64, (c + 1) * 64)
                a_p, a_k = self.ps[6], ("ps", 6)
                for dc in range(2):
                    self.MM(a_p[:64, 0:64], ki[:, dc, cs], qd[:, dc, cs], dc == 0, dc == 1, ["ki", "qd"], [a_k])
                self.TT(AT, a_p[:64, 0:64], self.tri_ge[:64, :], ALU.mult, [a_k, "cst"], ["AT"])
                for ec in range(4):
                    op_, ok_ = ob[ec]
                    for dc in range(2):
                        self.MM(op_[:, cs], Sbf[:, dc, ec * 128:(ec + 1) * 128], qd[:, dc, cs], dc == 0, False, ["Sbf", "qd"], [ok_])
                    self.MM(op_[:, cs], vtok[:, c, ec * 128:(ec + 1) * 128], AT, False, True, ["vtok", "AT"], [ok_])
                for dc in range(2):
                    s_p, s_k = self.ps[4 + dc], ("ps", 4 + dc)
                    self.MM(s_p[:, :], ktok[:, c, dc, :], vtok[:, c, :], True, True, ["ktok", "vtok"], [s_k])
                    self.STT(Sv[:, dc, :], Sv[:, dc, :], Eend[:, dc, c:c + 1], s_p[:, :], ALU.mult, ALU.add, ["ST", "Eend", s_k], ["ST"])
                self.CP("act", Sbf[:, :, :], Sv[:, :, :], ["ST"], ["Sbf"])
            r_p, r_k = self.ps[7], ("ps", 7)
            for ec in range(4):
                s_, sk_ = self.sq()
                self.ACT(s_[:], ob[ec][0][:, :], AF.Square, [ob[ec][1]], [sk_])
                self.MM(r_p[:, :], self.ones[:], s_[:], ec == 0, ec == 3, ["ones", sk_], [r_k])
            self.TS(rinv, r_p[:, :], 1.0 / 512.0, 1e-6, ALU.mult, ALU.add, [r_k], ["rinv"])
            self.ACT(rinv, rinv, AF.Sqrt, ["rinv"], ["rinv"])
            self.P.op("dve", lambda e: e.reciprocal(out=rinv, in_=rinv), reads=["rinv"], writes=["rinv"])
            for ec in range(4):
                self.TT(t1, ob[ec][0][:, :], rinv, ALU.mult, [ob[ec][1], "rinv"], ["t1"])
                self.STT(self.U[:, hd * 4 + ec, :], sg[:, ec, :], nw(ec), t1, ALU.mult, ALU.mult, ["sg", "glv", "t1"], [("U", hd * 4 + ec)])
        self.DMA("sp", "sts_gla", self.st_gla, ST, ["ST"], ["st_gla", "stg"])
        self.linear(Wout, 128, KD, 0, D, 128, lambda k: (self.U[:, k, :], ("U", k)), resid)

    def gdn(self, resid, p):
        self.mix_setup()
        nc = self.nc
        Win = self.inp("gdn_w_in", [1, D, 12352])[0]
        Wout = self.inp("gdn_w_out", [1, 4096, D])[0]
        if not hasattr(self, "gdv"):
            gdnv = self.inp("gdnv", [128, 4 * 64 + 1 + 64])
            self.gdv = self.T("gdv_sb", [128, 4 * 64 + 1 + 64])
            self.gtail = self.T("gtail", [128, 64, 3])
            self.st_gdn = nc.dram_tensor("st_gdn", [128, 4096], F32).ap()
            self.DMA("sp", "c1", self.gdv[:], gdnv, [], ["gdv"])
            self.P.op("dve", lambda e: e.memset(self.gtail[:], 0.0), writes=["gtail"])
            self.ACT(self.gdv[:64, 257:289], self.gdv[:64, 257:289], AF.Exp, ["gdv"], ["gdv"])
            self.TS(self.gdv[:64, 257:289], self.gdv[:64, 257:289], -1.0, None, ALU.mult, None, ["gdv"], ["gdv"])
        cw = lambda j, ch: self.gdv[:, j * 64 + ch:j * 64 + ch + 1]
        normw = self.gdv[:, 256:257]
        negA = self.gdv[:64, 257:289]
        dtb = self.gdv[:64, 289:321]
        ST = self.ST
        if p == 0:
            self.P.op("dve", lambda e: e.memset(ST, 0.0), writes=[("ST", h_) for h_ in range(32)])
        else:
            self.DMA("sp", "stl_gdn", ST, self.st_gdn, ["st_gdn", ("x", 0)], [("ST", h_) for h_ in range(32)])
        o = [8192]

        def F(n, parts=128):
            a = self.ufs(o[0], n)[:parts]
            o[0] += (n + 7) // 8 * 8
            return a

        def B(n, parts=128):
            a = self.ubs(o[0], n)[:parts]
            o[0] += ((n + 1) // 2 + 7) // 8 * 8
            return a
        r8 = lambda ap_: ap_.rearrange("p (c x) -> p c x", c=8)
        ba = r8(F(512, 64))
        beta = r8(F(256, 64))
        gg = r8(self.SC[:64, 6656:6912])
        gc = r8(F(256, 64))
        bg = r8(F(256, 64))
        tailf = r8(F(256, 64))
        dend = r8(self.SC[:, 6912:7168])
        cbuf = F(515)
        qn = B(512); kn = B(512); vT = B(512)
        acc = F(512); rn = F(512); t1 = F(512)
        Gs = r8(F(512, 64))
        QK = r8(F(512, 64))
        ktk = r8(self.SC[:64, 4096:5120])
        sets = []
        for si in range(2):
            S_ = {}
            S_["vbt"] = r8(self.SC[:64, 5120 + 512 * si:5632 + 512 * si].bitcast(BF16))
            S_["szT"] = B(512)
            for nm in ("dg", "tmpa", "Dm", "DT", "dgt"):
                S_[nm] = F(64, 64)
            for nm in ("dgh", "dgl", "Na", "Nb", "Ma", "Mb", "Uu", "Aq"):
                S_[nm] = B(64, 64)
            S_["kb"] = B(128, 64); S_["usb"] = F(128, 64); S_["wT"] = B(64); S_["vn"] = B(128, 64)
            S_["qdc"] = B(64); S_["ktt"] = B(128, 64); S_["Sbf"] = B(128); S_["eq"] = F(64)
            sets.append(S_)
        assert o[0] <= 16384, o[0]

        wt, wk = self.wtile(Win[:, 12288:12352].rearrange("(k p) n -> p k n", p=128), 128, 16, 64)
        for c in range(8):
            bp, bk = self.psum()
            for k in range(KD):
                self.MM(bp[:64, 0:64], self.h[:, k, c * 64:(c + 1) * 64], wt[:, k, 0:64], k == 0, k == KD - 1, [wk, ("h", k)], [bk])
            self.CP("act", ba[:, c, :], bp[:64, 0:64], [bk], ["ba"])
        self.ACT(beta[:, :, :], ba[:, :, 0:32], AF.Sigmoid, ["ba"], ["beta"])
        self.TT(gg[:, :, :], ba[:, :, 32:64], dtb[:, None, :].broadcast_to([64, 8, 32]), ALU.add, ["ba", "gdv"], ["gg"])
        self.ACT(gg[:, :, :], gg[:, :, :], AF.Exp, ["gg"], ["gg"])
        self.ACT(gg[:, :, :], gg[:, :, :], AF.Ln, ["gg"], ["gg"], bias=1.0)
        self.TT(gg[:, :, :], gg[:, :, :], negA[:, None, :].broadcast_to([64, 8, 32]), ALU.mult, ["gg", "gdv"], ["gg"])
        ghi = self.SC[:64, 6144:6272].bitcast(BF16).rearrange("p (c x) -> p c x", c=8)
        glo = self.SC[:64, 6272:6400].bitcast(BF16).rearrange("p (c x) -> p c x", c=8)
        gtmp = self.SC[:64, 6400:6656].rearrange("p (c x) -> p c x", c=8)
        trib = self.cstb[:64, 128:192]
        onesb = self.cstb[:64, 768:896]
        self.CP("dve", ghi, gg[:, :, :], ["gg"], ["ghi"])
        self.TT(gtmp, gg[:, :, :], ghi, ALU.subtract, ["gg", "ghi"], ["gtmp"])
        self.CP("dve", glo, gtmp, ["gtmp"], ["glo"])
        for c in range(8):
            cp_, ck_ = self.psum()
            self.MM(cp_[:64, 0:32], trib, ghi[:, c, :], True, False, ["cst", "ghi"], [ck_])
            self.MM(cp_[:64, 0:32], trib, glo[:, c, :], False, True, ["cst", "glo"], [ck_])
            self.CP("act", gc[:, c, :], cp_[:64, 0:32], [ck_], ["gc"])
            ep_, ek_ = self.psum()
            self.MM(ep_[:, 0:32], onesb, ghi[:, c, :], True, False, ["cst", "ghi"], [ek_])
            self.MM(ep_[:, 0:32], onesb, glo[:, c, :], False, True, ["cst", "glo"], [ek_])
            self.ACT(dend[:, c, :], ep_[:, 0:32], AF.Exp, [ek_], ["dend"])
            self.TT(tailf[:, c, :], ep_[:64, 0:32], gc[:, c, :], ALU.subtract, [ek_, "gc"], ["tailf"])
        self.ACT(tailf[:, :, :], tailf[:, :, :], AF.Exp, ["tailf"], ["tailf"])
        self.ACT(bg[:, :, :], gc[:, :, :], AF.Exp, ["gc"], ["bg"])
        self.TT(bg[:, :, :], bg[:, :, :], beta[:, :, :], ALU.mult, ["bg", "beta"], ["bg"])

        import os as _os3
        _cs = int(_os3.environ.get("GDN_CS", "9"))

        def conv_silu(ch, pt, pk, dst, dkey, l2=None):
            self.ACT(cbuf[:, 3:515], pt[:, :], AF.Copy, [pk], ["cbuf"])
            if _cs <= 1:
                return
            self.CP("dve", cbuf[:, 0:3], self.gtail[:, ch, :], ["gtail"], ["cbuf"])
            self.CP("dve", self.gtail[:, ch, :], cbuf[:, 512:515], ["cbuf"], ["gtail"])
            self.TS(acc, cbuf[:, 0:512], cw(0, ch), None, ALU.mult, None, ["cbuf", "gdv"], ["acc"])
            for j in (1, 2, 3):
                self.STT(acc, cbuf[:, j:j + 512], cw(j, ch), acc, ALU.mult, ALU.add, ["cbuf", "gdv", "acc"], ["acc"])
            if _cs <= 2:
                return
            if l2 is None:
                self.ACT(dst, acc, AF.Silu, ["acc"], [dkey])
                return
            self.ACT(acc, acc, AF.Silu, ["acc"], ["acc"])
            if _cs <= 3:
                return
            s_, sk_ = self.sq()
            self.ACT(s_[:], acc, AF.Square, ["acc"], [sk_])
            lp, lk = self.psum()
            self.MM(lp[:, :], self.ones[:], s_[:], True, True, ["ones", sk_], [lk])
            if _cs <= 4:
                return
            self.TS(rn, lp[:, :], 1e-6, None, ALU.add, None, [lk], ["rn"])
            if _cs <= 5:
                return
            self.ACT(rn, rn, AF.Sqrt, ["rn"], ["rn"])
            if _cs <= 6:
                return
            self.P.op("dve", lambda e: e.reciprocal(out=rn, in_=rn), reads=["rn"], writes=["rn"])
            if _cs <= 7:
                return
            self.STT(dst, acc, l2, rn, ALU.mult, ALU.mult, ["acc", "rn"], [dkey])

        import os as _os
        _dbg = int(_os.environ.get("GDN_DBG", "0"))
        for hq in range(16 if _dbg != 3 else 0):
            self.linear(Win, 128, KD, hq * 128, 128, 128, self.hact,
                        lambda n, pt, pk: conv_silu(hq, pt, pk, qn, "qn", l2=128.0 ** -0.5), cpt=1)
            self.linear(Win, 128, KD, 2048 + hq * 128, 128, 128, self.hact,
                        lambda n, pt, pk: conv_silu(16 + hq, pt, pk, kn, "kn", l2=1.0), cpt=1)
            for c in range(8 if _dbg != 5 else 0):
                cs = slice(c * 64, (c + 1) * 64)
                g_p, g_k = self.psum()
                if _dbg not in (6, 7, 8):
                    self.MM(g_p[:64, 0:64], kn[:, cs], kn[:, cs], True, True, ["kn"], [g_k])
                if _dbg != 8:
                    self.MM(g_p[:64, 64:128], kn[:, cs], qn[:, cs], True, True, ["kn", "qn"], [g_k])
                if _dbg != 7:
                    self.MM(g_p[:64, 128:256], kn[:, cs], self.identb[:, :], True, True, ["kn", "identb"], [g_k])
                if _dbg not in (6, 7, 8):
                    self.CP("act", Gs[:, c, :], g_p[:64, 0:64], [g_k], ["Gs"])
                if _dbg != 8:
                    self.CP("act", QK[:, c, :], g_p[:64, 64:128], [g_k], ["QK"])
                if _dbg != 7:
                    self.CP("act", ktk[:, c, :], g_p[:64, 128:256], [g_k], ["ktk"])
            hs = (2 * hq, 2 * hq + 1) if _dbg not in (4, 5, 6, 7, 8) else ()
            for si, h in enumerate(hs):
                S_ = sets[si]
                self.linear(Win, 128, KD, 4096 + h * 128, 128, 128, self.hact,
                            lambda n, pt, pk: conv_silu(32 + h, pt, pk, vT, "vT"), cpt=1)
                self.linear(Win, 128, KD, 8192 + h * 128, 128, 128, self.hact,
                            lambda n, pt, pk: self.ACT(S_["szT"], pt[:, :], AF.Silu, [pk], [f"szT{si}"]), cpt=1)
                for c in range(8):
                    v_p, v_k = self.psum()
                    self.MM(v_p[:64, 0:128], vT[:, c * 64:(c + 1) * 64], self.identb[:, :], True, True, ["vT", "identb"], [v_k])
                    self.TS(S_["vbt"][:, c, :], v_p[:64, 0:128], beta[:, c, h:h + 1], None, ALU.mult, None, [v_k, "beta"], [f"vbt{si}"])

            def chain(si, h):
                S_ = sets[si]
                k_ = lambda nm: f"{nm}{si}"
                dg, tmpa, Dm, DT, dgt, dgh, dgl = S_["dg"], S_["tmpa"], S_["Dm"], S_["DT"], S_["dgt"], S_["dgh"], S_["dgl"]
                NN, MM_, Uu = [S_["Na"], S_["Nb"]], [S_["Ma"], S_["Mb"]], S_["Uu"]
                kb, usb, wT, vn, Aq, qdc, ktt, Sbf, eq = (S_[x] for x in ("kb", "usb", "wT", "vn", "Aq", "qdc", "ktt", "Sbf", "eq"))
                vbt_, szT_ = S_["vbt"], S_["szT"]
                b0 = 4 * si
                o_p, o_k = self.ps[b0], ("ps", b0)
                P1, K1 = self.ps[b0 + 1], ("ps", b0 + 1)
                P2, K2 = self.ps[b0 + 2], ("ps", b0 + 2)
                P3, K3 = self.ps[b0 + 3], ("ps", b0 + 3)
                Sv = ST[:, h * 128:(h + 1) * 128]
                sk = ("ST", h)
                self.CP("dve", Sbf, Sv, [sk], [k_("Sbf")])
                yield
                for c in range(8 if _dbg != 1 else 0):
                    cs = slice(c * 64, (c + 1) * 64)
                    gci = gc[:, c, h:h + 1]
                    self.TS(dg, self.identf[:64, 0:64], gci, None, ALU.mult, None, ["cst", "gc"], [k_("dg")]); yield
                    self.CP("dve", dgh, dg, [k_("dg")], [k_("dgh")]); yield
                    self.TT(dgt, dg, dgh, ALU.subtract, [k_("dg"), k_("dgh")], [k_("dgt")]); yield
                    self.CP("dve", dgl, dgt, [k_("dgt")], [k_("dgl")]); yield
                    self.MM(P1[:, 0:64], onesb, dgh, True, False, ["cst", k_("dgh")], [K1])
                    self.MM(P1[:, 0:64], onesb, dgl, False, True, ["cst", k_("dgl")], [K1]); yield
                    self.TS(tmpa, P1[:64, 0:64], gci, 0.0, ALU.subtract, ALU.max, [K1, "gc"], [k_("tmpa")]); yield
                    self.ACT(Dm, tmpa, AF.Exp, [k_("tmpa")], [k_("Dm")], scale=-1.0); yield
                    self.TS(DT, P1[:64, 0:64], gci, 0.0, ALU.subtract, ALU.min, [K1, "gc"], [k_("DT")]); yield
                    self.ACT(DT, DT, AF.Exp, [k_("DT")], [k_("DT")]); yield
                    self.ACT(eq, P1[:, 0:64], AF.Exp, [K1], [k_("eq")]); yield
                    self.TT(qdc, qn[:, cs], eq, ALU.mult, ["qn", k_("eq")], [k_("qdc")]); yield
                    Nn, Mm = NN[0], MM_[0]
                    self.TT(Nn, Gs[:, c, :], Dm, ALU.mult, ["Gs", k_("Dm")], [k_("N0")]); yield
                    self.STT(Nn, Nn, beta[:, c, h:h + 1], self.ntri_gt[:64, :], ALU.mult, ALU.mult, [k_("N0"), "beta", "cst"], [k_("N0")]); yield
                    self.MM(P2[:64, 0:64], Nn, self.identb[:64, 0:64], True, True, [k_("N0"), "identb"], [K2]); yield
                    self.CP("act", Mm, P2[:64, 0:64], [K2], [k_("M0")]); yield
                    self.TT(Uu, Mm, self.identf[:64, 0:64], ALU.add, [k_("M0"), "cst"], [k_("Uu")]); yield
                    cur = 0
                    for lvl in range(5):
                        nx = 1 - cur
                        Nc, Mc, Nx, Mx = NN[cur], MM_[cur], NN[nx], MM_[nx]
                        kN, kM, kNx, kMx = k_(f"N{cur}"), k_(f"M{cur}"), k_(f"N{nx}"), k_(f"M{nx}")
                        self.MM(P2[:64, 64:128], Mc, Nc, True, True, [kM, kN], [K2])
                        if lvl < 4:
                            self.MM(P2[:64, 128:192], Nc, Mc, True, True, [kM, kN], [K2])
                        yield
                        self.CP("act", Nx, P2[:64, 64:128], [K2], [kNx]); yield
                        if lvl < 4:
                            self.CP("act", Mx, P2[:64, 128:192], [K2], [kMx]); yield
                        self.MM(P3[:64, 0:64], Nx, Uu, True, True, [kNx, k_("Uu")], [K3]); yield
                        self.TT(Uu, Uu, P3[:64, 0:64], ALU.add, [k_("Uu"), K3], [k_("Uu")]); yield
                        cur = nx
                    self.TS(kb, ktk[:, c, :], bg[:, c, h:h + 1], None, ALU.mult, None, ["ktk", "bg"], [k_("kb")]); yield
                    self.MM(P2[:, 256:320], kb, Uu, True, True, [k_("kb"), k_("Uu")], [K2]); yield
                    self.CP("act", wT, P2[:, 256:320], [K2], [k_("wT")]); yield
                    self.MM(P3[:64, 128:256], Uu, vbt_[:, c, :], True, True, [k_("Uu"), k_("vbt")], [K3]); yield
                    self.CP("act", usb, P3[:64, 128:256], [K3], [k_("usb")]); yield
                    self.MM(P3[:64, 256:384], wT, Sbf, True, True, [k_("wT"), k_("Sbf")], [K3]); yield
                    self.TT(vn, usb, P3[:64, 256:384], ALU.subtract, [k_("usb"), K3], [k_("vn")]); yield
                    self.TT(tmpa, QK[:, c, :], DT, ALU.mult, ["QK", k_("DT")], [k_("tmpa")]); yield
                    self.TT(Aq, tmpa, self.tri_ge[:64, :], ALU.mult, [k_("tmpa"), "cst"], [k_("Aq")]); yield
                    self.MM(o_p[:, cs], Sbf, qdc, True, False, [k_("Sbf"), k_("qdc")], [o_k])
                    self.MM(o_p[:, cs], vn, Aq, False, True, [k_("vn"), k_("Aq")], [o_k]); yield
                    self.TS(ktt, ktk[:, c, :], tailf[:, c, h:h + 1], None, ALU.mult, None, ["ktk", "tailf"], [k_("ktt")]); yield
                    self.MM(P2[:, 384:512], ktt, vn, True, True, [k_("ktt"), k_("vn")], [K2]); yield
                    self.STT(Sv, Sv, dend[:, c, h:h + 1], P2[:, 384:512], ALU.mult, ALU.add, [sk, "dend", K2], [sk]); yield
                    self.CP("act", Sbf, Sv, [sk], [k_("Sbf")]); yield
                s_, sk_ = self.sq()
                self.ACT(s_[:], o_p[:, :], AF.Square, [o_k], [sk_])
                self.MM(P1[:, :], self.ones[:], s_[:], True, True, ["ones", sk_], [K1])
                self.TS(rn, P1[:, :], 1.0 / 128.0, 1e-6, ALU.mult, ALU.add, [K1], ["rn"])
                self.ACT(rn, rn, AF.Sqrt, ["rn"], ["rn"])
                self.P.op("dve", lambda e: e.reciprocal(out=rn, in_=rn), reads=["rn"], writes=["rn"])
                self.TT(t1, o_p[:, :], rn, ALU.mult, [o_k, "rn"], ["t1"])
                self.STT(self.U[:, h, :], szT_, normw, t1, ALU.mult, ALU.mult, [k_("szT"), "gdv", "t1"], [("U", h)])
                yield

            gens = [chain(si, h) for si, h in enumerate(hs)]
            while gens:
                for g_ in list(gens):
                    try:
                        next(g_)
                    except StopIteration:
                        gens.remove(g_)
        self.DMA("sp", "sts_gdn", self.st_gdn, ST, [("ST", h_) for h_ in range(32)], ["st_gdn", "stg"])
        self.linear(Wout, 128, 32, 0, D, 128, lambda k: (self.U[:, k, :], ("U", k)), resid)


_CACHE = {}


def _vecP(v, p=128):
    v = np.asarray(v, np.float32)
    lead = v.shape[:-1]
    c = v.shape[-1] // p
    v = v.reshape(*lead, c, p)
    v = np.moveaxis(v, -1, 0)
    return np.ascontiguousarray(v.reshape(p, -1))


def make_inputs(inputs, b):
    f = lambda a: np.ascontiguousarray(np.asarray(a, np.float32))
    m = {}
    m["xT"] = np.ascontiguousarray(np.asarray(inputs["x"][b], np.float32).T)
    m["cT"] = _vecP(inputs["c"][b])
    m["lng"] = _vecP(inputs["ln_g"])
    m["lnb"] = _vecP(inputs["ln_b"])
    m["bmod"] = _vecP(inputs["b_mod"])
    for k in ("w_mod", "w_ff1", "w_ff2", "rglru_w_in", "rglru_w_out", "rglru_w_rgate", "rglru_w_igate",
              "gla_w_in", "gla_w_out", "gla_w_alpha_up", "gdn_w_in", "gdn_w_out"):
        m[k] = f(inputs[k])
    cst = np.zeros((128, 1024), np.float32)
    cst[:, 0:128] = np.eye(128, dtype=np.float32)
    pp = np.arange(128)[:, None]
    ff = np.arange(64)[None, :]
    cst[:, 128:192] = (ff >= pp).astype(np.float32)
    cst[:, 192:256] = (ff < pp).astype(np.float32)
    cst[:, 256:768] = ((np.arange(512) % 64) != 0).astype(np.float32)[None, :]
    cst[:, 768:896] = 1.0
    cst[:, 896:960] = -(ff < pp).astype(np.float32)
    m["cst"] = cst
    m["glav"] = np.concatenate([_vecP(inputs["gla_b_alpha"][0]), _vecP(inputs["gla_norm_w"][0])], axis=1)
    gd = np.zeros((128, 321), np.float32)
    gd[:, 0:256] = _vecP(inputs["gdn_conv_w"][0])
    gd[:, 256] = np.asarray(inputs["gdn_norm_w"][0], np.float32)
    gd[:, 257:289] = np.asarray(inputs["gdn_a_log"][0], np.float32)[None, :]
    gd[:, 289:321] = np.asarray(inputs["gdn_dt_bias"][0], np.float32)[None, :]
    m["gdnv"] = gd
    cwv = np.asarray(inputs["rglru_conv_w"], np.float32)
    parts = [cwv[:, 0], cwv[:, 1], cwv[:, 2], cwv[:, 3], np.asarray(inputs["rglru_conv_b"], np.float32),
             np.asarray(inputs["rglru_b_rgate"], np.float32).reshape(2, RW), np.asarray(inputs["rglru_b_igate"], np.float32).reshape(2, RW),
             np.asarray(inputs["rglru_lambda"], np.float32)]
    rv = np.stack(parts, axis=1)
    m["rvec"] = _vecP(rv, 80)
    return m


def kernel(**inputs):
    layers = tuple(inputs.pop("_layers", range(DEPTH)))
    n_pass = int(inputs.pop("_n_pass", SEQ // TP))
    ncores = int(inputs.pop("_cores", 8))
    key = (layers, n_pass)
    if key not in _CACHE:
        mk = MK(layers, n_pass)
        _CACHE[key] = (mk.build(), set(mk.din.keys()))
    nc, names = _CACHE[key]
    full = [make_inputs(inputs, b) for b in range(min(4, ncores))]
    in_maps = [{k: v for k, v in full[i % len(full)].items() if k in names} for i in range(ncores)]
    res = run_bass_kernel_spmd(nc, in_maps, core_ids=list(range(ncores)))
    nb = min(4, ncores)
    out = np.stack([np.ascontiguousarray(res.results[b]["yT"].T) for b in range(nb)], axis=0)
    return out.astype(np.float32)
```

```python
import numpy as np
import concourse.bass as bass
import concourse.mybir as mybir
from concourse.bass_utils import run_bass_kernel_spmd

F32 = mybir.dt.float32
BF16 = mybir.dt.bfloat16
ALU = mybir.AluOpType
AF = mybir.ActivationFunctionType

ENGS = ("pe", "act", "dve", "pool", "sp")

D = 2048
KD = 16
SEQ = 2048
TP = 512
DEPTH = 4
ALPHA = float((2 * DEPTH) ** 0.25)
DFF = 8192
RW = 2560
RCH = 32
NWB = 4
KG = 8


class Prog:
    def __init__(self, nc):
        self.nc = nc
        self.q = {e: [] for e in ENGS}
        self.cnt = {e: 0 for e in ENGS}
        self.sem = {e: nc.alloc_semaphore(name=f"c_{e}") for e in ENGS if e != "sp"}
        self.last_w = {}
        self.readers = {}
        self.seen = {e: {} for e in ENGS}
        self.slots = {}

    def _deps(self, eng, reads, writes):
        deps = []
        for k in reads:
            w = self.last_w.get(k)
            if w is not None:
                deps.append(w)
            if isinstance(k, tuple) and k[0] == "ps":
                deps.extend(t for t in self.readers.get(k, ()) if t[0] != eng)
        for k in writes:
            w = self.last_w.get(k)
            if w is not None:
                deps.append(w)
            deps.extend(self.readers.get(k, ()))
        waits = {}
        for (src, val) in deps:
            if src == "pe" and eng == "pe":
                continue
            if val > waits.get(src, 0):
                waits[src] = val
        out = []
        for src, val in waits.items():
            if self.seen[eng].get(src, 0) >= val:
                continue
            self.seen[eng][src] = val
            out.append((src, val))
        return out

    def _commit(self, tok, reads, writes):
        for k in writes:
            self.last_w[k] = tok
            self.readers[k] = []
        for k in reads:
            self.readers.setdefault(k, []).append(tok)

    def op(self, eng, fn, reads=(), writes=()):
        waits = self._deps(eng, reads, writes)
        self.cnt[eng] += 1
        tok = (eng, self.cnt[eng])
        self.q[eng].append((waits, fn, eng))
        self._commit(tok, reads, writes)

    def dma(self, eng, slot, fn, reads=(), writes=()):
        if slot not in self.slots:
            self.slots[slot] = [self.nc.alloc_semaphore(name=f"d_{slot}"), 0]
        waits = self._deps(eng, reads, writes)
        s = self.slots[slot]
        s[1] += 16
        tok = (("slot", slot), s[1])
        self.q[eng].append((waits, fn, ("slot", slot)))
        self._commit(tok, reads, writes)

    def wait_all(self, eng, keys):
        waits = self._deps(eng, keys, ())
        self.q[eng].append((waits, None, None))

    def _semof(self, src):
        if isinstance(src, tuple):
            return self.slots[src[1]][0]
        return self.sem[src]

    def emit(self):
        nc = self.nc
        prog = self
        emap = {"pe": "tensor", "act": "scalar", "dve": "vector", "pool": "gpsimd", "sp": "sync"}
        with nc.Block() as block:
            for e in ENGS:
                def body(engobj, e=e):
                    for waits, fn, kind in prog.q[e]:
                        for src, val in waits:
                            engobj.wait_ge(prog._semof(src), val)
                        if fn is None:
                            continue
                        ins = fn(engobj)
                        if isinstance(kind, tuple):
                            ins.then_inc(prog.slots[kind[1]][0], 16)
                        else:
                            ins.then_inc(prog.sem[e], 1)
                getattr(block, emap[e])(body)


class MK:
    def __init__(self, layers=tuple(range(DEPTH)), n_pass=SEQ // TP):
        self.layers = tuple(layers)
        self.n_layers = len(self.layers)
        self.n_pass = n_pass
        nc = self.nc = bass.Bass("TRN2", target_bir_lowering=False)
        self.P = Prog(nc)
        self.din = {}
        self.wi = 0
        self.pi = 0
        self.sqi = 0

    def inp(self, name, shape):
        if name not in self.din:
            self.din[name] = self.nc.dram_tensor(name, list(shape), F32, kind="ExternalInput").ap()
        return self.din[name]

    def T(self, name, shape, dt=F32):
        return self.nc.alloc_sbuf_tensor(name, list(shape), dt)

    def ACT(self, out, in_, func, r, w, scale=None, bias=None):
        kw = {}
        if scale is not None:
            kw["scale"] = scale
        if bias is not None:
            kw["bias"] = bias
        self.P.op("act", lambda e: e.activation(out=out, in_=in_, func=func, **kw), reads=r, writes=w)

    def TS(self, out, in0, s1, s2, op0, op1, r, w, eng="dve"):
        if op1 is None:
            self.P.op(eng, lambda e: e.tensor_scalar(out=out, in0=in0, scalar1=s1, scalar2=None, op0=op0), reads=r, writes=w)
        else:
            self.P.op(eng, lambda e: e.tensor_scalar(out=out, in0=in0, scalar1=s1, scalar2=s2, op0=op0, op1=op1), reads=r, writes=w)

    def TT(self, out, in0, in1, op, r, w, eng="dve"):
        self.P.op(eng, lambda e: e.tensor_tensor(out=out, in0=in0, in1=in1, op=op), reads=r, writes=w)

    def STT(self, out, in0, scalar, in1, op0, op1, r, w):
        self.P.op("dve", lambda e: e.scalar_tensor_tensor(out=out, in0=in0, scalar=scalar, in1=in1, op0=op0, op1=op1), reads=r, writes=w)

    def CP(self, eng, out, in_, r, w):
        if eng == "act":
            self.P.op("act", lambda e: e.copy(out=out, in_=in_), reads=r, writes=w)
        else:
            self.P.op(eng, lambda e: e.tensor_copy(out=out, in_=in_), reads=r, writes=w)

    def MM(self, out, lhsT, rhs, start, stop, r, w):
        self.P.op("pe", lambda e: e.matmul(out, lhsT=lhsT, rhs=rhs, start=start, stop=stop), reads=r, writes=w)

    def DMA(self, eng, slot, out, in_, r, w):
        if slot in ("c0", "c1"):
            self.cslot = getattr(self, "cslot", 0) + 1
            slot = f"{slot}_{self.cslot}"
        self.P.dma(eng, slot, lambda e: e.dma_start(out=out, in_=in_), reads=r, writes=w)

    def psum(self):
        i = self.pi % 8
        self.pi += 1
        return self.ps[i], ("ps", i)

    def sq(self):
        i = self.sqi % 2
        self.sqi += 1
        return self.sqb[i], ("sq", i)

    def wtile(self, src, kp, kc, ncols):
        i = self.wi % NWB
        self.wi += 1
        t = self.wb[i]
        key = ("wb", i)
        self.DMA("pool", f"wb{i}", t[:kp, :kc, :ncols], src, [], [key])
        return t, key

    def wfull(self, Wc, ncols):
        tl = []
        for hf in range(16 // KG):
            tl.append(self.wtile(Wc[hf * KG * 128:(hf + 1) * KG * 128, :].rearrange("(k p) n -> p k n", p=128), 128, KG, ncols))
        return lambda k: (tl[k // KG][0][:, k % KG, 0:ncols], tl[k // KG][1])

    def linear(self, W, kp, KC, col0, ncols, ocw, act, on_block, blk0=0, cpt=4):
        tcols = ocw * cpt
        assert ncols % tcols == 0, (ncols, tcols)
        nkg = (KC + KG - 1) // KG
        for og in range(ncols // tcols):
            banks = [self.psum() for _ in range(cpt)]
            for kg in range(nkg):
                kc = min(KG, KC - kg * KG)
                c0 = col0 + og * tcols
                src = W[kg * KG * kp:(kg * KG + kc) * kp, c0:c0 + tcols].rearrange("(k p) n -> p k n", p=kp)
                t, wkey = self.wtile(src, kp, kc, tcols)
                for j in range(cpt):
                    pt, pk = banks[j]
                    for k in range(kc):
                        a, akey = act(kg * KG + k)
                        self.MM(pt[:ocw, :], t[:kp, k, j * ocw:(j + 1) * ocw], a,
                                (kg == 0 and k == 0), (kg == nkg - 1 and k == kc - 1), [wkey, akey], [pk])
            for j in range(cpt):
                on_block(blk0 + og * cpt + j, banks[j][0], banks[j][1])

    def build(self):
        nc, P = self.nc, self.P
        L = self.n_layers
        xT = self.inp("xT", [D, SEQ])
        cT = self.inp("cT", [128, KD])
        lng = self.inp("lng", [128, DEPTH * 2 * KD])
        lnb = self.inp("lnb", [128, DEPTH * 2 * KD])
        bmod = self.inp("bmod", [128, DEPTH * 96])
        w_mod = self.inp("w_mod", [DEPTH, D, 6 * D])
        w_ff1 = self.inp("w_ff1", [DEPTH, D, DFF])
        w_ff2 = self.inp("w_ff2", [DEPTH, DFF, D])
        if any(l % 3 == 0 for l in self.layers):
            self.r_w_in = self.inp("rglru_w_in", [2, D, 2 * RW])
            self.r_w_out = self.inp("rglru_w_out", [2, RW, D])
            self.r_wrg = self.inp("rglru_w_rgate", [2, 16, 160, 160])
            self.r_wig = self.inp("rglru_w_igate", [2, 16, 160, 160])
        rvec = self.inp("rvec", [80, 2 * 8 * RCH])
        self.yT = nc.dram_tensor("yT", [D, SEQ], F32, kind="ExternalOutput").ap()

        self.xres = self.T("xres", [128, KD, TP])
        self.h = self.T("h", [128, KD, TP], BF16)
        self.U = self.T("U", [128, 64, TP], BF16)
        self.wb = [self.T(f"wb{i}", [128, KG, 512], BF16) for i in range(NWB)]
        self.ps = [nc.alloc_psum_tensor(f"ps{i}", [128, 512], F32) for i in range(8)]
        self.sqb = [self.T(f"sq{i}", [128, TP]) for i in range(2)]
        self.ones = self.T("ones", [128, 128])
        self.mean = self.T("mean", [128, TP])
        self.rstd = self.T("rstd", [128, TP])
        self.tmpv = self.T("tmpv", [128, TP])
        self.cact = self.T("cact", [128, KD], BF16)
        self.cf = self.T("cf", [128, KD])
        self.mod = self.T("mod", [128, DEPTH, 96])
        self.bm = self.T("bm", [128, DEPTH * 96])
        self.g_sb = self.T("g_sb", [128, DEPTH * 2 * KD])
        self.b_sb = self.T("b_sb", [128, DEPTH * 2 * KD])
        self.SC = self.T("SC", [128, 7200])
        self.rv = self.T("rv", [80, 2, 8, RCH])
        self.cneg = self.T("cneg", [80, 2, 2, RCH])
        self.hstate = self.T("hstate", [80, 2, RCH])
        self.tails = self.T("tails", [80, 2, RCH, 3])
        self.gw = [self.T(f"gw{i}", [80, 2, 2, 160], BF16) for i in range(2)]
        self.gwi = 0
        self.xrb = self.T("xrb", [80, 2, TP], BF16)

        P.op("dve", lambda e: e.memset(self.ones[:], 1.0), writes=["ones"])
        P.op("dve", lambda e: e.memset(self.hstate[:], 0.0), writes=["hstate"])
        P.op("dve", lambda e: e.memset(self.tails[:], 0.0), writes=["tails"])
        self.DMA("sp", "c0", self.cf[:], cT, [], ["cf"])
        self.DMA("sp", "c0", self.bm[:], bmod, [], ["bm"])
        self.DMA("sp", "c0", self.g_sb[:], lng, [], ["g_sb"])
        self.DMA("sp", "c0", self.b_sb[:], lnb, [], ["b_sb"])
        self.DMA("sp", "c0", self.rv[:].rearrange("p a b c -> p (a b c)"), rvec, [], ["rv"])
        self.ACT(self.cact[:], self.cf[:], AF.Silu, ["cf"], ["cact"])
        for s in range(2):
            self.ACT(self.cneg[:, s, 0, :], self.rv[:, s, 7, :], AF.Exp, ["rv"], ["cneg"], scale=-1.0)
            self.ACT(self.cneg[:, s, 0, :], self.cneg[:, s, 0, :], AF.Ln, ["cneg"], ["cneg"], bias=1.0)
            self.TS(self.cneg[:, s, 1, :], self.cneg[:, s, 0, :], -16.0, None, ALU.mult, None, ["cneg"], ["cneg"])
            self.TS(self.cneg[:, s, 0, :], self.cneg[:, s, 0, :], -8.0, None, ALU.mult, None, ["cneg"], ["cneg"])

        for l in self.layers:
            pm, pmk = self.psum()

            def act_c(k):
                return self.cact[:, k:k + 1], "cact"

            for og in range(24):
                getw = self.wfull(w_mod[l][:, og * 512:(og + 1) * 512], 512)
                for j in range(4):
                    col = og * 4 + j
                    for k in range(16):
                        t, wkey = getw(k)
                        self.MM(pm[:, col:col + 1], t[:, j * 128:(j + 1) * 128], self.cact[:, k:k + 1], k == 0, k == 15, [wkey, "cact"], [pmk])
            self.TT(self.mod[:, l, :], pm[:, 0:96], self.bm[:, l * 96:(l + 1) * 96], ALU.add, [pmk, "bm"], ["mod"])
            for a in (16, 32, 64, 80):
                self.TS(self.mod[:, l, a:a + 16], self.mod[:, l, a:a + 16], 1.0, None, ALU.add, None, ["mod"], ["mod"])

        for p in range(self.n_pass):
            t0 = p * TP
            self.DMA("sp", "xin", self.xres[:], xT[:, t0:t0 + TP].rearrange("(k p) t -> p k t", p=128),
                     [], [("x", k) for k in range(KD)])
            for l in self.layers:
                kind, slot = l % 3, l // 3
                self.sublayer(l, 0, kind, slot, p)
                self.sublayer(l, 1, 3, l, p)
            self.DMA("sp", "xout", self.yT[:, t0:t0 + TP].rearrange("(k p) t -> p k t", p=128), self.xres[:],
                     [("x", k) for k in range(KD)], ["yT"])
        P.wait_all("sp", ["yT"])
        P.emit()
        return nc

    def sublayer(self, l, which, kind, slot, p):
        mb = 48 * which
        sh = lambda k: self.mod[:, l, mb + k:mb + k + 1]
        sc1 = lambda k: self.mod[:, l, mb + 16 + k:mb + 17 + k]
        gt1 = lambda k: self.mod[:, l, mb + 32 + k:mb + 33 + k]
        for k in range(KD):
            self.ACT(self.h[:, k, :], self.xres[:, k, :], AF.Identity, [("x", k), "mod"], [("h", k)], scale=sc1(k), bias=sh(k))
        for k in range(KD):
            self.TS(self.xres[:, k, :], self.xres[:, k, :], ALPHA, None, ALU.mult, None, [("x", k), "stg"], [("x", k)])

        def resid(n, pt, pk):
            self.STT(self.xres[:, n, :], pt[:, :], gt1(n), self.xres[:, n, :], ALU.mult, ALU.add, [pk, ("x", n), "mod"], [("x", n)])

        import os as _os2
        if kind == 3:
            if not _os2.environ.get("MK_SKIP_FFN"):
                self.ffn(l, resid)
        elif kind == 0:
            self.rglru(slot, resid, p)
        elif kind == 2:
            self.gla(resid, p)
        else:
            self.gdn(resid, p)
        if not _os2.environ.get("MK_SKIP_LN"):
            self.layernorm(l, which)

    def hact(self, k):
        return self.h[:, k, :], ("h", k)

    def ffn(self, l, resid):
        w1 = self.din["w_ff1"][l]
        w2 = self.din["w_ff2"][l]

        def relu2(n, pt, pk):
            s, sk = self.sq()
            self.ACT(s[:], pt[:, :], AF.Square, [pk], [sk])
            self.STT(self.U[:, n, :], pt[:, :], 0.0, s[:], ALU.is_gt, ALU.mult, [pk, sk], [("U", n)])

        self.linear(w1, 128, KD, 0, DFF, 128, self.hact, relu2)
        self.linear(w2, 128, 64, 0, D, 128, lambda k: (self.U[:, k, :], ("U", k)), resid)

    def layernorm(self, l, which):
        s1, s1k = self.psum()
        s2, s2k = self.psum()
        for k in range(KD):
            s, sk = self.sq()
            self.ACT(s[:], self.xres[:, k, :], AF.Square, [("x", k)], [sk])
            self.MM(s1[:, :], self.ones[:], self.xres[:, k, :], k == 0, k == KD - 1, ["ones", ("x", k)], [s1k])
            self.MM(s2[:, :], self.ones[:], s[:], k == 0, k == KD - 1, ["ones", sk], [s2k])
        self.TS(self.mean[:], s1[:, :], 1.0 / D, None, ALU.mult, None, [s1k], ["mean"])
        self.TT(self.tmpv[:], self.mean[:], self.mean[:], ALU.mult, ["mean"], ["tmpv"])
        self.STT(self.tmpv[:], s2[:, :], 1.0 / D, self.tmpv[:], ALU.mult, ALU.subtract, [s2k, "tmpv"], ["tmpv"])
        self.TS(self.tmpv[:], self.tmpv[:], 1e-5, None, ALU.add, None, ["tmpv"], ["tmpv"])
        self.ACT(self.tmpv[:], self.tmpv[:], AF.Sqrt, ["tmpv"], ["tmpv"])
        self.P.op("dve", lambda e: e.reciprocal(out=self.rstd[:], in_=self.tmpv[:]), reads=["tmpv"], writes=["rstd"])
        gi = (l * 2 + which) * KD
        for k in range(KD):
            self.TT(self.xres[:, k, :], self.xres[:, k, :], self.mean[:], ALU.subtract, [("x", k), "mean"], [("x", k)])
            self.TT(self.xres[:, k, :], self.xres[:, k, :], self.rstd[:], ALU.mult, [("x", k), "rstd"], [("x", k)])
            self.ACT(self.xres[:, k, :], self.xres[:, k, :], AF.Identity, [("x", k), "g_sb", "b_sb"], [("x", k)],
                     scale=self.g_sb[:, gi + k:gi + k + 1], bias=self.b_sb[:, gi + k:gi + k + 1])

    def rglru(self, s, resid, p):
        w_in = self.r_w_in[s]
        w_out = self.r_w_out[s]
        U = self.U
        SC = self.SC
        def sc(i, w=2 * TP):
            return SC[:80, i * 1024:i * 1024 + w]
        recb = SC[:80, 6144:6144 + 2 * 515].rearrange("p (i t) -> p i t", i=2)
        xs = sc(0).rearrange("p (i t) -> p i t", i=2)
        xr = sc(1).rearrange("p (i t) -> p i t", i=2)
        rg = sc(2).rearrange("p (i t) -> p i t", i=2)
        ig = sc(3).rearrange("p (i t) -> p i t", i=2)
        aa = sc(4).rearrange("p (i t) -> p i t", i=2)
        bb = sc(5).rearrange("p (i t) -> p i t", i=2)
        xrb = self.xrb
        cw = lambda j, c: self.rv[:, s, j, c:c + 1]

        def gate_blk(c, pt, pk):
            x = xs[:, 0, :]
            self.ACT(x, pt[:80, :], AF.Copy, [pk], ["xs"])
            self.ACT(xr[:, 0, :], pt[:80, :], AF.Square, [pk], ["xr"])
            self.TS(xr[:, 0, :], xr[:, 0, :], 0.044715, 1.0, ALU.mult, ALU.add, ["xr"], ["xr"])
            self.TT(xr[:, 0, :], xr[:, 0, :], x, ALU.mult, ["xr", "xs"], ["xr"])
            self.ACT(xr[:, 0, :], xr[:, 0, :], AF.Sigmoid, ["xr"], ["xr"], scale=1.5957691216057308)
            self.TT(U[:80, c, :], xr[:, 0, :], x, ALU.mult, ["xr", "xs"], [("U", c)])

        self.linear(w_in, 128, KD, 0, RW, 80, self.hact, gate_blk)

        def rec_blk(c, pt, pk):
            i = c % 2
            n = c // 2
            self.ACT(recb[:, i, 3:515], pt[:80, :], AF.Copy, [pk], ["recb"])
            self.CP("dve", recb[:, i, 0:3], self.tails[:, s, c, :], ["tails"], ["recb"])
            self.CP("dve", self.tails[:, s, c, :], recb[:, i, 512:515], ["recb"], ["tails"])
            self.TS(xr[:, i, :], recb[:, i, 0:512], cw(0, c), cw(4, c), ALU.mult, ALU.add, ["recb", "rv"], ["xr"])
            for j in (1, 2, 3):
                self.STT(xr[:, i, :], recb[:, i, j:j + 512], cw(j, c), xr[:, i, :], ALU.mult, ALU.add, ["recb", "rv", "xr"], ["xr"])
            if i == 0:
                return
            self.CP("dve", xrb[:, :, :], xr[:, :, :], ["xr"], ["xrb"])
            gwt = self.gw[self.gwi % 2]
            gk = ("gw", self.gwi % 2)
            self.gwi += 1
            self.DMA("pool", f"gw{gk[1]}a", gwt[:, 0, :, :], self.r_wrg[s, n].rearrange("(i p) e -> p i e", p=80), [], [gk])
            self.DMA("pool", f"gw{gk[1]}b", gwt[:, 1, :, :], self.r_wig[s, n].rearrange("(i p) e -> p i e", p=80), [], [gk])
            for g, dst, bj in ((0, rg, 5), (1, ig, 6)):
                for j in range(2):
                    gp, gpk = self.psum()
                    for ii in range(2):
                        self.MM(gp[:80, :], gwt[:, g, ii, j * 80:(j + 1) * 80], xrb[:, ii, :], ii == 0, ii == 1, [gk, "xrb"], [gpk])
                    cc = 2 * n + j
                    self.ACT(dst[:, j, :], gp[:80, :], AF.Sigmoid, [gpk, "rv"], ["rg" if g == 0 else "ig"], bias=self.rv[:, s, bj, cc:cc + 1])
            for j in range(2):
                cc = 2 * n + j
                self.ACT(aa[:, j, :], rg[:, j, :], AF.Exp, ["rg", "cneg"], ["aa"], scale=self.cneg[:, s, 0, cc:cc + 1])
                self.ACT(bb[:, j, :], rg[:, j, :], AF.Exp, ["rg", "cneg"], ["bb"], scale=self.cneg[:, s, 1, cc:cc + 1])
            self.TS(bb[:, :, :], bb[:, :, :], -1.0, 1.0, ALU.mult, ALU.add, ["bb"], ["bb"])
            self.TS(bb[:, :, :], bb[:, :, :], 0.0, None, ALU.max, None, ["bb"], ["bb"])
            self.ACT(bb[:, :, :], bb[:, :, :], AF.Sqrt, ["bb"], ["bb"])
            self.TT(bb[:, :, :], bb[:, :, :], ig[:, :, :], ALU.mult, ["bb", "ig"], ["bb"])
            self.TT(bb[:, :, :], bb[:, :, :], xr[:, :, :], ALU.mult, ["bb", "xr"], ["bb"])
            for j in range(2):
                cc = 2 * n + j
                self.P.op("dve", lambda e, j=j, cc=cc: e.tensor_tensor_scan(out=rg[:, j, :], data0=aa[:, j, :], data1=bb[:, j, :],
                                                                            initial=self.hstate[:, s, cc:cc + 1], op0=ALU.mult, op1=ALU.add),
                          reads=["aa", "bb", "hstate"], writes=["rg"])
                self.CP("dve", self.hstate[:, s, cc:cc + 1], rg[:, j, TP - 1:TP], ["rg"], ["hstate"])
            self.TT(U[:80, 2 * n:2 * n + 2, :], U[:80, 2 * n:2 * n + 2, :], rg[:, :, :], ALU.mult,
                    [("U", 2 * n), ("U", 2 * n + 1), "rg"], [("U", 2 * n), ("U", 2 * n + 1)])

        self.linear(w_in, 128, KD, RW, RW, 80, self.hact, rec_blk)
        self.linear(w_out, 80, RCH, 0, D, 128, lambda k: (U[:80, k, :], ("U", k)), resid)


    def mix_setup(self):
        if hasattr(self, "UF"):
            return
        nc = self.nc
        self.UB = self.U[:].rearrange("p a b -> p (a b)")
        self.UF = self.UB.bitcast(F32)
        self.SCB = self.SC[:].bitcast(BF16)
        self.ST = self.SC[:, 0:4096]
        cst = self.inp("cst", [128, 1024])
        self.cst = self.T("cst_sb", [128, 1024])
        self.identb = self.T("identb", [128, 128], BF16)
        self.DMA("sp", "c1", self.cst[:], cst, [], ["cst"])
        self.CP("dve", self.identb[:], self.cst[:, 0:128], ["cst"], ["identb"])
        self.cstb = self.T("cstb", [128, 1024], BF16)
        self.CP("dve", self.cstb[:], self.cst[:], ["cst"], ["cst"])
        self.identf = self.cst[:, 0:128]
        self.tri_ge = self.cst[:, 128:192]
        self.tri_gt = self.cst[:, 192:256]
        self.ntri_gt = self.cst[:, 896:960]
        self.cmask = self.cst[:, 256:768]
        self.ones64 = self.cst[:, 768:896]

    def ufs(self, off, n):
        return self.UF[:, off:off + n]

    def ubs(self, off, n):
        return self.UB[:, 2 * off:2 * off + n]

    def gla(self, resid, p):
        self.mix_setup()
        nc = self.nc
        Win = self.inp("gla_w_in", [1, D, 6160])[0]
        Wout = self.inp("gla_w_out", [1, D, D])[0]
        if not hasattr(self, "wup"):
            wupd = self.inp("gla_w_alpha_up", [1, 16, 1024])[0]
            glav = self.inp("glav", [128, 12])
            self.wup = self.T("wup_sb", [16, 1024], BF16)
            self.glv = self.T("glv_sb", [128, 12])
            self.st_gla = nc.dram_tensor("st_gla", [128, 4096], F32).ap()
            self.DMA("pool", "wupd", self.wup[:], wupd, [], ["wup"])
            self.DMA("sp", "c1", self.glv[:], glav, [], ["glv"])
            self.TS(self.glv[:, 0:8], self.glv[:, 0:8], -1.0, None, ALU.mult, None, ["glv"], ["glv"])
        nba = lambda z: self.glv[:, z:z + 1]
        nw = lambda e: self.glv[:, 8 + e:9 + e]
        ST = self.ST
        if p == 0:
            self.P.op("dve", lambda e: e.memset(ST, 0.0), writes=["ST"])
        else:
            self.DMA("sp", "stl_gla", ST, self.st_gla, ["st_gla", ("x", 0)], ["ST"])
        v3 = lambda ap_: ap_.rearrange("p (d t) -> p d t", d=2)
        bc = v3(self.ufs(4096, 1024)); Ep = v3(self.ufs(5120, 1024)); Em = v3(self.ufs(6144, 1024))
        qd = v3(self.ubs(7168, 1024)); ki = v3(self.ubs(7680, 1024)); kt = v3(self.ubs(8192, 1024))
        ktok = self.ubs(8704, 2048)[:64].rearrange("p (c d e) -> p c d e", c=8, d=2)
        vtok = self.ubs(9728, 4096)[:64].rearrange("p (c e) -> p c e", c=8)
        sg = self.ufs(11776, 2048).rearrange("p (a t) -> p a t", a=4)
        t1 = self.ufs(14336, 512); rinv = self.ufs(14848, 512)
        Sbf = v3(self.ubs(15360, 1024))
        AT = self.ubs(15872, 64)[:64]
        alow = self.ubs(15904, 512)[:16]
        Eend = self.ufs(16160, 16).rearrange("p (d c) -> p d c", d=2)

        def alow_blk(n, pt, pk):
            self.CP("act", alow, pt[:16, :], [pk], ["alow"])
        self.linear(Win, 128, KD, 6144, 16, 16, self.hact, alow_blk, cpt=1)
        for hd in range(4):
            Sv = v3(ST[:, hd * 1024:(hd + 1) * 1024])
            for dc in range(2):
                zc = hd * 2 + dc
                zp, zk = self.psum()
                self.MM(zp[:, :], self.wup[:16, zc * 128:(zc + 1) * 128], alow, True, True, ["wup", "alow"], [zk])
                self.ACT(Ep[:, dc, :], zp[:, :], AF.Exp, [zk, "glv"], ["Ep"], scale=-1.0, bias=nba(zc))
                self.ACT(Ep[:, dc, :], Ep[:, dc, :], AF.Ln, ["Ep"], ["Ep"], bias=1.0)
                self.TS(Ep[:, dc, :], Ep[:, dc, :], -1.0 / 16.0, None, ALU.mult, None, ["Ep"], ["Ep"])
                self.P.op("dve", lambda e, dc=dc: e.tensor_tensor_scan(out=bc[:, dc, :], data0=self.cmask, data1=Ep[:, dc, :], initial=0.0,
                                                                       op0=ALU.mult, op1=ALU.add), reads=["Ep", "cst"], writes=["bc"])
            self.ACT(Eend[:, :, :], bc.rearrange("p d (c t) -> p d c t", t=64)[:, :, :, 63], AF.Exp, ["bc"], ["Eend"])
            self.ACT(Ep[:, :, :], bc[:, :, :], AF.Exp, ["bc", "Ep"], ["Ep"])
            self.ACT(Em[:, :, :], bc[:, :, :], AF.Exp, ["bc"], ["Em"], scale=-1.0)

            def q_blk(n, pt, pk):
                self.STT(qd[:, n, :], pt[:, :], 1.0 / 16.0, Ep[:, n, :], ALU.mult, ALU.mult, [pk, "Ep"], ["qd"])
            self.linear(Win, 128, KD, hd * 256, 256, 128, self.hact, q_blk, cpt=2)

            def k_blk(n, pt, pk):
                self.TT(ki[:, n, :], pt[:, :], Em[:, n, :], ALU.mult, [pk, "Em"], ["ki"])
                self.TT(kt[:, n, :].rearrange("p (c t) -> p c t", t=64), ki[:, n, :].rearrange("p (c t) -> p c t", t=64),
                        Eend[:, n, :, None].broadcast_to([128, 8, 64]), ALU.mult, ["ki", "Eend"], ["kt"])
            self.linear(Win, 128, KD, 1024 + hd * 256, 256, 128, self.hact, k_blk, cpt=2)
            for c in range(8):
                for dc in range(2):
                    tp, tk = self.psum()
                    self.MM(tp[:64, 0:128], kt[:, dc, c * 64:(c + 1) * 64], self.identb[:, :], True, True, ["kt", "identb"], [tk])
                    self.CP("act", ktok[:, c, dc, :], tp[:64, 0:128], [tk], ["ktok"])
            getw = self.wfull(Win[:, 2048 + hd * 512:2048 + (hd + 1) * 512], 512)
            for c in range(8):
                vp, vk = self.psum()
                for k in range(KD):
                    wt, wk = getw(k)
                    self.MM(vp[:64, :], self.h[:, k, c * 64:(c + 1) * 64], wt, k == 0, k == KD - 1, [wk, ("h", k)], [vk])
                self.CP("dve", vtok[:, c, :], vp[:64, :], [vk], ["vtok"])

            def g_blk(n, pt, pk):
                self.ACT(sg[:, n, :], pt[:, :], AF.Silu, [pk], ["sg"])
            self.linear(Win, 128, KD, 4096 + hd * 512, 512, 128, self.hact, g_blk, cpt=4)
            self.CP("dve", Sbf[:, :, :], Sv[:, :, :], ["ST"], ["Sbf"])
            ob = [(self.ps[i], ("ps", i)) for i in range(4)]
            for c in range(8):
                cs = slice(c * 64, (c + 1) * 64)
                a_p, a_k = self.ps[6], ("ps", 6)
                for dc in range(2):
                    self.MM(a_p[:64, 0:64], ki[:, dc, cs], qd[:, dc, cs], dc == 0, dc == 1, ["ki", "qd"], [a_k])
                self.TT(AT, a_p[:64, 0:64], self.tri_ge[:64, :], ALU.mult, [a_k, "cst"], ["AT"])
                for ec in range(4):
                    op_, ok_ = ob[ec]
                    for dc in range(2):
                        self.MM(op_[:, cs], Sbf[:, dc, ec * 128:(ec + 1) * 128], qd[:, dc, cs], dc == 0, False, ["Sbf", "qd"], [ok_])
                    self.MM(op_[:, cs], vtok[:, c, ec * 128:(ec + 1) * 128], AT, False, True, ["vtok", "AT"], [ok_])
                for dc in range(2):
                    s_p, s_k = self.ps[4 + dc], ("ps", 4 + dc)
                    self.MM(s_p[:, :], ktok[:, c, dc, :], vtok[:, c, :], True, True, ["ktok", "vtok"], [s_k])
                    self.STT(Sv[:, dc, :], Sv[:, dc, :], Eend[:, dc, c:c + 1], s_p[:, :], ALU.mult, ALU.add, ["ST", "Eend", s_k], ["ST"])
                self.CP("act", Sbf[:, :, :], Sv[:, :, :], ["ST"], ["Sbf"])
            r_p, r_k = self.ps[7], ("ps", 7)
            for ec in range(4):
                s_, sk_ = self.sq()
                self.ACT(s_[:], ob[ec][0][:, :], AF.Square, [ob[ec][1]], [sk_])
                self.MM(r_p[:, :], self.ones[:], s_[:], ec == 0, ec == 3, ["ones", sk_], [r_k])
            self.TS(rinv, r_p[:, :], 1.0 / 512.0, 1e-6, ALU.mult, ALU.add, [r_k], ["rinv"])
            self.ACT(rinv, rinv, AF.Sqrt, ["rinv"], ["rinv"])
            self.P.op("dve", lambda e: e.reciprocal(out=rinv, in_=rinv), reads=["rinv"], writes=["rinv"])
            for ec in range(4):
                self.TT(t1, ob[ec][0][:, :], rinv, ALU.mult, [ob[ec][1], "rinv"], ["t1"])
                self.STT(self.U[:, hd * 4 + ec, :], sg[:, ec, :], nw(ec), t1, ALU.mult, ALU.mult, ["sg", "glv", "t1"], [("U", hd * 4 + ec)])
        self.DMA("sp", "sts_gla", self.st_gla, ST, ["ST"], ["st_gla", "stg"])
        self.linear(Wout, 128, KD, 0, D, 128, lambda k: (self.U[:, k, :], ("U", k)), resid)

    def gdn(self, resid, p):
        self.mix_setup()
        nc = self.nc
        Win = self.inp("gdn_w_in", [1, D, 12352])[0]
        Wout = self.inp("gdn_w_out", [1, 4096, D])[0]
        if not hasattr(self, "gdv"):
            gdnv = self.inp("gdnv", [128, 4 * 64 + 1 + 64])
            self.gdv = self.T("gdv_sb", [128, 4 * 64 + 1 + 64])
            self.gtail = self.T("gtail", [128, 64, 3])
            self.st_gdn = nc.dram_tensor("st_gdn", [128, 4096], F32).ap()
            self.DMA("sp", "c1", self.gdv[:], gdnv, [], ["gdv"])
            self.P.op("dve", lambda e: e.memset(self.gtail[:], 0.0), writes=["gtail"])
            self.ACT(self.gdv[:64, 257:289], self.gdv[:64, 257:289], AF.Exp, ["gdv"], ["gdv"])
            self.TS(self.gdv[:64, 257:289], self.gdv[:64, 257:289], -1.0, None, ALU.mult, None, ["gdv"], ["gdv"])
        cw = lambda j, ch: self.gdv[:, j * 64 + ch:j * 64 + ch + 1]
        normw = self.gdv[:, 256:257]
        negA = self.gdv[:64, 257:289]
        dtb = self.gdv[:64, 289:321]
        ST = self.ST
        if p == 0:
            self.P.op("dve", lambda e: e.memset(ST, 0.0), writes=[("ST", h_) for h_ in range(32)])
        else:
            self.DMA("sp", "stl_gdn", ST, self.st_gdn, ["st_gdn", ("x", 0)], [("ST", h_) for h_ in range(32)])
        o = [8192]

        def F(n, parts=128):
            a = self.ufs(o[0], n)[:parts]
            o[0] += (n + 7) // 8 * 8
            return a

        def B(n, parts=128):
            a = self.ubs(o[0], n)[:parts]
            o[0] += ((n + 1) // 2 + 7) // 8 * 8
            return a
        r8 = lambda ap_: ap_.rearrange("p (c x) -> p c x", c=8)
        ba = r8(F(512, 64))
        beta = r8(F(256, 64))
        gg = r8(self.SC[:64, 6656:6912])
        gc = r8(F(256, 64))
        bg = r8(F(256, 64))
        tailf = r8(F(256, 64))
        dend = r8(self.SC[:, 6912:7168])
        cbuf = F(515)
        qn = B(512); kn = B(512); vT = B(512)
        acc = F(512); rn = F(512); t1 = F(512)
        Gs = r8(F(512, 64))
        QK = r8(F(512, 64))
        ktk = r8(self.SC[:64, 4096:5120])
        sets = []
        for si in range(2):
            S_ = {}
            S_["vbt"] = r8(self.SC[:64, 5120 + 512 * si:5632 + 512 * si].bitcast(BF16))
            S_["szT"] = B(512)
            for nm in ("dg", "tmpa", "Dm", "DT", "dgt"):
                S_[nm] = F(64, 64)
            for nm in ("dgh", "dgl", "Na", "Nb", "Ma", "Mb", "Uu", "Aq"):
                S_[nm] = B(64, 64)
            S_["kb"] = B(128, 64); S_["usb"] = F(128, 64); S_["wT"] = B(64); S_["vn"] = B(128, 64)
            S_["qdc"] = B(64); S_["ktt"] = B(128, 64); S_["Sbf"] = B(128); S_["eq"] = F(64)
            sets.append(S_)
        assert o[0] <= 16384, o[0]

        getw = self.wfull(Win[:, 12288:12352], 64)
        for c in range(8):
            bp, bk = self.psum()
            for k in range(KD):
                wt, wk = getw(k)
                self.MM(bp[:64, 0:64], self.h[:, k, c * 64:(c + 1) * 64], wt, k == 0, k == KD - 1, [wk, ("h", k)], [bk])
            self.CP("act", ba[:, c, :], bp[:64, 0:64], [bk], ["ba"])
        self.ACT(beta[:, :, :], ba[:, :, 0:32], AF.Sigmoid, ["ba"], ["beta"])
        self.TT(gg[:, :, :], ba[:, :, 32:64], dtb[:, None, :].broadcast_to([64, 8, 32]), ALU.add, ["ba", "gdv"], ["gg"])
        self.ACT(gg[:, :, :], gg[:, :, :], AF.Exp, ["gg"], ["gg"])
        self.ACT(gg[:, :, :], gg[:, :, :], AF.Ln, ["gg"], ["gg"], bias=1.0)
        self.TT(gg[:, :, :], gg[:, :, :], negA[:, None, :].broadcast_to([64, 8, 32]), ALU.mult, ["gg", "gdv"], ["gg"])
        ghi = self.SC[:64, 6144:6272].bitcast(BF16).rearrange("p (c x) -> p c x", c=8)
        glo = self.SC[:64, 6272:6400].bitcast(BF16).rearrange("p (c x) -> p c x", c=8)
        gtmp = self.SC[:64, 6400:6656].rearrange("p (c x) -> p c x", c=8)
        trib = self.cstb[:64, 128:192]
        onesb = self.cstb[:64, 768:896]
        self.CP("dve", ghi, gg[:, :, :], ["gg"], ["ghi"])
        self.TT(gtmp, gg[:, :, :], ghi, ALU.subtract, ["gg", "ghi"], ["gtmp"])
        self.CP("dve", glo, gtmp, ["gtmp"], ["glo"])
        for c in range(8):
            cp_, ck_ = self.psum()
            self.MM(cp_[:64, 0:32], trib, ghi[:, c, :], True, False, ["cst", "ghi"], [ck_])
            self.MM(cp_[:64, 0:32], trib, glo[:, c, :], False, True, ["cst", "glo"], [ck_])
            self.CP("act", gc[:, c, :], cp_[:64, 0:32], [ck_], ["gc"])
            ep_, ek_ = self.psum()
            self.MM(ep_[:, 0:32], onesb, ghi[:, c, :], True, False, ["cst", "ghi"], [ek_])
            self.MM(ep_[:, 0:32], onesb, glo[:, c, :], False, True, ["cst", "glo"], [ek_])
            self.ACT(dend[:, c, :], ep_[:, 0:32], AF.Exp, [ek_], ["dend"])
            self.TT(tailf[:, c, :], ep_[:64, 0:32], gc[:, c, :], ALU.subtract, [ek_, "gc"], ["tailf"])
        self.ACT(tailf[:, :, :], tailf[:, :, :], AF.Exp, ["tailf"], ["tailf"])
        self.ACT(bg[:, :, :], gc[:, :, :], AF.Exp, ["gc"], ["bg"])
        self.TT(bg[:, :, :], bg[:, :, :], beta[:, :, :], ALU.mult, ["bg", "beta"], ["bg"])

        import os as _os3
        _cs = int(_os3.environ.get("GDN_CS", "9"))

        def conv_silu(ch, pt, pk, dst, dkey, l2=None):
            self.ACT(cbuf[:, 3:515], pt[:, :], AF.Copy, [pk], ["cbuf"])
            if _cs <= 1:
                return
            self.CP("dve", cbuf[:, 0:3], self.gtail[:, ch, :], ["gtail"], ["cbuf"])
            self.CP("dve", self.gtail[:, ch, :], cbuf[:, 512:515], ["cbuf"], ["gtail"])
            self.TS(acc, cbuf[:, 0:512], cw(0, ch), None, ALU.mult, None, ["cbuf", "gdv"], ["acc"])
            for j in (1, 2, 3):
                self.STT(acc, cbuf[:, j:j + 512], cw(j, ch), acc, ALU.mult, ALU.add, ["cbuf", "gdv", "acc"], ["acc"])
            if _cs <= 2:
                return
            if l2 is None:
                self.ACT(dst, acc, AF.Silu, ["acc"], [dkey])
                return
            self.ACT(acc, acc, AF.Silu, ["acc"], ["acc"])
            if _cs <= 3:
                return
            s_, sk_ = self.sq()
            self.ACT(s_[:], acc, AF.Square, ["acc"], [sk_])
            lp, lk = self.psum()
            self.MM(lp[:, :], self.ones[:], s_[:], True, True, ["ones", sk_], [lk])
            if _cs <= 4:
                return
            self.TS(rn, lp[:, :], 1e-6, None, ALU.add, None, [lk], ["rn"])
            if _cs <= 5:
                return
            self.ACT(rn, rn, AF.Sqrt, ["rn"], ["rn"])
            if _cs <= 6:
                return
            self.P.op("dve", lambda e: e.reciprocal(out=rn, in_=rn), reads=["rn"], writes=["rn"])
            if _cs <= 7:
                return
            self.STT(dst, acc, l2, rn, ALU.mult, ALU.mult, ["acc", "rn"], [dkey])

        import os as _os
        _dbg = int(_os.environ.get("GDN_DBG", "0"))
        for hq in range(16 if _dbg != 3 else 0):
            self.linear(Win, 128, KD, hq * 128, 128, 128, self.hact,
                        lambda n, pt, pk: conv_silu(hq, pt, pk, qn, "qn", l2=128.0 ** -0.5), cpt=1)
            self.linear(Win, 128, KD, 2048 + hq * 128, 128, 128, self.hact,
                        lambda n, pt, pk: conv_silu(16 + hq, pt, pk, kn, "kn", l2=1.0), cpt=1)
            for c in range(8 if _dbg != 5 else 0):
                cs = slice(c * 64, (c + 1) * 64)
                g_p, g_k = self.psum()
                if _dbg not in (6, 7, 8):
                    self.MM(g_p[:64, 0:64], kn[:, cs], kn[:, cs], True, True, ["kn"], [g_k])
                if _dbg != 8:
                    self.MM(g_p[:64, 64:128], kn[:, cs], qn[:, cs], True, True, ["kn", "qn"], [g_k])
                if _dbg != 7:
                    self.MM(g_p[:64, 128:256], kn[:, cs], self.identb[:, :], True, True, ["kn", "identb"], [g_k])
                if _dbg not in (6, 7, 8):
                    self.CP("act", Gs[:, c, :], g_p[:64, 0:64], [g_k], ["Gs"])
                if _dbg != 8:
                    self.CP("act", QK[:, c, :], g_p[:64, 64:128], [g_k], ["QK"])
                if _dbg != 7:
                    self.CP("act", ktk[:, c, :], g_p[:64, 128:256], [g_k], ["ktk"])
            hs = (2 * hq, 2 * hq + 1) if _dbg not in (4, 5, 6, 7, 8) else ()
            for si, h in enumerate(hs):
                S_ = sets[si]
                self.linear(Win, 128, KD, 4096 + h * 128, 128, 128, self.hact,
                            lambda n, pt, pk: conv_silu(32 + h, pt, pk, vT, "vT"), cpt=1)
                self.linear(Win, 128, KD, 8192 + h * 128, 128, 128, self.hact,
                            lambda n, pt, pk: self.ACT(S_["szT"], pt[:, :], AF.Silu, [pk], [f"szT{si}"]), cpt=1)
                for c in range(8):
                    v_p, v_k = self.psum()
                    self.MM(v_p[:64, 0:128], vT[:, c * 64:(c + 1) * 64], self.identb[:, :], True, True, ["vT", "identb"], [v_k])
                    self.TS(S_["vbt"][:, c, :], v_p[:64, 0:128], beta[:, c, h:h + 1], None, ALU.mult, None, [v_k, "beta"], [f"vbt{si}"])

            def chain(si, h):
                S_ = sets[si]
                k_ = lambda nm: f"{nm}{si}"
                dg, tmpa, Dm, DT, dgt, dgh, dgl = S_["dg"], S_["tmpa"], S_["Dm"], S_["DT"], S_["dgt"], S_["dgh"], S_["dgl"]
                NN, MM_, Uu = [S_["Na"], S_["Nb"]], [S_["Ma"], S_["Mb"]], S_["Uu"]
                kb, usb, wT, vn, Aq, qdc, ktt, Sbf, eq = (S_[x] for x in ("kb", "usb", "wT", "vn", "Aq", "qdc", "ktt", "Sbf", "eq"))
                vbt_, szT_ = S_["vbt"], S_["szT"]
                b0 = 4 * si
                o_p, o_k = self.ps[b0], ("ps", b0)
                P1, K1 = self.ps[b0 + 1], ("ps", b0 + 1)
                P2, K2 = self.ps[b0 + 2], ("ps", b0 + 2)
                P3, K3 = self.ps[b0 + 3], ("ps", b0 + 3)
                Sv = ST[:, h * 128:(h + 1) * 128]
                sk = ("ST", h)
                self.CP("dve", Sbf, Sv, [sk], [k_("Sbf")])
                yield
                for c in range(8 if _dbg != 1 else 0):
                    cs = slice(c * 64, (c + 1) * 64)
                    gci = gc[:, c, h:h + 1]
                    self.TS(dg, self.identf[:64, 0:64], gci, None, ALU.mult, None, ["cst", "gc"], [k_("dg")]); yield
                    self.CP("dve", dgh, dg, [k_("dg")], [k_("dgh")]); yield
                    self.TT(dgt, dg, dgh, ALU.subtract, [k_("dg"), k_("dgh")], [k_("dgt")]); yield
                    self.CP("dve", dgl, dgt, [k_("dgt")], [k_("dgl")]); yield
                    self.MM(P1[:, 0:64], onesb, dgh, True, False, ["cst", k_("dgh")], [K1])
                    self.MM(P1[:, 0:64], onesb, dgl, False, True, ["cst", k_("dgl")], [K1]); yield
                    self.TS(tmpa, P1[:64, 0:64], gci, 0.0, ALU.subtract, ALU.max, [K1, "gc"], [k_("tmpa")]); yield
                    self.ACT(Dm, tmpa, AF.Exp, [k_("tmpa")], [k_("Dm")], scale=-1.0); yield
                    self.TS(DT, P1[:64, 0:64], gci, 0.0, ALU.subtract, ALU.min, [K1, "gc"], [k_("DT")]); yield
                    self.ACT(DT, DT, AF.Exp, [k_("DT")], [k_("DT")]); yield
                    self.ACT(eq, P1[:, 0:64], AF.Exp, [K1], [k_("eq")]); yield
                    self.TT(qdc, qn[:, cs], eq, ALU.mult, ["qn", k_("eq")], [k_("qdc")]); yield
                    Nn, Mm = NN[0], MM_[0]
                    self.TT(Nn, Gs[:, c, :], Dm, ALU.mult, ["Gs", k_("Dm")], [k_("N0")]); yield
                    self.STT(Nn, Nn, beta[:, c, h:h + 1], self.ntri_gt[:64, :], ALU.mult, ALU.mult, [k_("N0"), "beta", "cst"], [k_("N0")]); yield
                    self.MM(P2[:64, 0:64], Nn, self.identb[:64, 0:64], True, True, [k_("N0"), "identb"], [K2]); yield
                    self.CP("act", Mm, P2[:64, 0:64], [K2], [k_("M0")]); yield
                    self.TT(Uu, Mm, self.identf[:64, 0:64], ALU.add, [k_("M0"), "cst"], [k_("Uu")]); yield
                    cur = 0
                    for lvl in range(5):
                        nx = 1 - cur
                        Nc, Mc, Nx, Mx = NN[cur], MM_[cur], NN[nx], MM_[nx]
                        kN, kM, kNx, kMx = k_(f"N{cur}"), k_(f"M{cur}"), k_(f"N{nx}"), k_(f"M{nx}")
                        self.MM(P2[:64, 64:128], Mc, Nc, True, True, [kM, kN], [K2])
                        if lvl < 4:
                            self.MM(P2[:64, 128:192], Nc, Mc, True, True, [kM, kN], [K2])
                        yield
                        self.CP("act", Nx, P2[:64, 64:128], [K2], [kNx]); yield
                        if lvl < 4:
                            self.CP("act", Mx, P2[:64, 128:192], [K2], [kMx]); yield
                        self.MM(P3[:64, 0:64], Nx, Uu, True, True, [kNx, k_("Uu")], [K3]); yield
                        self.TT(Uu, Uu, P3[:64, 0:64], ALU.add, [k_("Uu"), K3], [k_("Uu")]); yield
                        cur = nx
                    self.TS(kb, ktk[:, c, :], bg[:, c, h:h + 1], None, ALU.mult, None, ["ktk", "bg"], [k_("kb")]); yield
                    self.MM(P2[:, 256:320], kb, Uu, True, True, [k_("kb"), k_("Uu")], [K2]); yield
                    self.CP("act", wT, P2[:, 256:320], [K2], [k_("wT")]); yield
                    self.MM(P3[:64, 128:256], Uu, vbt_[:, c, :], True, True, [k_("Uu"), k_("vbt")], [K3]); yield
                    self.CP("act", usb, P3[:64, 128:256], [K3], [k_("usb")]); yield
                    self.MM(P3[:64, 256:384], wT, Sbf, True, True, [k_("wT"), k_("Sbf")], [K3]); yield
                    self.TT(vn, usb, P3[:64, 256:384], ALU.subtract, [k_("usb"), K3], [k_("vn")]); yield
                    self.TT(tmpa, QK[:, c, :], DT, ALU.mult, ["QK", k_("DT")], [k_("tmpa")]); yield
                    self.TT(Aq, tmpa, self.tri_ge[:64, :], ALU.mult, [k_("tmpa"), "cst"], [k_("Aq")]); yield
                    self.MM(o_p[:, cs], Sbf, qdc, True, False, [k_("Sbf"), k_("qdc")], [o_k])
                    self.MM(o_p[:, cs], vn, Aq, False, True, [k_("vn"), k_("Aq")], [o_k]); yield
                    self.TS(ktt, ktk[:, c, :], tailf[:, c, h:h + 1], None, ALU.mult, None, ["ktk", "tailf"], [k_("ktt")]); yield
                    self.MM(P2[:, 384:512], ktt, vn, True, True, [k_("ktt"), k_("vn")], [K2]); yield
                    self.STT(Sv, Sv, dend[:, c, h:h + 1], P2[:, 384:512], ALU.mult, ALU.add, [sk, "dend", K2], [sk]); yield
                    self.CP("act", Sbf, Sv, [sk], [k_("Sbf")]); yield
                s_, sk_ = self.sq()
                self.ACT(s_[:], o_p[:, :], AF.Square, [o_k], [sk_])
                self.MM(P1[:, :], self.ones[:], s_[:], True, True, ["ones", sk_], [K1])
                self.TS(rn, P1[:, :], 1.0 / 128.0, 1e-6, ALU.mult, ALU.add, [K1], ["rn"])
                self.ACT(rn, rn, AF.Sqrt, ["rn"], ["rn"])
                self.P.op("dve", lambda e: e.reciprocal(out=rn, in_=rn), reads=["rn"], writes=["rn"])
                self.TT(t1, o_p[:, :], rn, ALU.mult, [o_k, "rn"], ["t1"])
                self.STT(self.U[:, h, :], szT_, normw, t1, ALU.mult, ALU.mult, [k_("szT"), "gdv", "t1"], [("U", h)])
                yield

            gens = [chain(si, h) for si, h in enumerate(hs)]
            while gens:
                for g_ in list(gens):
                    try:
                        next(g_)
                    except StopIteration:
                        gens.remove(g_)
        self.DMA("sp", "sts_gdn", self.st_gdn, ST, [("ST", h_) for h_ in range(32)], ["st_gdn", "stg"])
        self.linear(Wout, 128, 32, 0, D, 128, lambda k: (self.U[:, k, :], ("U", k)), resid)


_CACHE = {}


def _vecP(v, p=128):
    v = np.asarray(v, np.float32)
    lead = v.shape[:-1]
    c = v.shape[-1] // p
    v = v.reshape(*lead, c, p)
    v = np.moveaxis(v, -1, 0)
    return np.ascontiguousarray(v.reshape(p, -1))


def make_inputs(inputs, b):
    f = lambda a: np.ascontiguousarray(np.asarray(a, np.float32))
    m = {}
    m["xT"] = np.ascontiguousarray(np.asarray(inputs["x"][b], np.float32).T)
    m["cT"] = _vecP(inputs["c"][b])
    m["lng"] = _vecP(inputs["ln_g"])
    m["lnb"] = _vecP(inputs["ln_b"])
    m["bmod"] = _vecP(inputs["b_mod"])
    for k in ("w_mod", "w_ff1", "w_ff2", "rglru_w_in", "rglru_w_out", "rglru_w_rgate", "rglru_w_igate",
              "gla_w_in", "gla_w_out", "gla_w_alpha_up", "gdn_w_in", "gdn_w_out"):
        m[k] = f(inputs[k])
    cst = np.zeros((128, 1024), np.float32)
    cst[:, 0:128] = np.eye(128, dtype=np.float32)
    pp = np.arange(128)[:, None]
    ff = np.arange(64)[None, :]
    cst[:, 128:192] = (ff >= pp).astype(np.float32)
    cst[:, 192:256] = (ff < pp).astype(np.float32)
    cst[:, 256:768] = ((np.arange(512) % 64) != 0).astype(np.float32)[None, :]
    cst[:, 768:896] = 1.0
    cst[:, 896:960] = -(ff < pp).astype(np.float32)
    m["cst"] = cst
    m["glav"] = np.concatenate([_vecP(inputs["gla_b_alpha"][0]), _vecP(inputs["gla_norm_w"][0])], axis=1)
    gd = np.zeros((128, 321), np.float32)
    gd[:, 0:256] = _vecP(inputs["gdn_conv_w"][0])
    gd[:, 256] = np.asarray(inputs["gdn_norm_w"][0], np.float32)
    gd[:, 257:289] = np.asarray(inputs["gdn_a_log"][0], np.float32)[None, :]
    gd[:, 289:321] = np.asarray(inputs["gdn_dt_bias"][0], np.float32)[None, :]
    m["gdnv"] = gd
    cwv = np.asarray(inputs["rglru_conv_w"], np.float32)
    parts = [cwv[:, 0], cwv[:, 1], cwv[:, 2], cwv[:, 3], np.asarray(inputs["rglru_conv_b"], np.float32),
             np.asarray(inputs["rglru_b_rgate"], np.float32).reshape(2, RW), np.asarray(inputs["rglru_b_igate"], np.float32).reshape(2, RW),
             np.asarray(inputs["rglru_lambda"], np.float32)]
    rv = np.stack(parts, axis=1)
    m["rvec"] = _vecP(rv, 80)
    return m


def kernel(**inputs):
    layers = tuple(inputs.pop("_layers", range(DEPTH)))
    n_pass = int(inputs.pop("_n_pass", SEQ // TP))
    ncores = int(inputs.pop("_cores", 8))
    key = (layers, n_pass)
    if key not in _CACHE:
        mk = MK(layers, n_pass)
        _CACHE[key] = (mk.build(), set(mk.din.keys()))
    nc, names = _CACHE[key]
    full = [make_inputs(inputs, b) for b in range(min(4, ncores))]
    in_maps = [{k: v for k, v in full[i % len(full)].items() if k in names} for i in range(ncores)]
    res = run_bass_kernel_spmd(nc, in_maps, core_ids=list(range(ncores)))
    nb = min(4, ncores)
    out = np.stack([np.ascontiguousarray(res.results[b]["yT"].T) for b in range(nb)], axis=0)
    return out.astype(np.float32)
```

```python
import numpy as np
import concourse.bass as bass
import concourse.mybir as mybir
from concourse.bass_utils import run_bass_kernel_spmd

F32 = mybir.dt.float32
BF16 = mybir.dt.bfloat16
ALU = mybir.AluOpType
AF = mybir.ActivationFunctionType

ENGS = ("pe", "act", "dve", "pool", "sp")

D = 2048
KD = 16
SEQ = 2048
TP = 512
DEPTH = 4
ALPHA = float((2 * DEPTH) ** 0.25)
DFF = 8192
RW = 2560
RCH = 32
NWB = 4
KG = 8


class Prog:
    def __init__(self, nc):
        self.nc = nc
        self.q = {e: [] for e in ENGS}
        self.cnt = {e: 0 for e in ENGS}
        self.sem = {e: nc.alloc_semaphore(name=f"c_{e}") for e in ENGS if e != "sp"}
        self.last_w = {}
        self.readers = {}
        self.seen = {e: {} for e in ENGS}
        self.slots = {}

    def _deps(self, eng, reads, writes):
        deps = []
        for k in reads:
            w = self.last_w.get(k)
            if w is not None:
                deps.append(w)
            if isinstance(k, tuple) and k[0] == "ps":
                deps.extend(t for t in self.readers.get(k, ()) if t[0] != eng)
        for k in writes:
            w = self.last_w.get(k)
            if w is not None:
                deps.append(w)
            deps.extend(self.readers.get(k, ()))
        waits = {}
        for (src, val) in deps:
            if src == "pe" and eng == "pe":
                continue
            if val > waits.get(src, 0):
                waits[src] = val
        out = []
        for src, val in waits.items():
            if self.seen[eng].get(src, 0) >= val:
                continue
            self.seen[eng][src] = val
            out.append((src, val))
        return out

    def _commit(self, tok, reads, writes):
        for k in writes:
            self.last_w[k] = tok
            self.readers[k] = []
        for k in reads:
            self.readers.setdefault(k, []).append(tok)

    def op(self, eng, fn, reads=(), writes=()):
        waits = self._deps(eng, reads, writes)
        self.cnt[eng] += 1
        tok = (eng, self.cnt[eng])
        self.q[eng].append((waits, fn, eng))
        self._commit(tok, reads, writes)

    def dma(self, eng, slot, fn, reads=(), writes=()):
        if slot not in self.slots:
            self.slots[slot] = [self.nc.alloc_semaphore(name=f"d_{slot}"), 0]
        waits = self._deps(eng, reads, writes)
        s = self.slots[slot]
        s[1] += 16
        tok = (("slot", slot), s[1])
        self.q[eng].append((waits, fn, ("slot", slot)))
        self._commit(tok, reads, writes)

    def wait_all(self, eng, keys):
        waits = self._deps(eng, keys, ())
        self.q[eng].append((waits, None, None))

    def _semof(self, src):
        if isinstance(src, tuple):
            return self.slots[src[1]][0]
        return self.sem[src]

    def emit(self):
        nc = self.nc
        prog = self
        emap = {"pe": "tensor", "act": "scalar", "dve": "vector", "pool": "gpsimd", "sp": "sync"}
        with nc.Block() as block:
            for e in ENGS:
                def body(engobj, e=e):
                    for waits, fn, kind in prog.q[e]:
                        for src, val in waits:
                            engobj.wait_ge(prog._semof(src), val)
                        if fn is None:
                            continue
                        ins = fn(engobj)
                        if isinstance(kind, tuple):
                            ins.then_inc(prog.slots[kind[1]][0], 16)
                        else:
                            ins.then_inc(prog.sem[e], 1)
                getattr(block, emap[e])(body)


class MK:
    def __init__(self, layers=tuple(range(DEPTH)), n_pass=SEQ // TP):
        self.layers = tuple(layers)
        self.n_layers = len(self.layers)
        self.n_pass = n_pass
        nc = self.nc = bass.Bass("TRN2", target_bir_lowering=False)
        self.P = Prog(nc)
        self.din = {}
        self.wi = 0
        self.pi = 0
        self.sqi = 0

    def inp(self, name, shape):
        if name not in self.din:
            self.din[name] = self.nc.dram_tensor(name, list(shape), F32, kind="ExternalInput").ap()
        return self.din[name]

    def T(self, name, shape, dt=F32):
        return self.nc.alloc_sbuf_tensor(name, list(shape), dt)

    def ACT(self, out, in_, func, r, w, scale=None, bias=None):
        kw = {}
        if scale is not None:
            kw["scale"] = scale
        if bias is not None:
            kw["bias"] = bias
        self.P.op("act", lambda e: e.activation(out=out, in_=in_, func=func, **kw), reads=r, writes=w)

    def TS(self, out, in0, s1, s2, op0, op1, r, w, eng="dve"):
        if op1 is None:
            self.P.op(eng, lambda e: e.tensor_scalar(out=out, in0=in0, scalar1=s1, scalar2=None, op0=op0), reads=r, writes=w)
        else:
            self.P.op(eng, lambda e: e.tensor_scalar(out=out, in0=in0, scalar1=s1, scalar2=s2, op0=op0, op1=op1), reads=r, writes=w)

    def TT(self, out, in0, in1, op, r, w, eng="dve"):
        self.P.op(eng, lambda e: e.tensor_tensor(out=out, in0=in0, in1=in1, op=op), reads=r, writes=w)

    def STT(self, out, in0, scalar, in1, op0, op1, r, w):
        self.P.op("dve", lambda e: e.scalar_tensor_tensor(out=out, in0=in0, scalar=scalar, in1=in1, op0=op0, op1=op1), reads=r, writes=w)

    def CP(self, eng, out, in_, r, w):
        if eng == "act":
            self.P.op("act", lambda e: e.copy(out=out, in_=in_), reads=r, writes=w)
        else:
            self.P.op(eng, lambda e: e.tensor_copy(out=out, in_=in_), reads=r, writes=w)

    def MM(self, out, lhsT, rhs, start, stop, r, w):
        self.P.op("pe", lambda e: e.matmul(out, lhsT=lhsT, rhs=rhs, start=start, stop=stop), reads=r, writes=w)

    def DMA(self, eng, slot, out, in_, r, w):
        if slot in ("c0", "c1"):
            self.cslot = getattr(self, "cslot", 0) + 1
            slot = f"{slot}_{self.cslot}"
        self.P.dma(eng, slot, lambda e: e.dma_start(out=out, in_=in_), reads=r, writes=w)

    def psum(self):
        i = self.pi % 8
        self.pi += 1
        return self.ps[i], ("ps", i)

    def sq(self):
        i = self.sqi % 2
        self.sqi += 1
        return self.sqb[i], ("sq", i)

    def wtile(self, src, kp, kc, ncols):
        i = self.wi % NWB
        self.wi += 1
        t = self.wb[i]
        key = ("wb", i)
        self.DMA("pool", f"wb{i}", t[:kp, :kc, :ncols], src, [], [key])
        return t, key

    def wfull(self, Wc, ncols):
        tl = []
        for hf in range(16 // KG):
            tl.append(self.wtile(Wc[hf * KG * 128:(hf + 1) * KG * 128, :].rearrange("(k p) n -> p k n", p=128), 128, KG, ncols))
        return lambda k: (tl[k // KG][0][:, k % KG, 0:ncols], tl[k // KG][1])

    def linear(self, W, kp, KC, col0, ncols, ocw, act, on_block, blk0=0, cpt=4):
        tcols = ocw * cpt
        assert ncols % tcols == 0, (ncols, tcols)
        nkg = (KC + KG - 1) // KG
        for og in range(ncols // tcols):
            banks = [self.psum() for _ in range(cpt)]
            for kg in range(nkg):
                kc = min(KG, KC - kg * KG)
                c0 = col0 + og * tcols
                src = W[kg * KG * kp:(kg * KG + kc) * kp, c0:c0 + tcols].rearrange("(k p) n -> p k n", p=kp)
                t, wkey = self.wtile(src, kp, kc, tcols)
                for j in range(cpt):
                    pt, pk = banks[j]
                    for k in range(kc):
                        a, akey = act(kg * KG + k)
                        self.MM(pt[:ocw, :], t[:kp, k, j * ocw:(j + 1) * ocw], a,
                                (kg == 0 and k == 0), (kg == nkg - 1 and k == kc - 1), [wkey, akey], [pk])
            for j in range(cpt):
                on_block(blk0 + og * cpt + j, banks[j][0], banks[j][1])

    def build(self):
        nc, P = self.nc, self.P
        L = self.n_layers
        xT = self.inp("xT", [D, SEQ])
        cT = self.inp("cT", [128, KD])
        lng = self.inp("lng", [128, DEPTH * 2 * KD])
        lnb = self.inp("lnb", [128, DEPTH * 2 * KD])
        bmod = self.inp("bmod", [128, DEPTH * 96])
        w_mod = self.inp("w_mod", [DEPTH, D, 6 * D])
        w_ff1 = self.inp("w_ff1", [DEPTH, D, DFF])
        w_ff2 = self.inp("w_ff2", [DEPTH, DFF, D])
        if any(l % 3 == 0 for l in self.layers):
            self.r_w_in = self.inp("rglru_w_in", [2, D, 2 * RW])
            self.r_w_out = self.inp("rglru_w_out", [2, RW, D])
            self.r_wrg = self.inp("rglru_w_rgate", [2, 16, 160, 160])
            self.r_wig = self.inp("rglru_w_igate", [2, 16, 160, 160])
        rvec = self.inp("rvec", [80, 2 * 8 * RCH])
        self.yT = nc.dram_tensor("yT", [D, SEQ], F32, kind="ExternalOutput").ap()

        self.xres = self.T("xres", [128, KD, TP])
        self.h = self.T("h", [128, KD, TP], BF16)
        self.U = self.T("U", [128, 64, TP], BF16)
        self.wb = [self.T(f"wb{i}", [128, KG, 512], BF16) for i in range(NWB)]
        self.ps = [nc.alloc_psum_tensor(f"ps{i}", [128, 512], F32) for i in range(8)]
        self.sqb = [self.T(f"sq{i}", [128, TP]) for i in range(2)]
        self.ones = self.T("ones", [128, 128])
        self.mean = self.T("mean", [128, TP])
        self.rstd = self.T("rstd", [128, TP])
        self.tmpv = self.T("tmpv", [128, TP])
        self.cact = self.T("cact", [128, KD], BF16)
        self.cf = self.T("cf", [128, KD])
        self.mod = self.T("mod", [128, DEPTH, 96])
        self.bm = self.T("bm", [128, DEPTH * 96])
        self.g_sb = self.T("g_sb", [128, DEPTH * 2 * KD])
        self.b_sb = self.T("b_sb", [128, DEPTH * 2 * KD])
        self.SC = self.T("SC", [128, 7200])
        self.rv = self.T("rv", [80, 2, 8, RCH])
        self.cneg = self.T("cneg", [80, 2, 2, RCH])
        self.hstate = self.T("hstate", [80, 2, RCH])
        self.tails = self.T("tails", [80, 2, RCH, 3])
        self.gw = [self.T(f"gw{i}", [80, 2, 2, 160], BF16) for i in range(2)]
        self.gwi = 0
        self.xrb = self.T("xrb", [80, 2, TP], BF16)

        P.op("dve", lambda e: e.memset(self.ones[:], 1.0), writes=["ones"])
        P.op("dve", lambda e: e.memset(self.hstate[:], 0.0), writes=["hstate"])
        P.op("dve", lambda e: e.memset(self.tails[:], 0.0), writes=["tails"])
        self.DMA("sp", "c0", self.cf[:], cT, [], ["cf"])
        self.DMA("sp", "c0", self.bm[:], bmod, [], ["bm"])
        self.DMA("sp", "c0", self.g_sb[:], lng, [], ["g_sb"])
        self.DMA("sp", "c0", self.b_sb[:], lnb, [], ["b_sb"])
        self.DMA("sp", "c0", self.rv[:].rearrange("p a b c -> p (a b c)"), rvec, [], ["rv"])
        self.ACT(self.cact[:], self.cf[:], AF.Silu, ["cf"], ["cact"])
        for s in range(2):
            self.ACT(self.cneg[:, s, 0, :], self.rv[:, s, 7, :], AF.Exp, ["rv"], ["cneg"], scale=-1.0)
            self.ACT(self.cneg[:, s, 0, :], self.cneg[:, s, 0, :], AF.Ln, ["cneg"], ["cneg"], bias=1.0)
            self.TS(self.cneg[:, s, 1, :], self.cneg[:, s, 0, :], -16.0, None, ALU.mult, None, ["cneg"], ["cneg"])
            self.TS(self.cneg[:, s, 0, :], self.cneg[:, s, 0, :], -8.0, None, ALU.mult, None, ["cneg"], ["cneg"])

        for l in self.layers:
            pm, pmk = self.psum()

            def act_c(k):
                return self.cact[:, k:k + 1], "cact"

            for og in range(24):
                getw = self.wfull(w_mod[l][:, og * 512:(og + 1) * 512], 512)
                for j in range(4):
                    col = og * 4 + j
                    for k in range(16):
                        t, wkey = getw(k)
                        self.MM(pm[:, col:col + 1], t[:, j * 128:(j + 1) * 128], self.cact[:, k:k + 1], k == 0, k == 15, [wkey, "cact"], [pmk])
            self.TT(self.mod[:, l, :], pm[:, 0:96], self.bm[:, l * 96:(l + 1) * 96], ALU.add, [pmk, "bm"], ["mod"])
            for a in (16, 32, 64, 80):
                self.TS(self.mod[:, l, a:a + 16], self.mod[:, l, a:a + 16], 1.0, None, ALU.add, None, ["mod"], ["mod"])

        for p in range(self.n_pass):
            t0 = p * TP
            self.DMA("sp", "xin", self.xres[:], xT[:, t0:t0 + TP].rearrange("(k p) t -> p k t", p=128),
                     [], [("x", k) for k in range(KD)])
            for l in self.layers:
                kind, slot = l % 3, l // 3
                self.sublayer(l, 0, kind, slot, p)
                self.sublayer(l, 1, 3, l, p)
            self.DMA("sp", "xout", self.yT[:, t0:t0 + TP].rearrange("(k p) t -> p k t", p=128), self.xres[:],
                     [("x", k) for k in range(KD)], ["yT"])
        P.wait_all("sp", ["yT"])
        P.emit()
        return nc

    def sublayer(self, l, which, kind, slot, p):
        mb = 48 * which
        sh = lambda k: self.mod[:, l, mb + k:mb + k + 1]
        sc1 = lambda k: self.mod[:, l, mb + 16 + k:mb + 17 + k]
        gt1 = lambda k: self.mod[:, l, mb + 32 + k:mb + 33 + k]
        for k in range(KD):
            self.ACT(self.h[:, k, :], self.xres[:, k, :], AF.Identity, [("x", k), "mod"], [("h", k)], scale=sc1(k), bias=sh(k))
        for k in range(KD):
            self.TS(self.xres[:, k, :], self.xres[:, k, :], ALPHA, None, ALU.mult, None, [("x", k), "stg"], [("x", k)])

        def resid(n, pt, pk):
            self.STT(self.xres[:, n, :], pt[:, :], gt1(n), self.xres[:, n, :], ALU.mult, ALU.add, [pk, ("x", n), "mod"], [("x", n)])

        import os as _os2
        if kind == 3:
            if not _os2.environ.get("MK_SKIP_FFN"):
                self.ffn(l, resid)
        elif kind == 0:
            self.rglru(slot, resid, p)
        elif kind == 2:
            self.gla(resid, p)
        else:
            self.gdn(resid, p)
        if not _os2.environ.get("MK_SKIP_LN"):
            self.layernorm(l, which)

    def hact(self, k):
        return self.h[:, k, :], ("h", k)

    def ffn(self, l, resid):
        w1 = self.din["w_ff1"][l]
        w2 = self.din["w_ff2"][l]

        def relu2(n, pt, pk):
            s, sk = self.sq()
            self.ACT(s[:], pt[:, :], AF.Square, [pk], [sk])
            self.STT(self.U[:, n, :], pt[:, :], 0.0, s[:], ALU.is_gt, ALU.mult, [pk, sk], [("U", n)])

        self.linear(w1, 128, KD, 0, DFF, 128, self.hact, relu2)
        self.linear(w2, 128, 64, 0, D, 128, lambda k: (self.U[:, k, :], ("U", k)), resid)

    def layernorm(self, l, which):
        s1, s1k = self.psum()
        s2, s2k = self.psum()
        for k in range(KD):
            s, sk = self.sq()
            self.ACT(s[:], self.xres[:, k, :], AF.Square, [("x", k)], [sk])
            self.MM(s1[:, :], self.ones[:], self.xres[:, k, :], k == 0, k == KD - 1, ["ones", ("x", k)], [s1k])
            self.MM(s2[:, :], self.ones[:], s[:], k == 0, k == KD - 1, ["ones", sk], [s2k])
        self.TS(self.mean[:], s1[:, :], 1.0 / D, None, ALU.mult, None, [s1k], ["mean"])
        self.TT(self.tmpv[:], self.mean[:], self.mean[:], ALU.mult, ["mean"], ["tmpv"])
        self.STT(self.tmpv[:], s2[:, :], 1.0 / D, self.tmpv[:], ALU.mult, ALU.subtract, [s2k, "tmpv"], ["tmpv"])
        self.TS(self.tmpv[:], self.tmpv[:], 1e-5, None, ALU.add, None, ["tmpv"], ["tmpv"])
        self.ACT(self.tmpv[:], self.tmpv[:], AF.Sqrt, ["tmpv"], ["tmpv"])
        self.P.op("dve", lambda e: e.reciprocal(out=self.rstd[:], in_=self.tmpv[:]), reads=["tmpv"], writes=["rstd"])
        gi = (l * 2 + which) * KD
        for k in range(KD):
            self.TT(self.xres[:, k, :], self.xres[:, k, :], self.mean[:], ALU.subtract, [("x", k), "mean"], [("x", k)])
            self.TT(self.xres[:, k, :], self.xres[:, k, :], self.rstd[:], ALU.mult, [("x", k), "rstd"], [("x", k)])
            self.ACT(self.xres[:, k, :], self.xres[:, k, :], AF.Identity, [("x", k), "g_sb", "b_sb"], [("x", k)],
                     scale=self.g_sb[:, gi + k:gi + k + 1], bias=self.b_sb[:, gi + k:gi + k + 1])

    def rglru(self, s, resid, p):
        w_in = self.r_w_in[s]
        w_out = self.r_w_out[s]
        U = self.U
        SC = self.SC
        def sc(i, w=2 * TP):
            return SC[:80, i * 1024:i * 1024 + w]
        recb = SC[:80, 6144:6144 + 2 * 515].rearrange("p (i t) -> p i t", i=2)
        xs = sc(0).rearrange("p (i t) -> p i t", i=2)
        xr = sc(1).rearrange("p (i t) -> p i t", i=2)
        rg = sc(2).rearrange("p (i t) -> p i t", i=2)
        ig = sc(3).rearrange("p (i t) -> p i t", i=2)
        aa = sc(4).rearrange("p (i t) -> p i t", i=2)
        bb = sc(5).rearrange("p (i t) -> p i t", i=2)
        xrb = self.xrb
        cw = lambda j, c: self.rv[:, s, j, c:c + 1]

        def gate_blk(c, pt, pk):
            x = xs[:, 0, :]
            self.ACT(x, pt[:80, :], AF.Copy, [pk], ["xs"])
            self.ACT(xr[:, 0, :], pt[:80, :], AF.Square, [pk], ["xr"])
            self.TS(xr[:, 0, :], xr[:, 0, :], 0.044715, 1.0, ALU.mult, ALU.add, ["xr"], ["xr"])
            self.TT(xr[:, 0, :], xr[:, 0, :], x, ALU.mult, ["xr", "xs"], ["xr"])
            self.ACT(xr[:, 0, :], xr[:, 0, :], AF.Sigmoid, ["xr"], ["xr"], scale=1.5957691216057308)
            self.TT(U[:80, c, :], xr[:, 0, :], x, ALU.mult, ["xr", "xs"], [("U", c)])

        self.linear(w_in, 128, KD, 0, RW, 80, self.hact, gate_blk)

        def rec_blk(c, pt, pk):
            i = c % 2
            n = c // 2
            self.ACT(recb[:, i, 3:515], pt[:80, :], AF.Copy, [pk], ["recb"])
            self.CP("dve", recb[:, i, 0:3], self.tails[:, s, c, :], ["tails"], ["recb"])
            self.CP("dve", self.tails[:, s, c, :], recb[:, i, 512:515], ["recb"], ["tails"])
            self.TS(xr[:, i, :], recb[:, i, 0:512], cw(0, c), cw(4, c), ALU.mult, ALU.add, ["recb", "rv"], ["xr"])
            for j in (1, 2, 3):
                self.STT(xr[:, i, :], recb[:, i, j:j + 512], cw(j, c), xr[:, i, :], ALU.mult, ALU.add, ["recb", "rv", "xr"], ["xr"])
            if i == 0:
                return
            self.CP("dve", xrb[:, :, :], xr[:, :, :], ["xr"], ["xrb"])
            gwt = self.gw[self.gwi % 2]
            gk = ("gw", self.gwi % 2)
            self.gwi += 1
            self.DMA("pool", f"gw{gk[1]}a", gwt[:, 0, :, :], self.r_wrg[s, n].rearrange("(i p) e -> p i e", p=80), [], [gk])
            self.DMA("pool", f"gw{gk[1]}b", gwt[:, 1, :, :], self.r_wig[s, n].rearrange("(i p) e -> p i e", p=80), [], [gk])
            for g, dst, bj in ((0, rg, 5), (1, ig, 6)):
                for j in range(2):
                    gp, gpk = self.psum()
                    for ii in range(2):
                        self.MM(gp[:80, :], gwt[:, g, ii, j * 80:(j + 1) * 80], xrb[:, ii, :], ii == 0, ii == 1, [gk, "xrb"], [gpk])
                    cc = 2 * n + j
                    self.ACT(dst[:, j, :], gp[:80, :], AF.Sigmoid, [gpk, "rv"], ["rg" if g == 0 else "ig"], bias=self.rv[:, s, bj, cc:cc + 1])
            for j in range(2):
                cc = 2 * n + j
                self.ACT(aa[:, j, :], rg[:, j, :], AF.Exp, ["rg", "cneg"], ["aa"], scale=self.cneg[:, s, 0, cc:cc + 1])
                self.ACT(bb[:, j, :], rg[:, j, :], AF.Exp, ["rg", "cneg"], ["bb"], scale=self.cneg[:, s, 1, cc:cc + 1])
            self.TS(bb[:, :, :], bb[:, :, :], -1.0, 1.0, ALU.mult, ALU.add, ["bb"], ["bb"])
            self.TS(bb[:, :, :], bb[:, :, :], 0.0, None, ALU.max, None, ["bb"], ["bb"])
            self.ACT(bb[:, :, :], bb[:, :, :], AF.Sqrt, ["bb"], ["bb"])
            self.TT(bb[:, :, :], bb[:, :, :], ig[:, :, :], ALU.mult, ["bb", "ig"], ["bb"])
            self.TT(bb[:, :, :], bb[:, :, :], xr[:, :, :], ALU.mult, ["bb", "xr"], ["bb"])
            for j in range(2):
                cc = 2 * n + j
                self.P.op("dve", lambda e, j=j, cc=cc: e.tensor_tensor_scan(out=rg[:, j, :], data0=aa[:, j, :], data1=bb[:, j, :],
                                                                            initial=self.hstate[:, s, cc:cc + 1], op0=ALU.mult, op1=ALU.add),
                          reads=["aa", "bb", "hstate"], writes=["rg"])
                self.CP("dve", self.hstate[:, s, cc:cc + 1], rg[:, j, TP - 1:TP], ["rg"], ["hstate"])
            self.TT(U[:80, 2 * n:2 * n + 2, :], U[:80, 2 * n:2 * n + 2, :], rg[:, :, :], ALU.mult,
                    [("U", 2 * n), ("U", 2 * n + 1), "rg"], [("U", 2 * n), ("U", 2 * n + 1)])

        self.linear(w_in, 128, KD, RW, RW, 80, self.hact, rec_blk)
        self.linear(w_out, 80, RCH, 0, D, 128, lambda k: (U[:80, k, :], ("U", k)), resid)


    def mix_setup(self):
        if hasattr(self, "UF"):
            return
        nc = self.nc
        self.UB = self.U[:].rearrange("p a b -> p (a b)")
        self.UF = self.UB.bitcast(F32)
        self.SCB = self.SC[:].bitcast(BF16)
        self.ST = self.SC[:, 0:4096]
        cst = self.inp("cst", [128, 1024])
        self.cst = self.T("cst_sb", [128, 1024])
        self.identb = self.T("identb", [128, 128], BF16)
        self.DMA("sp", "c1", self.cst[:], cst, [], ["cst"])
        self.CP("dve", self.identb[:], self.cst[:, 0:128], ["cst"], ["identb"])
        self.cstb = self.T("cstb", [128, 1024], BF16)
        self.CP("dve", self.cstb[:], self.cst[:], ["cst"], ["cst"])
        self.identf = self.cst[:, 0:128]
        self.tri_ge = self.cst[:, 128:192]
        self.tri_gt = self.cst[:, 192:256]
        self.ntri_gt = self.cst[:, 896:960]
        self.cmask = self.cst[:, 256:768]
        self.ones64 = self.cst[:, 768:896]

    def ufs(self, off, n):
        return self.UF[:, off:off + n]

    def ubs(self, off, n):
        return self.UB[:, 2 * off:2 * off + n]

    def gla(self, resid, p):
        self.mix_setup()
        nc = self.nc
        Win = self.inp("gla_w_in", [1, D, 6160])[0]
        Wout = self.inp("gla_w_out", [1, D, D])[0]
        if not hasattr(self, "wup"):
            wupd = self.inp("gla_w_alpha_up", [1, 16, 1024])[0]
            glav = self.inp("glav", [128, 12])
            self.wup = self.T("wup_sb", [16, 1024], BF16)
            self.glv = self.T("glv_sb", [128, 12])
            self.st_gla = nc.dram_tensor("st_gla", [128, 4096], F32).ap()
            self.DMA("pool", "wupd", self.wup[:], wupd, [], ["wup"])
            self.DMA("sp", "c1", self.glv[:], glav, [], ["glv"])
            self.TS(self.glv[:, 0:8], self.glv[:, 0:8], -1.0, None, ALU.mult, None, ["glv"], ["glv"])
        nba = lambda z: self.glv[:, z:z + 1]
        nw = lambda e: self.glv[:, 8 + e:9 + e]
        ST = self.ST
        if p == 0:
            self.P.op("dve", lambda e: e.memset(ST, 0.0), writes=["ST"])
        else:
            self.DMA("sp", "stl_gla", ST, self.st_gla, ["st_gla", ("x", 0)], ["ST"])
        v3 = lambda ap_: ap_.rearrange("p (d t) -> p d t", d=2)
        bc = v3(self.ufs(4096, 1024)); Ep = v3(self.ufs(5120, 1024)); Em = v3(self.ufs(6144, 1024))
        qd = v3(self.ubs(7168, 1024)); ki = v3(self.ubs(7680, 1024)); kt = v3(self.ubs(8192, 1024))
        ktok = self.ubs(8704, 2048)[:64].rearrange("p (c d e) -> p c d e", c=8, d=2)
        vtok = self.ubs(9728, 4096)[:64].rearrange("p (c e) -> p c e", c=8)
        sg = self.ufs(11776, 2048).rearrange("p (a t) -> p a t", a=4)
        t1 = self.ufs(14336, 512); rinv = self.ufs(14848, 512)
        Sbf = v3(self.ubs(15360, 1024))
        AT = self.ubs(15872, 64)[:64]
        alow = self.ubs(15904, 512)[:16]
        Eend = self.ufs(16160, 16).rearrange("p (d c) -> p d c", d=2)

        def alow_blk(n, pt, pk):
            self.CP("act", alow, pt[:16, :], [pk], ["alow"])
        self.linear(Win, 128, KD, 6144, 16, 16, self.hact, alow_blk, cpt=1)
        for hd in range(4):
            Sv = v3(ST[:, hd * 1024:(hd + 1) * 1024])
            for dc in range(2):
                zc = hd * 2 + dc
                zp, zk = self.psum()
                self.MM(zp[:, :], self.wup[:16, zc * 128:(zc + 1) * 128], alow, True, True, ["wup", "alow"], [zk])
                self.ACT(Ep[:, dc, :], zp[:, :], AF.Exp, [zk, "glv"], ["Ep"], scale=-1.0, bias=nba(zc))
                self.ACT(Ep[:, dc, :], Ep[:, dc, :], AF.Ln, ["Ep"], ["Ep"], bias=1.0)
                self.TS(Ep[:, dc, :], Ep[:, dc, :], -1.0 / 16.0, None, ALU.mult, None, ["Ep"], ["Ep"])
                self.P.op("dve", lambda e, dc=dc: e.tensor_tensor_scan(out=bc[:, dc, :], data0=self.cmask, data1=Ep[:, dc, :], initial=0.0,
                                                                       op0=ALU.mult, op1=ALU.add), reads=["Ep", "cst"], writes=["bc"])
            self.ACT(Eend[:, :, :], bc.rearrange("p d (c t) -> p d c t", t=64)[:, :, :, 63], AF.Exp, ["bc"], ["Eend"])
            self.ACT(Ep[:, :, :], bc[:, :, :], AF.Exp, ["bc", "Ep"], ["Ep"])
            self.ACT(Em[:, :, :], bc[:, :, :], AF.Exp, ["bc"], ["Em"], scale=-1.0)

            def q_blk(n, pt, pk):
                self.STT(qd[:, n, :], pt[:, :], 1.0 / 16.0, Ep[:, n, :], ALU.mult, ALU.mult, [pk, "Ep"], ["qd"])
            self.linear(Win, 128, KD, hd * 256, 256, 128, self.hact, q_blk, cpt=2)

            def k_blk(n, pt, pk):
                self.TT(ki[:, n, :], pt[:, :], Em[:, n, :], ALU.mult, [pk, "Em"], ["ki"])
                self.TT(kt[:, n, :].rearrange("p (c t) -> p c t", t=64), ki[:, n, :].rearrange("p (c t) -> p c t", t=64),
                        Eend[:, n, :, None].broadcast_to([128, 8, 64]), ALU.mult, ["ki", "Eend"], ["kt"])
            self.linear(Win, 128, KD, 1024 + hd * 256, 256, 128, self.hact, k_blk, cpt=2)
            for c in range(8):
                for dc in range(2):
                    tp, tk = self.psum()
                    self.MM(tp[:64, 0:128], kt[:, dc, c * 64:(c + 1) * 64], self.identb[:, :], True, True, ["kt", "identb"], [tk])
                    self.CP("act", ktok[:, c, dc, :], tp[:64, 0:128], [tk], ["ktok"])
            getw = self.wfull(Win[:, 2048 + hd * 512:2048 + (hd + 1) * 512], 512)
            for c in range(8):
                vp, vk = self.psum()
                for k in range(KD):
                    wt, wk = getw(k)
                    self.MM(vp[:64, :], self.h[:, k, c * 64:(c + 1) * 64], wt, k == 0, k == KD - 1, [wk, ("h", k)], [vk])
                self.CP("dve", vtok[:, c, :], vp[:64, :], [vk], ["vtok"])

            def g_blk(n, pt, pk):
                self.ACT(sg[:, n, :], pt[:, :], AF.Silu, [pk], ["sg"])
            self.linear(Win, 128, KD, 4096 + hd * 512, 512, 128, self.hact, g_blk, cpt=4)
            self.CP("dve", Sbf[:, :, :], Sv[:, :, :], ["ST"], ["Sbf"])
            ob = [(self.ps[i], ("ps", i)) for i in range(4)]
            for c in range(8):
                cs = slice(c * 64, (c + 1) * 64)
                a_p, a_k = self.ps[6], ("ps", 6)
                for dc in range(2):
                    self.MM(a_p[:64, 0:64], ki[:, dc, cs], qd[:, dc, cs], dc == 0, dc == 1, ["ki", "qd"], [a_k])
                self.TT(AT, a_p[:64, 0:64], self.tri_ge[:64, :], ALU.mult, [a_k, "cst"], ["AT"])
                for ec in range(4):
                    op_, ok_ = ob[ec]
                    for dc in range(2):
                        self.MM(op_[:, cs], Sbf[:, dc, ec * 128:(ec + 1) * 128], qd[:, dc, cs], dc == 0, False, ["Sbf", "qd"], [ok_])
                    self.MM(op_[:, cs], vtok[:, c, ec * 128:(ec + 1) * 128], AT, False, True, ["vtok", "AT"], [ok_])
                for dc in range(2):
                    s_p, s_k = self.ps[4 + dc], ("ps", 4 + dc)
                    self.MM(s_p[:, :], ktok[:, c, dc, :], vtok[:, c, :], True, True, ["ktok", "vtok"], [s_k])
                    self.STT(Sv[:, dc, :], Sv[:, dc, :], Eend[:, dc, c:c + 1], s_p[:, :], ALU.mult, ALU.add, ["ST", "Eend", s_k], ["ST"])
                self.CP("act", Sbf[:, :, :], Sv[:, :, :], ["ST"], ["Sbf"])
            r_p, r_k = self.ps[7], ("ps", 7)
            for ec in range(4):
                s_, sk_ = self.sq()
                self.ACT(s_[:], ob[ec][0][:, :], AF.Square, [ob[ec][1]], [sk_])
                self.MM(r_p[:, :], self.ones[:], s_[:], ec == 0, ec == 3, ["ones", sk_], [r_k])
            self.TS(rinv, r_p[:, :], 1.0 / 512.0, 1e-6, ALU.mult, ALU.add, [r_k], ["rinv"])
            self.ACT(rinv, rinv, AF.Sqrt, ["rinv"], ["rinv"])
            self.P.op("dve", lambda e: e.reciprocal(out=rinv, in_=rinv), reads=["rinv"], writes=["rinv"])
            for ec in range(4):
                self.TT(t1, ob[ec][0][:, :], rinv, ALU.mult, [ob[ec][1], "rinv"], ["t1"])
                self.STT(self.U[:, hd * 4 + ec, :], sg[:, ec, :], nw(ec), t1, ALU.mult, ALU.mult, ["sg", "glv", "t1"], [("U", hd * 4 + ec)])
        self.DMA("sp", "sts_gla", self.st_gla, ST, ["ST"], ["st_gla", "stg"])
        self.linear(Wout, 128, KD, 0, D, 128, lambda k: (self.U[:, k, :], ("U", k)), resid)

    def gdn(self, resid, p):
        self.mix_setup()
        nc = self.nc
        Win = self.inp("gdn_w_in", [1, D, 12352])[0]
        Wout = self.inp("gdn_w_out", [1, 4096, D])[0]
        if not hasattr(self, "gdv"):
            gdnv = self.inp("gdnv", [128, 4 * 64 + 1 + 64])
            self.gdv = self.T("gdv_sb", [128, 4 * 64 + 1 + 64])
            self.gtail = self.T("gtail", [128, 64, 3])
            self.st_gdn = nc.dram_tensor("st_gdn", [128, 4096], F32).ap()
            self.DMA("sp", "c1", self.gdv[:], gdnv, [], ["gdv"])
            self.P.op("dve", lambda e: e.memset(self.gtail[:], 0.0), writes=["gtail"])
            self.ACT(self.gdv[:64, 257:289], self.gdv[:64, 257:289], AF.Exp, ["gdv"], ["gdv"])
            self.TS(self.gdv[:64, 257:289], self.gdv[:64, 257:289], -1.0, None, ALU.mult, None, ["gdv"], ["gdv"])
        cw = lambda j, ch: self.gdv[:, j * 64 + ch:j * 64 + ch + 1]
        normw = self.gdv[:, 256:257]
        negA = self.gdv[:64, 257:289]
        dtb = self.gdv[:64, 289:321]
        ST = self.ST
        if p == 0:
            self.P.op("dve", lambda e: e.memset(ST, 0.0), writes=[("ST", h_) for h_ in range(32)])
        else:
            self.DMA("sp", "stl_gdn", ST, self.st_gdn, ["st_gdn", ("x", 0)], [("ST", h_) for h_ in range(32)])
        o = [8192]

        def F(n, parts=128):
            a = self.ufs(o[0], n)[:parts]
            o[0] += (n + 7) // 8 * 8
            return a

        def B(n, parts=128):
            a = self.ubs(o[0], n)[:parts]
            o[0] += ((n + 1) // 2 + 7) // 8 * 8
            return a
        r8 = lambda ap_: ap_.rearrange("p (c x) -> p c x", c=8)
        ba = r8(F(512, 64))
        beta = r8(F(256, 64))
        gg = r8(self.SC[:64, 6656:6912])
        gc = r8(F(256, 64))
        bg = r8(F(256, 64))
        tailf = r8(F(256, 64))
        dend = r8(self.SC[:, 6912:7168])
        cbuf = F(515)
        qn = B(512); kn = B(512); vT = B(512)
        acc = F(512); rn = F(512); t1 = F(512)
        Gs = r8(F(512, 64))
        QK = r8(F(512, 64))
        ktk = r8(self.SC[:64, 4096:5120])
        sets = []
        for si in range(2):
            S_ = {}
            S_["vbt"] = r8(self.SC[:64, 5120 + 512 * si:5632 + 512 * si].bitcast(BF16))
            S_["szT"] = B(512)
            for nm in ("dg", "tmpa", "Dm", "DT", "dgt"):
                S_[nm] = F(64, 64)
            for nm in ("dgh", "dgl", "Uu", "Aq"):
                S_[nm] = B(64, 64)
            for nm in ("NMa", "NMb"):
                S_[nm] = B(128, 64)
            S_["kb"] = B(128, 64); S_["usb"] = F(128, 64); S_["wT"] = B(64); S_["vn"] = B(128, 64)
            S_["qdc"] = B(64); S_["ktt"] = B(128, 64); S_["Sbf"] = B(128); S_["eq"] = F(64)
            sets.append(S_)
        assert o[0] <= 16384, o[0]

        getw = self.wfull(Win[:, 12288:12352], 64)
        for c in range(8):
            bp, bk = self.psum()
            for k in range(KD):
                wt, wk = getw(k)
                self.MM(bp[:64, 0:64], self.h[:, k, c * 64:(c + 1) * 64], wt, k == 0, k == KD - 1, [wk, ("h", k)], [bk])
            self.CP("act", ba[:, c, :], bp[:64, 0:64], [bk], ["ba"])
        self.ACT(beta[:, :, :], ba[:, :, 0:32], AF.Sigmoid, ["ba"], ["beta"])
        self.TT(gg[:, :, :], ba[:, :, 32:64], dtb[:, None, :].broadcast_to([64, 8, 32]), ALU.add, ["ba", "gdv"], ["gg"])
        self.ACT(gg[:, :, :], gg[:, :, :], AF.Exp, ["gg"], ["gg"])
        self.ACT(gg[:, :, :], gg[:, :, :], AF.Ln, ["gg"], ["gg"], bias=1.0)
        self.TT(gg[:, :, :], gg[:, :, :], negA[:, None, :].broadcast_to([64, 8, 32]), ALU.mult, ["gg", "gdv"], ["gg"])
        ghi = self.SC[:64, 6144:6272].bitcast(BF16).rearrange("p (c x) -> p c x", c=8)
        glo = self.SC[:64, 6272:6400].bitcast(BF16).rearrange("p (c x) -> p c x", c=8)
        gtmp = self.SC[:64, 6400:6656].rearrange("p (c x) -> p c x", c=8)
        trib = self.cstb[:64, 128:192]
        onesb = self.cstb[:64, 768:896]
        self.CP("dve", ghi, gg[:, :, :], ["gg"], ["ghi"])
        self.TT(gtmp, gg[:, :, :], ghi, ALU.subtract, ["gg", "ghi"], ["gtmp"])
        self.CP("dve", glo, gtmp, ["gtmp"], ["glo"])
        for c in range(8):
            cp_, ck_ = self.psum()
            self.MM(cp_[:64, 0:32], trib, ghi[:, c, :], True, False, ["cst", "ghi"], [ck_])
            self.MM(cp_[:64, 0:32], trib, glo[:, c, :], False, True, ["cst", "glo"], [ck_])
            self.CP("act", gc[:, c, :], cp_[:64, 0:32], [ck_], ["gc"])
            ep_, ek_ = self.psum()
            self.MM(ep_[:, 0:32], onesb, ghi[:, c, :], True, False, ["cst", "ghi"], [ek_])
            self.MM(ep_[:, 0:32], onesb, glo[:, c, :], False, True, ["cst", "glo"], [ek_])
            self.ACT(dend[:, c, :], ep_[:, 0:32], AF.Exp, [ek_], ["dend"])
            self.TT(tailf[:, c, :], ep_[:64, 0:32], gc[:, c, :], ALU.subtract, [ek_, "gc"], ["tailf"])
        self.ACT(tailf[:, :, :], tailf[:, :, :], AF.Exp, ["tailf"], ["tailf"])
        self.ACT(bg[:, :, :], gc[:, :, :], AF.Exp, ["gc"], ["bg"])
        self.TT(bg[:, :, :], bg[:, :, :], beta[:, :, :], ALU.mult, ["bg", "beta"], ["bg"])

        import os as _os3
        _cs = int(_os3.environ.get("GDN_CS", "9"))

        def conv_silu(ch, pt, pk, dst, dkey, l2=None):
            self.ACT(cbuf[:, 3:515], pt[:, :], AF.Copy, [pk], ["cbuf"])
            if _cs <= 1:
                return
            self.CP("dve", cbuf[:, 0:3], self.gtail[:, ch, :], ["gtail"], ["cbuf"])
            self.CP("dve", self.gtail[:, ch, :], cbuf[:, 512:515], ["cbuf"], ["gtail"])
            self.TS(acc, cbuf[:, 0:512], cw(0, ch), None, ALU.mult, None, ["cbuf", "gdv"], ["acc"])
            for j in (1, 2, 3):
                self.STT(acc, cbuf[:, j:j + 512], cw(j, ch), acc, ALU.mult, ALU.add, ["cbuf", "gdv", "acc"], ["acc"])
            if _cs <= 2:
                return
            if l2 is None:
                self.ACT(dst, acc, AF.Silu, ["acc"], [dkey])
                return
            self.ACT(acc, acc, AF.Silu, ["acc"], ["acc"])
            if _cs <= 3:
                return
            s_, sk_ = self.sq()
            self.ACT(s_[:], acc, AF.Square, ["acc"], [sk_])
            lp, lk = self.psum()
            self.MM(lp[:, :], self.ones[:], s_[:], True, True, ["ones", sk_], [lk])
            if _cs <= 4:
                return
            self.TS(rn, lp[:, :], 1e-6, None, ALU.add, None, [lk], ["rn"])
            if _cs <= 5:
                return
            self.ACT(rn, rn, AF.Sqrt, ["rn"], ["rn"])
            if _cs <= 6:
                return
            self.P.op("dve", lambda e: e.reciprocal(out=rn, in_=rn), reads=["rn"], writes=["rn"])
            if _cs <= 7:
                return
            self.STT(dst, acc, l2, rn, ALU.mult, ALU.mult, ["acc", "rn"], [dkey])

        import os as _os
        _dbg = int(_os.environ.get("GDN_DBG", "0"))
        for hq in range(16 if _dbg != 3 else 0):
            self.linear(Win, 128, KD, hq * 128, 128, 128, self.hact,
                        lambda n, pt, pk: conv_silu(hq, pt, pk, qn, "qn", l2=128.0 ** -0.5), cpt=1)
            self.linear(Win, 128, KD, 2048 + hq * 128, 128, 128, self.hact,
                        lambda n, pt, pk: conv_silu(16 + hq, pt, pk, kn, "kn", l2=1.0), cpt=1)
            for c in range(8 if _dbg != 5 else 0):
                cs = slice(c * 64, (c + 1) * 64)
                g_p, g_k = self.psum()
                if _dbg not in (6, 7, 8):
                    self.MM(g_p[:64, 0:64], kn[:, cs], kn[:, cs], True, True, ["kn"], [g_k])
                if _dbg != 8:
                    self.MM(g_p[:64, 64:128], kn[:, cs], qn[:, cs], True, True, ["kn", "qn"], [g_k])
                if _dbg != 7:
                    self.MM(g_p[:64, 128:256], kn[:, cs], self.identb[:, :], True, True, ["kn", "identb"], [g_k])
                if _dbg not in (6, 7, 8):
                    self.CP("act", Gs[:, c, :], g_p[:64, 0:64], [g_k], ["Gs"])
                if _dbg != 8:
                    self.CP("act", QK[:, c, :], g_p[:64, 64:128], [g_k], ["QK"])
                if _dbg != 7:
                    self.CP("act", ktk[:, c, :], g_p[:64, 128:256], [g_k], ["ktk"])
            hs = (2 * hq, 2 * hq + 1) if _dbg not in (4, 5, 6, 7, 8) else ()
            for si, h in enumerate(hs):
                S_ = sets[si]
                self.linear(Win, 128, KD, 4096 + h * 128, 128, 128, self.hact,
                            lambda n, pt, pk: conv_silu(32 + h, pt, pk, vT, "vT"), cpt=1)
                self.linear(Win, 128, KD, 8192 + h * 128, 128, 128, self.hact,
                            lambda n, pt, pk: self.ACT(S_["szT"], pt[:, :], AF.Silu, [pk], [f"szT{si}"]), cpt=1)
                for c in range(8):
                    v_p, v_k = self.psum()
                    self.MM(v_p[:64, 0:128], vT[:, c * 64:(c + 1) * 64], self.identb[:, :], True, True, ["vT", "identb"], [v_k])
                    self.TS(S_["vbt"][:, c, :], v_p[:64, 0:128], beta[:, c, h:h + 1], None, ALU.mult, None, [v_k, "beta"], [f"vbt{si}"])

            def chain(si, h):
                S_ = sets[si]
                k_ = lambda nm: f"{nm}{si}"
                dg, tmpa, Dm, DT, dgt, dgh, dgl = S_["dg"], S_["tmpa"], S_["Dm"], S_["DT"], S_["dgt"], S_["dgh"], S_["dgl"]
                NMs = [S_["NMa"], S_["NMb"]]
                NN = [t_[:, 0:64] for t_ in NMs]
                MM_ = [t_[:, 64:128] for t_ in NMs]
                Uu = S_["Uu"]
                kb, usb, wT, vn, Aq, qdc, ktt, Sbf, eq = (S_[x] for x in ("kb", "usb", "wT", "vn", "Aq", "qdc", "ktt", "Sbf", "eq"))
                vbt_, szT_ = S_["vbt"], S_["szT"]
                b0 = 4 * si
                o_p, o_k = self.ps[b0], ("ps", b0)
                P1, K1 = self.ps[b0 + 1], ("ps", b0 + 1)
                P2, K2 = self.ps[b0 + 2], ("ps", b0 + 2)
                P3, K3 = self.ps[b0 + 3], ("ps", b0 + 3)
                Sv = ST[:, h * 128:(h + 1) * 128]
                sk = ("ST", h)
                self.CP("dve", Sbf, Sv, [sk], [k_("Sbf")])
                yield
                for c in range(8 if _dbg != 1 else 0):
                    cs = slice(c * 64, (c + 1) * 64)
                    gci = gc[:, c, h:h + 1]
                    self.TS(dg, self.identf[:64, 0:64], gci, None, ALU.mult, None, ["cst", "gc"], [k_("dg")]); yield
                    self.CP("dve", dgh, dg, [k_("dg")], [k_("dgh")]); yield
                    self.TT(dgt, dg, dgh, ALU.subtract, [k_("dg"), k_("dgh")], [k_("dgt")]); yield
                    self.CP("dve", dgl, dgt, [k_("dgt")], [k_("dgl")]); yield
                    self.MM(P1[:, 0:64], onesb, dgh, True, False, ["cst", k_("dgh")], [K1])
                    self.MM(P1[:, 0:64], onesb, dgl, False, True, ["cst", k_("dgl")], [K1]); yield
                    self.TS(tmpa, P1[:64, 0:64], gci, 0.0, ALU.subtract, ALU.max, [K1, "gc"], [k_("tmpa")]); yield
                    self.ACT(Dm, tmpa, AF.Exp, [k_("tmpa")], [k_("Dm")], scale=-1.0); yield
                    self.TS(DT, P1[:64, 0:64], gci, 0.0, ALU.subtract, ALU.min, [K1, "gc"], [k_("DT")]); yield
                    self.ACT(DT, DT, AF.Exp, [k_("DT")], [k_("DT")]); yield
                    self.ACT(eq, P1[:, 0:64], AF.Exp, [K1], [k_("eq")]); yield
                    self.TT(qdc, qn[:, cs], eq, ALU.mult, ["qn", k_("eq")], [k_("qdc")]); yield
                    Nn, Mm = NN[0], MM_[0]
                    self.TT(Nn, Gs[:, c, :], Dm, ALU.mult, ["Gs", k_("Dm")], [k_("N0")]); yield
                    self.STT(Nn, Nn, beta[:, c, h:h + 1], self.ntri_gt[:64, :], ALU.mult, ALU.mult, [k_("N0"), "beta", "cst"], [k_("N0")]); yield
                    self.MM(P2[:64, 0:64], Nn, self.identb[:64, 0:64], True, True, [k_("N0"), "identb"], [K2]); yield
                    self.CP("act", Mm, P2[:64, 0:64], [K2], [k_("M0")]); yield
                    self.TT(Uu, Mm, self.identf[:64, 0:64], ALU.add, [k_("M0"), "cst"], [k_("Uu")]); yield
                    cur = 0
                    for lvl in range(5):
                        nx = 1 - cur
                        Nc, Mc, Nx, Mx = NN[cur], MM_[cur], NN[nx], MM_[nx]
                        kN, kM, kNx, kMx = k_(f"N{cur}"), k_(f"M{cur}"), k_(f"N{nx}"), k_(f"M{nx}")
                        self.MM(P2[:64, 64:128], Mc, Nc, True, True, [kM, kN], [K2])
                        if lvl < 4:
                            self.MM(P2[:64, 128:192], Nc, Mc, True, True, [kM, kN], [K2])
                        yield
                        if lvl < 4:
                            self.CP("act", NMs[nx][:, 0:128], P2[:64, 64:192], [K2], [kNx, kMx]); yield
                        else:
                            self.CP("act", Nx, P2[:64, 64:128], [K2], [kNx]); yield
                        self.MM(P3[:64, 0:64], Nx, Uu, True, True, [kNx, k_("Uu")], [K3]); yield
                        self.TT(Uu, Uu, P3[:64, 0:64], ALU.add, [k_("Uu"), K3], [k_("Uu")]); yield
                        cur = nx
                    self.TS(kb, ktk[:, c, :], bg[:, c, h:h + 1], None, ALU.mult, None, ["ktk", "bg"], [k_("kb")]); yield
                    self.MM(P2[:, 256:320], kb, Uu, True, True, [k_("kb"), k_("Uu")], [K2]); yield
                    self.CP("act", wT, P2[:, 256:320], [K2], [k_("wT")]); yield
                    self.MM(P3[:64, 128:256], Uu, vbt_[:, c, :], True, True, [k_("Uu"), k_("vbt")], [K3]); yield
                    self.CP("act", usb, P3[:64, 128:256], [K3], [k_("usb")]); yield
                    self.MM(P3[:64, 256:384], wT, Sbf, True, True, [k_("wT"), k_("Sbf")], [K3]); yield
                    self.TT(vn, usb, P3[:64, 256:384], ALU.subtract, [k_("usb"), K3], [k_("vn")]); yield
                    self.TT(tmpa, QK[:, c, :], DT, ALU.mult, ["QK", k_("DT")], [k_("tmpa")]); yield
                    self.TT(Aq, tmpa, self.tri_ge[:64, :], ALU.mult, [k_("tmpa"), "cst"], [k_("Aq")]); yield
                    self.MM(o_p[:, cs], Sbf, qdc, True, False, [k_("Sbf"), k_("qdc")], [o_k])
                    self.MM(o_p[:, cs], vn, Aq, False, True, [k_("vn"), k_("Aq")], [o_k]); yield
                    self.TS(ktt, ktk[:, c, :], tailf[:, c, h:h + 1], None, ALU.mult, None, ["ktk", "tailf"], [k_("ktt")]); yield
                    self.MM(P2[:, 384:512], ktt, vn, True, True, [k_("ktt"), k_("vn")], [K2]); yield
                    self.STT(Sv, Sv, dend[:, c, h:h + 1], P2[:, 384:512], ALU.mult, ALU.add, [sk, "dend", K2], [sk]); yield
                    self.CP("act", Sbf, Sv, [sk], [k_("Sbf")]); yield
                s_, sk_ = self.sq()
                self.ACT(s_[:], o_p[:, :], AF.Square, [o_k], [sk_])
                self.MM(P1[:, :], self.ones[:], s_[:], True, True, ["ones", sk_], [K1])
                self.TS(rn, P1[:, :], 1.0 / 128.0, 1e-6, ALU.mult, ALU.add, [K1], ["rn"])
                self.ACT(rn, rn, AF.Sqrt, ["rn"], ["rn"])
                self.P.op("dve", lambda e: e.reciprocal(out=rn, in_=rn), reads=["rn"], writes=["rn"])
                self.TT(t1, o_p[:, :], rn, ALU.mult, [o_k, "rn"], ["t1"])
                self.STT(self.U[:, h, :], szT_, normw, t1, ALU.mult, ALU.mult, [k_("szT"), "gdv", "t1"], [("U", h)])
                yield

            gens = [chain(si, h) for si, h in enumerate(hs)]
            while gens:
                for g_ in list(gens):
                    try:
                        next(g_)
                    except StopIteration:
                        gens.remove(g_)
        self.DMA("sp", "sts_gdn", self.st_gdn, ST, [("ST", h_) for h_ in range(32)], ["st_gdn", "stg"])
        self.linear(Wout, 128, 32, 0, D, 128, lambda k: (self.U[:, k, :], ("U", k)), resid)


_CACHE = {}


def _vecP(v, p=128):
    v = np.asarray(v, np.float32)
    lead = v.shape[:-1]
    c = v.shape[-1] // p
    v = v.reshape(*lead, c, p)
    v = np.moveaxis(v, -1, 0)
    return np.ascontiguousarray(v.reshape(p, -1))


def make_inputs(inputs, b):
    f = lambda a: np.ascontiguousarray(np.asarray(a, np.float32))
    m = {}
    m["xT"] = np.ascontiguousarray(np.asarray(inputs["x"][b], np.float32).T)
    m["cT"] = _vecP(inputs["c"][b])
    m["lng"] = _vecP(inputs["ln_g"])
    m["lnb"] = _vecP(inputs["ln_b"])
    m["bmod"] = _vecP(inputs["b_mod"])
    for k in ("w_mod", "w_ff1", "w_ff2", "rglru_w_in", "rglru_w_out", "rglru_w_rgate", "rglru_w_igate",
              "gla_w_in", "gla_w_out", "gla_w_alpha_up", "gdn_w_in", "gdn_w_out"):
        m[k] = f(inputs[k])
    cst = np.zeros((128, 1024), np.float32)
    cst[:, 0:128] = np.eye(128, dtype=np.float32)
    pp = np.arange(128)[:, None]
    ff = np.arange(64)[None, :]
    cst[:, 128:192] = (ff >= pp).astype(np.float32)
    cst[:, 192:256] = (ff < pp).astype(np.float32)
    cst[:, 256:768] = ((np.arange(512) % 64) != 0).astype(np.float32)[None, :]
    cst[:, 768:896] = 1.0
    cst[:, 896:960] = -(ff < pp).astype(np.float32)
    m["cst"] = cst
    m["glav"] = np.concatenate([_vecP(inputs["gla_b_alpha"][0]), _vecP(inputs["gla_norm_w"][0])], axis=1)
    gd = np.zeros((128, 321), np.float32)
    gd[:, 0:256] = _vecP(inputs["gdn_conv_w"][0])
    gd[:, 256] = np.asarray(inputs["gdn_norm_w"][0], np.float32)
    gd[:, 257:289] = np.asarray(inputs["gdn_a_log"][0], np.float32)[None, :]
    gd[:, 289:321] = np.asarray(inputs["gdn_dt_bias"][0], np.float32)[None, :]
    m["gdnv"] = gd
    cwv = np.asarray(inputs["rglru_conv_w"], np.float32)
    parts = [cwv[:, 0], cwv[:, 1], cwv[:, 2], cwv[:, 3], np.asarray(inputs["rglru_conv_b"], np.float32),
             np.asarray(inputs["rglru_b_rgate"], np.float32).reshape(2, RW), np.asarray(inputs["rglru_b_igate"], np.float32).reshape(2, RW),
             np.asarray(inputs["rglru_lambda"], np.float32)]
    rv = np.stack(parts, axis=1)
    m["rvec"] = _vecP(rv, 80)
    return m


def kernel(**inputs):
    layers = tuple(inputs.pop("_layers", range(DEPTH)))
    n_pass = int(inputs.pop("_n_pass", SEQ // TP))
    ncores = int(inputs.pop("_cores", 8))
    key = (layers, n_pass)
    if key not in _CACHE:
        mk = MK(layers, n_pass)
        _CACHE[key] = (mk.build(), set(mk.din.keys()))
    nc, names = _CACHE[key]
    full = [make_inputs(inputs, b) for b in range(min(4, ncores))]
    in_maps = [{k: v for k, v in full[i % len(full)].items() if k in names} for i in range(ncores)]
    res = run_bass_kernel_spmd(nc, in_maps, core_ids=list(range(ncores)))
    nb = min(4, ncores)
    out = np.stack([np.ascontiguousarray(res.results[b]["yT"].T) for b in range(nb)], axis=0)
    return out.astype(np.float32)
```

```python
import numpy as np
import concourse.bass as bass
import concourse.mybir as mybir
from concourse.bass_utils import run_bass_kernel_spmd

F32 = mybir.dt.float32
BF16 = mybir.dt.bfloat16
ALU = mybir.AluOpType
AF = mybir.ActivationFunctionType

ENGS = ("pe", "act", "dve", "pool", "sp")

D = 2048
KD = 16
SEQ = 2048
TP = 512
DEPTH = 4
ALPHA = float((2 * DEPTH) ** 0.25)
DFF = 8192
RW = 2560
RCH = 32
NWB = 8
KG = 4


class Prog:
    def __init__(self, nc):
        self.nc = nc
        self.q = {e: [] for e in ENGS}
        self.cnt = {e: 0 for e in ENGS}
        self.sem = {e: nc.alloc_semaphore(name=f"c_{e}") for e in ENGS if e != "sp"}
        self.last_w = {}
        self.readers = {}
        self.seen = {e: {} for e in ENGS}
        self.slots = {}

    def _deps(self, eng, reads, writes):
        deps = []
        for k in reads:
            w = self.last_w.get(k)
            if w is not None:
                deps.append(w)
            if isinstance(k, tuple) and k[0] == "ps":
                deps.extend(t for t in self.readers.get(k, ()) if t[0] != eng)
        for k in writes:
            w = self.last_w.get(k)
            if w is not None:
                deps.append(w)
            deps.extend(self.readers.get(k, ()))
        waits = {}
        for (src, val) in deps:
            if src == "pe" and eng == "pe":
                continue
            if val > waits.get(src, 0):
                waits[src] = val
        out = []
        for src, val in waits.items():
            if self.seen[eng].get(src, 0) >= val:
                continue
            self.seen[eng][src] = val
            out.append((src, val))
        return out

    def _commit(self, tok, reads, writes):
        for k in writes:
            self.last_w[k] = tok
            self.readers[k] = []
        for k in reads:
            self.readers.setdefault(k, []).append(tok)

    def op(self, eng, fn, reads=(), writes=()):
        waits = self._deps(eng, reads, writes)
        self.cnt[eng] += 1
        tok = (eng, self.cnt[eng])
        self.q[eng].append((waits, fn, eng))
        self._commit(tok, reads, writes)

    def dma(self, eng, slot, fn, reads=(), writes=()):
        if slot not in self.slots:
            self.slots[slot] = [self.nc.alloc_semaphore(name=f"d_{slot}"), 0]
        waits = self._deps(eng, reads, writes)
        s = self.slots[slot]
        s[1] += 16
        tok = (("slot", slot), s[1])
        self.q[eng].append((waits, fn, ("slot", slot)))
        self._commit(tok, reads, writes)

    def wait_all(self, eng, keys):
        waits = self._deps(eng, keys, ())
        self.q[eng].append((waits, None, None))

    def _semof(self, src):
        if isinstance(src, tuple):
            return self.slots[src[1]][0]
        return self.sem[src]

    def emit(self):
        nc = self.nc
        prog = self
        emap = {"pe": "tensor", "act": "scalar", "dve": "vector", "pool": "gpsimd", "sp": "sync"}
        with nc.Block() as block:
            for e in ENGS:
                def body(engobj, e=e):
                    for waits, fn, kind in prog.q[e]:
                        for src, val in waits:
                            engobj.wait_ge(prog._semof(src), val)
                        if fn is None:
                            continue
                        ins = fn(engobj)
                        if isinstance(kind, tuple):
                            ins.then_inc(prog.slots[kind[1]][0], 16)
                        else:
                            ins.then_inc(prog.sem[e], 1)
                getattr(block, emap[e])(body)


class MK:
    def __init__(self, layers=tuple(range(DEPTH)), n_pass=SEQ // TP):
        self.layers = tuple(layers)
        self.n_layers = len(self.layers)
        self.n_pass = n_pass
        nc = self.nc = bass.Bass("TRN2", target_bir_lowering=False)
        self.P = Prog(nc)
        self.din = {}
        self.wi = 0
        self.pi = 0
        self.sqi = 0

    def inp(self, name, shape):
        if name not in self.din:
            self.din[name] = self.nc.dram_tensor(name, list(shape), F32, kind="ExternalInput").ap()
        return self.din[name]

    def T(self, name, shape, dt=F32):
        return self.nc.alloc_sbuf_tensor(name, list(shape), dt)

    def ACT(self, out, in_, func, r, w, scale=None, bias=None):
        kw = {}
        if scale is not None:
            kw["scale"] = scale
        if bias is not None:
            kw["bias"] = bias
        self.P.op("act", lambda e: e.activation(out=out, in_=in_, func=func, **kw), reads=r, writes=w)

    def TS(self, out, in0, s1, s2, op0, op1, r, w, eng="dve"):
        if op1 is None:
            self.P.op(eng, lambda e: e.tensor_scalar(out=out, in0=in0, scalar1=s1, scalar2=None, op0=op0), reads=r, writes=w)
        else:
            self.P.op(eng, lambda e: e.tensor_scalar(out=out, in0=in0, scalar1=s1, scalar2=s2, op0=op0, op1=op1), reads=r, writes=w)

    def TT(self, out, in0, in1, op, r, w, eng="dve"):
        self.P.op(eng, lambda e: e.tensor_tensor(out=out, in0=in0, in1=in1, op=op), reads=r, writes=w)

    def STT(self, out, in0, scalar, in1, op0, op1, r, w):
        self.P.op("dve", lambda e: e.scalar_tensor_tensor(out=out, in0=in0, scalar=scalar, in1=in1, op0=op0, op1=op1), reads=r, writes=w)

    def CP(self, eng, out, in_, r, w):
        if eng == "act":
            self.P.op("act", lambda e: e.copy(out=out, in_=in_), reads=r, writes=w)
        else:
            self.P.op(eng, lambda e: e.tensor_copy(out=out, in_=in_), reads=r, writes=w)

    def MM(self, out, lhsT, rhs, start, stop, r, w):
        self.P.op("pe", lambda e: e.matmul(out, lhsT=lhsT, rhs=rhs, start=start, stop=stop), reads=r, writes=w)

    def DMA(self, eng, slot, out, in_, r, w):
        if slot in ("c0", "c1"):
            self.cslot = getattr(self, "cslot", 0) + 1
            slot = f"{slot}_{self.cslot}"
        self.P.dma(eng, slot, lambda e: e.dma_start(out=out, in_=in_), reads=r, writes=w)

    def psum(self):
        i = self.pi % 8
        self.pi += 1
        return self.ps[i], ("ps", i)

    def sq(self):
        i = self.sqi % 2
        self.sqi += 1
        return self.sqb[i], ("sq", i)

    def wtile(self, src, kp, kc, ncols):
        i = self.wi % NWB
        self.wi += 1
        t = self.wb[i]
        key = ("wb", i)
        self.DMA("pool", f"wb{i}", t[:kp, :kc, :ncols], src, [], [key])
        return t, key

    def wfull(self, Wc, ncols):
        tl = []
        for hf in range(16 // KG):
            tl.append(self.wtile(Wc[hf * KG * 128:(hf + 1) * KG * 128, :].rearrange("(k p) n -> p k n", p=128), 128, KG, ncols))
        return lambda k: (tl[k // KG][0][:, k % KG, 0:ncols], tl[k // KG][1])

    def linear(self, W, kp, KC, col0, ncols, ocw, act, on_block, blk0=0, cpt=4):
        tcols = ocw * cpt
        assert ncols % tcols == 0, (ncols, tcols)
        nkg = (KC + KG - 1) // KG
        for og in range(ncols // tcols):
            banks = [self.psum() for _ in range(cpt)]
            for kg in range(nkg):
                kc = min(KG, KC - kg * KG)
                c0 = col0 + og * tcols
                src = W[kg * KG * kp:(kg * KG + kc) * kp, c0:c0 + tcols].rearrange("(k p) n -> p k n", p=kp)
                t, wkey = self.wtile(src, kp, kc, tcols)
                for j in range(cpt):
                    pt, pk = banks[j]
                    for k in range(kc):
                        a, akey = act(kg * KG + k)
                        self.MM(pt[:ocw, :], t[:kp, k, j * ocw:(j + 1) * ocw], a,
                                (kg == 0 and k == 0), (kg == nkg - 1 and k == kc - 1), [wkey, akey], [pk])
            for j in range(cpt):
                on_block(blk0 + og * cpt + j, banks[j][0], banks[j][1])

    def build(self):
        nc, P = self.nc, self.P
        L = self.n_layers
        xT = self.inp("xT", [D, SEQ])
        cT = self.inp("cT", [128, KD])
        lng = self.inp("lng", [128, DEPTH * 2 * KD])
        lnb = self.inp("lnb", [128, DEPTH * 2 * KD])
        bmod = self.inp("bmod", [128, DEPTH * 96])
        w_mod = self.inp("w_mod", [DEPTH, D, 6 * D])
        w_ff1 = self.inp("w_ff1", [DEPTH, D, DFF])
        w_ff2 = self.inp("w_ff2", [DEPTH, DFF, D])
        if any(l % 3 == 0 for l in self.layers):
            self.r_w_in = self.inp("rglru_w_in", [2, D, 2 * RW])
            self.r_w_out = self.inp("rglru_w_out", [2, RW, D])
            self.r_wrg = self.inp("rglru_w_rgate", [2, 16, 160, 160])
            self.r_wig = self.inp("rglru_w_igate", [2, 16, 160, 160])
        rvec = self.inp("rvec", [80, 2 * 8 * RCH])
        self.yT = nc.dram_tensor("yT", [D, SEQ], F32, kind="ExternalOutput").ap()

        self.xres = self.T("xres", [128, KD, TP])
        self.h = self.T("h", [128, KD, TP], BF16)
        self.U = self.T("U", [128, 64, TP], BF16)
        self.wb = [self.T(f"wb{i}", [128, KG, 512], BF16) for i in range(NWB)]
        self.ps = [nc.alloc_psum_tensor(f"ps{i}", [128, 512], F32) for i in range(8)]
        self.sqb = [self.T(f"sq{i}", [128, TP]) for i in range(2)]
        self.ones = self.T("ones", [128, 128])
        self.mean = self.T("mean", [128, TP])
        self.rstd = self.T("rstd", [128, TP])
        self.tmpv = self.T("tmpv", [128, TP])
        self.cact = self.T("cact", [128, KD], BF16)
        self.cf = self.T("cf", [128, KD])
        self.mod = self.T("mod", [128, DEPTH, 96])
        self.bm = self.T("bm", [128, DEPTH * 96])
        self.g_sb = self.T("g_sb", [128, DEPTH * 2 * KD])
        self.b_sb = self.T("b_sb", [128, DEPTH * 2 * KD])
        self.SC = self.T("SC", [128, 7200])
        self.rv = self.T("rv", [80, 2, 8, RCH])
        self.cneg = self.T("cneg", [80, 2, 2, RCH])
        self.hstate = self.T("hstate", [80, 2, RCH])
        self.tails = self.T("tails", [80, 2, RCH, 3])
        self.gw = [self.T(f"gw{i}", [80, 2, 2, 160], BF16) for i in range(2)]
        self.gwi = 0
        self.xrb = self.T("xrb", [80, 2, TP], BF16)

        P.op("dve", lambda e: e.memset(self.ones[:], 1.0), writes=["ones"])
        P.op("dve", lambda e: e.memset(self.hstate[:], 0.0), writes=["hstate"])
        P.op("dve", lambda e: e.memset(self.tails[:], 0.0), writes=["tails"])
        self.DMA("sp", "c0", self.cf[:], cT, [], ["cf"])
        self.DMA("sp", "c0", self.bm[:], bmod, [], ["bm"])
        self.DMA("sp", "c0", self.g_sb[:], lng, [], ["g_sb"])
        self.DMA("sp", "c0", self.b_sb[:], lnb, [], ["b_sb"])
        self.DMA("sp", "c0", self.rv[:].rearrange("p a b c -> p (a b c)"), rvec, [], ["rv"])
        self.ACT(self.cact[:], self.cf[:], AF.Silu, ["cf"], ["cact"])
        for s in range(2):
            self.ACT(self.cneg[:, s, 0, :], self.rv[:, s, 7, :], AF.Exp, ["rv"], ["cneg"], scale=-1.0)
            self.ACT(self.cneg[:, s, 0, :], self.cneg[:, s, 0, :], AF.Ln, ["cneg"], ["cneg"], bias=1.0)
            self.TS(self.cneg[:, s, 1, :], self.cneg[:, s, 0, :], -16.0, None, ALU.mult, None, ["cneg"], ["cneg"])
            self.TS(self.cneg[:, s, 0, :], self.cneg[:, s, 0, :], -8.0, None, ALU.mult, None, ["cneg"], ["cneg"])

        for l in self.layers:
            pm, pmk = self.psum()

            def act_c(k):
                return self.cact[:, k:k + 1], "cact"

            for og in range(24):
                getw = self.wfull(w_mod[l][:, og * 512:(og + 1) * 512], 512)
                for j in range(4):
                    col = og * 4 + j
                    for k in range(16):
                        t, wkey = getw(k)
                        self.MM(pm[:, col:col + 1], t[:, j * 128:(j + 1) * 128], self.cact[:, k:k + 1], k == 0, k == 15, [wkey, "cact"], [pmk])
            self.TT(self.mod[:, l, :], pm[:, 0:96], self.bm[:, l * 96:(l + 1) * 96], ALU.add, [pmk, "bm"], ["mod"])
            for a in (16, 32, 64, 80):
                self.TS(self.mod[:, l, a:a + 16], self.mod[:, l, a:a + 16], 1.0, None, ALU.add, None, ["mod"], ["mod"])

        for p in range(self.n_pass):
            t0 = p * TP
            self.DMA("sp", "xin", self.xres[:], xT[:, t0:t0 + TP].rearrange("(k p) t -> p k t", p=128),
                     [], [("x", k) for k in range(KD)])
            for l in self.layers:
                kind, slot = l % 3, l // 3
                self.sublayer(l, 0, kind, slot, p)
                self.sublayer(l, 1, 3, l, p)
            self.DMA("sp", "xout", self.yT[:, t0:t0 + TP].rearrange("(k p) t -> p k t", p=128), self.xres[:],
                     [("x", k) for k in range(KD)], ["yT"])
        P.wait_all("sp", ["yT"])
        P.emit()
        return nc

    def sublayer(self, l, which, kind, slot, p):
        mb = 48 * which
        sh = lambda k: self.mod[:, l, mb + k:mb + k + 1]
        sc1 = lambda k: self.mod[:, l, mb + 16 + k:mb + 17 + k]
        gt1 = lambda k: self.mod[:, l, mb + 32 + k:mb + 33 + k]
        for k in range(KD):
            self.ACT(self.h[:, k, :], self.xres[:, k, :], AF.Identity, [("x", k), "mod"], [("h", k)], scale=sc1(k), bias=sh(k))
        for k in range(KD):
            self.TS(self.xres[:, k, :], self.xres[:, k, :], ALPHA, None, ALU.mult, None, [("x", k), "stg"], [("x", k)])

        def resid(n, pt, pk):
            self.STT(self.xres[:, n, :], pt[:, :], gt1(n), self.xres[:, n, :], ALU.mult, ALU.add, [pk, ("x", n), "mod"], [("x", n)])

        import os as _os2
        if kind == 3:
            if not _os2.environ.get("MK_SKIP_FFN"):
                self.ffn(l, resid)
        elif kind == 0:
            self.rglru(slot, resid, p)
        elif kind == 2:
            self.gla(resid, p)
        else:
            self.gdn(resid, p)
        if not _os2.environ.get("MK_SKIP_LN"):
            self.layernorm(l, which)

    def hact(self, k):
        return self.h[:, k, :], ("h", k)

    def ffn(self, l, resid):
        w1 = self.din["w_ff1"][l]
        w2 = self.din["w_ff2"][l]

        def relu2(n, pt, pk):
            s, sk = self.sq()
            self.ACT(s[:], pt[:, :], AF.Square, [pk], [sk])
            self.STT(self.U[:, n, :], pt[:, :], 0.0, s[:], ALU.is_gt, ALU.mult, [pk, sk], [("U", n)])

        self.linear(w1, 128, KD, 0, DFF, 128, self.hact, relu2)
        self.linear(w2, 128, 64, 0, D, 128, lambda k: (self.U[:, k, :], ("U", k)), resid)

    def layernorm(self, l, which):
        s1, s1k = self.psum()
        s2, s2k = self.psum()
        for k in range(KD):
            s, sk = self.sq()
            self.ACT(s[:], self.xres[:, k, :], AF.Square, [("x", k)], [sk])
            self.MM(s1[:, :], self.ones[:], self.xres[:, k, :], k == 0, k == KD - 1, ["ones", ("x", k)], [s1k])
            self.MM(s2[:, :], self.ones[:], s[:], k == 0, k == KD - 1, ["ones", sk], [s2k])
        self.TS(self.mean[:], s1[:, :], 1.0 / D, None, ALU.mult, None, [s1k], ["mean"])
        self.TT(self.tmpv[:], self.mean[:], self.mean[:], ALU.mult, ["mean"], ["tmpv"])
        self.STT(self.tmpv[:], s2[:, :], 1.0 / D, self.tmpv[:], ALU.mult, ALU.subtract, [s2k, "tmpv"], ["tmpv"])
        self.TS(self.tmpv[:], self.tmpv[:], 1e-5, None, ALU.add, None, ["tmpv"], ["tmpv"])
        self.ACT(self.tmpv[:], self.tmpv[:], AF.Sqrt, ["tmpv"], ["tmpv"])
        self.P.op("dve", lambda e: e.reciprocal(out=self.rstd[:], in_=self.tmpv[:]), reads=["tmpv"], writes=["rstd"])
        gi = (l * 2 + which) * KD
        for k in range(KD):
            self.TT(self.xres[:, k, :], self.xres[:, k, :], self.mean[:], ALU.subtract, [("x", k), "mean"], [("x", k)])
            self.TT(self.xres[:, k, :], self.xres[:, k, :], self.rstd[:], ALU.mult, [("x", k), "rstd"], [("x", k)])
            self.ACT(self.xres[:, k, :], self.xres[:, k, :], AF.Identity, [("x", k), "g_sb", "b_sb"], [("x", k)],
                     scale=self.g_sb[:, gi + k:gi + k + 1], bias=self.b_sb[:, gi + k:gi + k + 1])

    def rglru(self, s, resid, p):
        w_in = self.r_w_in[s]
        w_out = self.r_w_out[s]
        U = self.U
        SC = self.SC
        def sc(i, w=2 * TP):
            return SC[:80, i * 1024:i * 1024 + w]
        recb = SC[:80, 6144:6144 + 2 * 515].rearrange("p (i t) -> p i t", i=2)
        xs = sc(0).rearrange("p (i t) -> p i t", i=2)
        xr = sc(1).rearrange("p (i t) -> p i t", i=2)
        rg = sc(2).rearrange("p (i t) -> p i t", i=2)
        ig = sc(3).rearrange("p (i t) -> p i t", i=2)
        aa = sc(4).rearrange("p (i t) -> p i t", i=2)
        bb = sc(5).rearrange("p (i t) -> p i t", i=2)
        xrb = self.xrb
        cw = lambda j, c: self.rv[:, s, j, c:c + 1]

        def gate_blk(c, pt, pk):
            x = xs[:, 0, :]
            self.ACT(x, pt[:80, :], AF.Copy, [pk], ["xs"])
            self.ACT(xr[:, 0, :], pt[:80, :], AF.Square, [pk], ["xr"])
            self.TS(xr[:, 0, :], xr[:, 0, :], 0.044715, 1.0, ALU.mult, ALU.add, ["xr"], ["xr"])
            self.TT(xr[:, 0, :], xr[:, 0, :], x, ALU.mult, ["xr", "xs"], ["xr"])
            self.ACT(xr[:, 0, :], xr[:, 0, :], AF.Sigmoid, ["xr"], ["xr"], scale=1.5957691216057308)
            self.TT(U[:80, c, :], xr[:, 0, :], x, ALU.mult, ["xr", "xs"], [("U", c)])

        self.linear(w_in, 128, KD, 0, RW, 80, self.hact, gate_blk)

        def rec_blk(c, pt, pk):
            i = c % 2
            n = c // 2
            self.ACT(recb[:, i, 3:515], pt[:80, :], AF.Copy, [pk], ["recb"])
            self.CP("dve", recb[:, i, 0:3], self.tails[:, s, c, :], ["tails"], ["recb"])
            self.CP("dve", self.tails[:, s, c, :], recb[:, i, 512:515], ["recb"], ["tails"])
            self.TS(xr[:, i, :], recb[:, i, 0:512], cw(0, c), cw(4, c), ALU.mult, ALU.add, ["recb", "rv"], ["xr"])
            for j in (1, 2, 3):
                self.STT(xr[:, i, :], recb[:, i, j:j + 512], cw(j, c), xr[:, i, :], ALU.mult, ALU.add, ["recb", "rv", "xr"], ["xr"])
            if i == 0:
                return
            self.CP("dve", xrb[:, :, :], xr[:, :, :], ["xr"], ["xrb"])
            gwt = self.gw[self.gwi % 2]
            gk = ("gw", self.gwi % 2)
            self.gwi += 1
            self.DMA("pool", f"gw{gk[1]}a", gwt[:, 0, :, :], self.r_wrg[s, n].rearrange("(i p) e -> p i e", p=80), [], [gk])
            self.DMA("pool", f"gw{gk[1]}b", gwt[:, 1, :, :], self.r_wig[s, n].rearrange("(i p) e -> p i e", p=80), [], [gk])
            for g, dst, bj in ((0, rg, 5), (1, ig, 6)):
                for j in range(2):
                    gp, gpk = self.psum()
                    for ii in range(2):
                        self.MM(gp[:80, :], gwt[:, g, ii, j * 80:(j + 1) * 80], xrb[:, ii, :], ii == 0, ii == 1, [gk, "xrb"], [gpk])
                    cc = 2 * n + j
                    self.ACT(dst[:, j, :], gp[:80, :], AF.Sigmoid, [gpk, "rv"], ["rg" if g == 0 else "ig"], bias=self.rv[:, s, bj, cc:cc + 1])
            for j in range(2):
                cc = 2 * n + j
                self.ACT(aa[:, j, :], rg[:, j, :], AF.Exp, ["rg", "cneg"], ["aa"], scale=self.cneg[:, s, 0, cc:cc + 1])
                self.ACT(bb[:, j, :], rg[:, j, :], AF.Exp, ["rg", "cneg"], ["bb"], scale=self.cneg[:, s, 1, cc:cc + 1])
            self.TS(bb[:, :, :], bb[:, :, :], -1.0, 1.0, ALU.mult, ALU.add, ["bb"], ["bb"])
            self.TS(bb[:, :, :], bb[:, :, :], 0.0, None, ALU.max, None, ["bb"], ["bb"])
            self.ACT(bb[:, :, :], bb[:, :, :], AF.Sqrt, ["bb"], ["bb"])
            self.TT(bb[:, :, :], bb[:, :, :], ig[:, :, :], ALU.mult, ["bb", "ig"], ["bb"])
            self.TT(bb[:, :, :], bb[:, :, :], xr[:, :, :], ALU.mult, ["bb", "xr"], ["bb"])
            for j in range(2):
                cc = 2 * n + j
                self.P.op("dve", lambda e, j=j, cc=cc: e.tensor_tensor_scan(out=rg[:, j, :], data0=aa[:, j, :], data1=bb[:, j, :],
                                                                            initial=self.hstate[:, s, cc:cc + 1], op0=ALU.mult, op1=ALU.add),
                          reads=["aa", "bb", "hstate"], writes=["rg"])
                self.CP("dve", self.hstate[:, s, cc:cc + 1], rg[:, j, TP - 1:TP], ["rg"], ["hstate"])
            self.TT(U[:80, 2 * n:2 * n + 2, :], U[:80, 2 * n:2 * n + 2, :], rg[:, :, :], ALU.mult,
                    [("U", 2 * n), ("U", 2 * n + 1), "rg"], [("U", 2 * n), ("U", 2 * n + 1)])

        self.linear(w_in, 128, KD, RW, RW, 80, self.hact, rec_blk)
        self.linear(w_out, 80, RCH, 0, D, 128, lambda k: (U[:80, k, :], ("U", k)), resid)


    def mix_setup(self):
        if hasattr(self, "UF"):
            return
        nc = self.nc
        self.UB = self.U[:].rearrange("p a b -> p (a b)")
        self.UF = self.UB.bitcast(F32)
        self.SCB = self.SC[:].bitcast(BF16)
        self.ST = self.SC[:, 0:4096]
        cst = self.inp("cst", [128, 1024])
        self.cst = self.T("cst_sb", [128, 1024])
        self.identb = self.T("identb", [128, 128], BF16)
        self.DMA("sp", "c1", self.cst[:], cst, [], ["cst"])
        self.CP("dve", self.identb[:], self.cst[:, 0:128], ["cst"], ["identb"])
        self.cstb = self.T("cstb", [128, 1024], BF16)
        self.CP("dve", self.cstb[:], self.cst[:], ["cst"], ["cst"])
        self.identf = self.cst[:, 0:128]
        self.tri_ge = self.cst[:, 128:192]
        self.tri_gt = self.cst[:, 192:256]
        self.ntri_gt = self.cst[:, 896:960]
        self.cmask = self.cst[:, 256:768]
        self.ones64 = self.cst[:, 768:896]

    def ufs(self, off, n):
        return self.UF[:, off:off + n]

    def ubs(self, off, n):
        return self.UB[:, 2 * off:2 * off + n]

    def gla(self, resid, p):
        self.mix_setup()
        nc = self.nc
        Win = self.inp("gla_w_in", [1, D, 6160])[0]
        Wout = self.inp("gla_w_out", [1, D, D])[0]
        if not hasattr(self, "wup"):
            wupd = self.inp("gla_w_alpha_up", [1, 16, 1024])[0]
            glav = self.inp("glav", [128, 12])
            self.wup = self.T("wup_sb", [16, 1024], BF16)
            self.glv = self.T("glv_sb", [128, 12])
            self.st_gla = nc.dram_tensor("st_gla", [128, 4096], F32).ap()
            self.DMA("pool", "wupd", self.wup[:], wupd, [], ["wup"])
            self.DMA("sp", "c1", self.glv[:], glav, [], ["glv"])
            self.TS(self.glv[:, 0:8], self.glv[:, 0:8], -1.0, None, ALU.mult, None, ["glv"], ["glv"])
        nba = lambda z: self.glv[:, z:z + 1]
        nw = lambda e: self.glv[:, 8 + e:9 + e]
        ST = self.ST
        if p == 0:
            self.P.op("dve", lambda e: e.memset(ST, 0.0), writes=["ST"])
        else:
            self.DMA("sp", "stl_gla", ST, self.st_gla, ["st_gla", ("x", 0)], ["ST"])
        v3 = lambda ap_: ap_.rearrange("p (d t) -> p d t", d=2)
        bc = v3(self.ufs(4096, 1024)); Ep = v3(self.ufs(5120, 1024)); Em = v3(self.ufs(6144, 1024))
        qd = v3(self.ubs(7168, 1024)); ki = v3(self.ubs(7680, 1024)); kt = v3(self.ubs(8192, 1024))
        ktok = self.ubs(8704, 2048)[:64].rearrange("p (c d e) -> p c d e", c=8, d=2)
        vtok = self.ubs(9728, 4096)[:64].rearrange("p (c e) -> p c e", c=8)
        sg = self.ufs(11776, 2048).rearrange("p (a t) -> p a t", a=4)
        t1 = self.ufs(14336, 512); rinv = self.ufs(14848, 512)
        Sbf = v3(self.ubs(15360, 1024))
        AT = self.ubs(15872, 64)[:64]
        alow = self.ubs(15904, 512)[:16]
        Eend = self.ufs(16160, 16).rearrange("p (d c) -> p d c", d=2)

        def alow_blk(n, pt, pk):
            self.CP("act", alow, pt[:16, :], [pk], ["alow"])
        self.linear(Win, 128, KD, 6144, 16, 16, self.hact, alow_blk, cpt=1)
        for hd in range(4):
            Sv = v3(ST[:, hd * 1024:(hd + 1) * 1024])
            for dc in range(2):
                zc = hd * 2 + dc
                zp, zk = self.psum()
                self.MM(zp[:, :], self.wup[:16, zc * 128:(zc + 1) * 128], alow, True, True, ["wup", "alow"], [zk])
                self.ACT(Ep[:, dc, :], zp[:, :], AF.Exp, [zk, "glv"], ["Ep"], scale=-1.0, bias=nba(zc))
                self.ACT(Ep[:, dc, :], Ep[:, dc, :], AF.Ln, ["Ep"], ["Ep"], bias=1.0)
                self.TS(Ep[:, dc, :], Ep[:, dc, :], -1.0 / 16.0, None, ALU.mult, None, ["Ep"], ["Ep"])
                self.P.op("dve", lambda e, dc=dc: e.tensor_tensor_scan(out=bc[:, dc, :], data0=self.cmask, data1=Ep[:, dc, :], initial=0.0,
                                                                       op0=ALU.mult, op1=ALU.add), reads=["Ep", "cst"], writes=["bc"])
            self.ACT(Eend[:, :, :], bc.rearrange("p d (c t) -> p d c t", t=64)[:, :, :, 63], AF.Exp, ["bc"], ["Eend"])
            self.ACT(Ep[:, :, :], bc[:, :, :], AF.Exp, ["bc", "Ep"], ["Ep"])
            self.ACT(Em[:, :, :], bc[:, :, :], AF.Exp, ["bc"], ["Em"], scale=-1.0)

            def q_blk(n, pt, pk):
                self.STT(qd[:, n, :], pt[:, :], 1.0 / 16.0, Ep[:, n, :], ALU.mult, ALU.mult, [pk, "Ep"], ["qd"])
            self.linear(Win, 128, KD, hd * 256, 256, 128, self.hact, q_blk, cpt=2)

            def k_blk(n, pt, pk):
                self.TT(ki[:, n, :], pt[:, :], Em[:, n, :], ALU.mult, [pk, "Em"], ["ki"])
                self.TT(kt[:, n, :].rearrange("p (c t) -> p c t", t=64), ki[:, n, :].rearrange("p (c t) -> p c t", t=64),
                        Eend[:, n, :, None].broadcast_to([128, 8, 64]), ALU.mult, ["ki", "Eend"], ["kt"])
            self.linear(Win, 128, KD, 1024 + hd * 256, 256, 128, self.hact, k_blk, cpt=2)
            for c in range(8):
                for dc in range(2):
                    tp, tk = self.psum()
                    self.MM(tp[:64, 0:128], kt[:, dc, c * 64:(c + 1) * 64], self.identb[:, :], True, True, ["kt", "identb"], [tk])
                    self.CP("act", ktok[:, c, dc, :], tp[:64, 0:128], [tk], ["ktok"])
            getw = self.wfull(Win[:, 2048 + hd * 512:2048 + (hd + 1) * 512], 512)
            for c in range(8):
                vp, vk = self.psum()
                for k in range(KD):
                    wt, wk = getw(k)
                    self.MM(vp[:64, :], self.h[:, k, c * 64:(c + 1) * 64], wt, k == 0, k == KD - 1, [wk, ("h", k)], [vk])
                self.CP("dve", vtok[:, c, :], vp[:64, :], [vk], ["vtok"])

            def g_blk(n, pt, pk):
                self.ACT(sg[:, n, :], pt[:, :], AF.Silu, [pk], ["sg"])
            self.linear(Win, 128, KD, 4096 + hd * 512, 512, 128, self.hact, g_blk, cpt=4)
            self.CP("dve", Sbf[:, :, :], Sv[:, :, :], ["ST"], ["Sbf"])
            ob = [(self.ps[i], ("ps", i)) for i in range(4)]
            for c in range(8):
                cs = slice(c * 64, (c + 1) * 64)
                a_p, a_k = self.ps[6], ("ps", 6)
                for dc in range(2):
                    self.MM(a_p[:64, 0:64], ki[:, dc, cs], qd[:, dc, cs], dc == 0, dc == 1, ["ki", "qd"], [a_k])
                self.TT(AT, a_p[:64, 0:64], self.tri_ge[:64, :], ALU.mult, [a_k, "cst"], ["AT"])
                for ec in range(4):
                    op_, ok_ = ob[ec]
                    for dc in range(2):
                        self.MM(op_[:, cs], Sbf[:, dc, ec * 128:(ec + 1) * 128], qd[:, dc, cs], dc == 0, False, ["Sbf", "qd"], [ok_])
                    self.MM(op_[:, cs], vtok[:, c, ec * 128:(ec + 1) * 128], AT, False, True, ["vtok", "AT"], [ok_])
                for dc in range(2):
                    s_p, s_k = self.ps[4 + dc], ("ps", 4 + dc)
                    self.MM(s_p[:, :], ktok[:, c, dc, :], vtok[:, c, :], True, True, ["ktok", "vtok"], [s_k])
                    self.STT(Sv[:, dc, :], Sv[:, dc, :], Eend[:, dc, c:c + 1], s_p[:, :], ALU.mult, ALU.add, ["ST", "Eend", s_k], ["ST"])
                self.CP("act", Sbf[:, :, :], Sv[:, :, :], ["ST"], ["Sbf"])
            r_p, r_k = self.ps[7], ("ps", 7)
            for ec in range(4):
                s_, sk_ = self.sq()
                self.ACT(s_[:], ob[ec][0][:, :], AF.Square, [ob[ec][1]], [sk_])
                self.MM(r_p[:, :], self.ones[:], s_[:], ec == 0, ec == 3, ["ones", sk_], [r_k])
            self.TS(rinv, r_p[:, :], 1.0 / 512.0, 1e-6, ALU.mult, ALU.add, [r_k], ["rinv"])
            self.ACT(rinv, rinv, AF.Sqrt, ["rinv"], ["rinv"])
            self.P.op("dve", lambda e: e.reciprocal(out=rinv, in_=rinv), reads=["rinv"], writes=["rinv"])
            for ec in range(4):
                self.TT(t1, ob[ec][0][:, :], rinv, ALU.mult, [ob[ec][1], "rinv"], ["t1"])
                self.STT(self.U[:, hd * 4 + ec, :], sg[:, ec, :], nw(ec), t1, ALU.mult, ALU.mult, ["sg", "glv", "t1"], [("U", hd * 4 + ec)])
        self.DMA("sp", "sts_gla", self.st_gla, ST, ["ST"], ["st_gla", "stg"])
        self.linear(Wout, 128, KD, 0, D, 128, lambda k: (self.U[:, k, :], ("U", k)), resid)

    def gdn(self, resid, p):
        self.mix_setup()
        nc = self.nc
        Win = self.inp("gdn_w_in", [1, D, 12352])[0]
        Wout = self.inp("gdn_w_out", [1, 4096, D])[0]
        if not hasattr(self, "gdv"):
            gdnv = self.inp("gdnv", [128, 4 * 64 + 1 + 64])
            self.gdv = self.T("gdv_sb", [128, 4 * 64 + 1 + 64])
            self.gtail = self.T("gtail", [128, 64, 3])
            self.st_gdn = nc.dram_tensor("st_gdn", [128, 4096], F32).ap()
            self.DMA("sp", "c1", self.gdv[:], gdnv, [], ["gdv"])
            self.P.op("dve", lambda e: e.memset(self.gtail[:], 0.0), writes=["gtail"])
            self.ACT(self.gdv[:64, 257:289], self.gdv[:64, 257:289], AF.Exp, ["gdv"], ["gdv"])
            self.TS(self.gdv[:64, 257:289], self.gdv[:64, 257:289], -1.0, None, ALU.mult, None, ["gdv"], ["gdv"])
        cw = lambda j, ch: self.gdv[:, j * 64 + ch:j * 64 + ch + 1]
        normw = self.gdv[:, 256:257]
        negA = self.gdv[:64, 257:289]
        dtb = self.gdv[:64, 289:321]
        ST = self.ST
        if p == 0:
            self.P.op("dve", lambda e: e.memset(ST, 0.0), writes=[("ST", h_) for h_ in range(32)])
        else:
            self.DMA("sp", "stl_gdn", ST, self.st_gdn, ["st_gdn", ("x", 0)], [("ST", h_) for h_ in range(32)])
        o = [8192]

        def F(n, parts=128):
            a = self.ufs(o[0], n)[:parts]
            o[0] += (n + 7) // 8 * 8
            return a

        def B(n, parts=128):
            a = self.ubs(o[0], n)[:parts]
            o[0] += ((n + 1) // 2 + 7) // 8 * 8
            return a
        r8 = lambda ap_: ap_.rearrange("p (c x) -> p c x", c=8)
        ba = r8(F(512, 64))
        beta = r8(F(256, 64))
        gg = r8(self.SC[:64, 6656:6912])
        gc = r8(F(256, 64))
        bg = r8(F(256, 64))
        tailf = r8(F(256, 64))
        dend = r8(self.SC[:, 6912:7168])
        cbuf = F(515)
        qn = B(512); kn = B(512); vT = B(512)
        acc = F(512); rn = F(512); t1 = F(512)
        Gs = r8(F(512, 64))
        QK = r8(F(512, 64))
        ktk = r8(self.SC[:64, 4096:5120])
        sets = []
        for si in range(2):
            S_ = {}
            S_["vbt"] = r8(self.SC[:64, 5120 + 512 * si:5632 + 512 * si].bitcast(BF16))
            S_["szT"] = B(512)
            for nm in ("dg", "tmpa", "Dm", "DT", "dgt"):
                S_[nm] = F(64, 64)
            for nm in ("dgh", "dgl", "Na", "Nb", "Ma", "Mb", "Uu", "Aq"):
                S_[nm] = B(64, 64)
            S_["kb"] = B(128, 64); S_["usb"] = F(128, 64); S_["wT"] = B(64); S_["vn"] = B(128, 64)
            S_["qdc"] = B(64); S_["ktt"] = B(128, 64); S_["Sbf"] = B(128); S_["eq"] = F(64)
            sets.append(S_)
        assert o[0] <= 16384, o[0]

        getw = self.wfull(Win[:, 12288:12352], 64)
        for c in range(8):
            bp, bk = self.psum()
            for k in range(KD):
                wt, wk = getw(k)
                self.MM(bp[:64, 0:64], self.h[:, k, c * 64:(c + 1) * 64], wt, k == 0, k == KD - 1, [wk, ("h", k)], [bk])
            self.CP("act", ba[:, c, :], bp[:64, 0:64], [bk], ["ba"])
        self.ACT(beta[:, :, :], ba[:, :, 0:32], AF.Sigmoid, ["ba"], ["beta"])
        self.TT(gg[:, :, :], ba[:, :, 32:64], dtb[:, None, :].broadcast_to([64, 8, 32]), ALU.add, ["ba", "gdv"], ["gg"])
        self.ACT(gg[:, :, :], gg[:, :, :], AF.Exp, ["gg"], ["gg"])
        self.ACT(gg[:, :, :], gg[:, :, :], AF.Ln, ["gg"], ["gg"], bias=1.0)
        self.TT(gg[:, :, :], gg[:, :, :], negA[:, None, :].broadcast_to([64, 8, 32]), ALU.mult, ["gg", "gdv"], ["gg"])
        ghi = self.SC[:64, 6144:6272].bitcast(BF16).rearrange("p (c x) -> p c x", c=8)
        glo = self.SC[:64, 6272:6400].bitcast(BF16).rearrange("p (c x) -> p c x", c=8)
        gtmp = self.SC[:64, 6400:6656].rearrange("p (c x) -> p c x", c=8)
        trib = self.cstb[:64, 128:192]
        onesb = self.cstb[:64, 768:896]
        self.CP("dve", ghi, gg[:, :, :], ["gg"], ["ghi"])
        self.TT(gtmp, gg[:, :, :], ghi, ALU.subtract, ["gg", "ghi"], ["gtmp"])
        self.CP("dve", glo, gtmp, ["gtmp"], ["glo"])
        for c in range(8):
            cp_, ck_ = self.psum()
            self.MM(cp_[:64, 0:32], trib, ghi[:, c, :], True, False, ["cst", "ghi"], [ck_])
            self.MM(cp_[:64, 0:32], trib, glo[:, c, :], False, True, ["cst", "glo"], [ck_])
            self.CP("act", gc[:, c, :], cp_[:64, 0:32], [ck_], ["gc"])
            ep_, ek_ = self.psum()
            self.MM(ep_[:, 0:32], onesb, ghi[:, c, :], True, False, ["cst", "ghi"], [ek_])
            self.MM(ep_[:, 0:32], onesb, glo[:, c, :], False, True, ["cst", "glo"], [ek_])
            self.ACT(dend[:, c, :], ep_[:, 0:32], AF.Exp, [ek_], ["dend"])
            self.TT(tailf[:, c, :], ep_[:64, 0:32], gc[:, c, :], ALU.subtract, [ek_, "gc"], ["tailf"])
        self.ACT(tailf[:, :, :], tailf[:, :, :], AF.Exp, ["tailf"], ["tailf"])
        self.ACT(bg[:, :, :], gc[:, :, :], AF.Exp, ["gc"], ["bg"])
        self.TT(bg[:, :, :], bg[:, :, :], beta[:, :, :], ALU.mult, ["bg", "beta"], ["bg"])

        import os as _os3
        _cs = int(_os3.environ.get("GDN_CS", "9"))

        def conv_silu(ch, pt, pk, dst, dkey, l2=None):
            self.ACT(cbuf[:, 3:515], pt[:, :], AF.Copy, [pk], ["cbuf"])
            if _cs <= 1:
                return
            self.CP("dve", cbuf[:, 0:3], self.gtail[:, ch, :], ["gtail"], ["cbuf"])
            self.CP("dve", self.gtail[:, ch, :], cbuf[:, 512:515], ["cbuf"], ["gtail"])
            self.TS(acc, cbuf[:, 0:512], cw(0, ch), None, ALU.mult, None, ["cbuf", "gdv"], ["acc"])
            for j in (1, 2, 3):
                self.STT(acc, cbuf[:, j:j + 512], cw(j, ch), acc, ALU.mult, ALU.add, ["cbuf", "gdv", "acc"], ["acc"])
            if _cs <= 2:
                return
            if l2 is None:
                self.ACT(dst, acc, AF.Silu, ["acc"], [dkey])
                return
            self.ACT(acc, acc, AF.Silu, ["acc"], ["acc"])
            if _cs <= 3:
                return
            s_, sk_ = self.sq()
            self.ACT(s_[:], acc, AF.Square, ["acc"], [sk_])
            lp, lk = self.psum()
            self.MM(lp[:, :], self.ones[:], s_[:], True, True, ["ones", sk_], [lk])
            if _cs <= 4:
                return
            self.TS(rn, lp[:, :], 1e-6, None, ALU.add, None, [lk], ["rn"])
            if _cs <= 5:
                return
            self.ACT(rn, rn, AF.Sqrt, ["rn"], ["rn"])
            if _cs <= 6:
                return
            self.P.op("dve", lambda e: e.reciprocal(out=rn, in_=rn), reads=["rn"], writes=["rn"])
            if _cs <= 7:
                return
            self.STT(dst, acc, l2, rn, ALU.mult, ALU.mult, ["acc", "rn"], [dkey])

        import os as _os
        _dbg = int(_os.environ.get("GDN_DBG", "0"))
        for hq in range(16 if _dbg != 3 else 0):
            self.linear(Win, 128, KD, hq * 128, 128, 128, self.hact,
                        lambda n, pt, pk: conv_silu(hq, pt, pk, qn, "qn", l2=128.0 ** -0.5), cpt=1)
            self.linear(Win, 128, KD, 2048 + hq * 128, 128, 128, self.hact,
                        lambda n, pt, pk: conv_silu(16 + hq, pt, pk, kn, "kn", l2=1.0), cpt=1)
            for c in range(8 if _dbg != 5 else 0):
                cs = slice(c * 64, (c + 1) * 64)
                g_p, g_k = self.psum()
                if _dbg not in (6, 7, 8):
                    self.MM(g_p[:64, 0:64], kn[:, cs], kn[:, cs], True, True, ["kn"], [g_k])
                if _dbg != 8:
                    self.MM(g_p[:64, 64:128], kn[:, cs], qn[:, cs], True, True, ["kn", "qn"], [g_k])
                if _dbg != 7:
                    self.MM(g_p[:64, 128:256], kn[:, cs], self.identb[:, :], True, True, ["kn", "identb"], [g_k])
                if _dbg not in (6, 7, 8):
                    self.CP("act", Gs[:, c, :], g_p[:64, 0:64], [g_k], ["Gs"])
                if _dbg != 8:
                    self.CP("act", QK[:, c, :], g_p[:64, 64:128], [g_k], ["QK"])
                if _dbg != 7:
                    self.CP("act", ktk[:, c, :], g_p[:64, 128:256], [g_k], ["ktk"])
            hs = (2 * hq, 2 * hq + 1) if _dbg not in (4, 5, 6, 7, 8) else ()
            for si, h in enumerate(hs):
                S_ = sets[si]
                self.linear(Win, 128, KD, 4096 + h * 128, 128, 128, self.hact,
                            lambda n, pt, pk: conv_silu(32 + h, pt, pk, vT, "vT"), cpt=1)
                self.linear(Win, 128, KD, 8192 + h * 128, 128, 128, self.hact,
                            lambda n, pt, pk: self.ACT(S_["szT"], pt[:, :], AF.Silu, [pk], [f"szT{si}"]), cpt=1)
                for c in range(8):
                    v_p, v_k = self.psum()
                    self.MM(v_p[:64, 0:128], vT[:, c * 64:(c + 1) * 64], self.identb[:, :], True, True, ["vT", "identb"], [v_k])
                    self.TS(S_["vbt"][:, c, :], v_p[:64, 0:128], beta[:, c, h:h + 1], None, ALU.mult, None, [v_k, "beta"], [f"vbt{si}"])

            def chain(si, h):
                S_ = sets[si]
                k_ = lambda nm: f"{nm}{si}"
                dg, tmpa, Dm, DT, dgt, dgh, dgl = S_["dg"], S_["tmpa"], S_["Dm"], S_["DT"], S_["dgt"], S_["dgh"], S_["dgl"]
                NN, MM_, Uu = [S_["Na"], S_["Nb"]], [S_["Ma"], S_["Mb"]], S_["Uu"]
                kb, usb, wT, vn, Aq, qdc, ktt, Sbf, eq = (S_[x] for x in ("kb", "usb", "wT", "vn", "Aq", "qdc", "ktt", "Sbf", "eq"))
                vbt_, szT_ = S_["vbt"], S_["szT"]
                b0 = 4 * si
                o_p, o_k = self.ps[b0], ("ps", b0)
                P1, K1 = self.ps[b0 + 1], ("ps", b0 + 1)
                P2, K2 = self.ps[b0 + 2], ("ps", b0 + 2)
                P3, K3 = self.ps[b0 + 3], ("ps", b0 + 3)
                Sv = ST[:, h * 128:(h + 1) * 128]
                sk = ("ST", h)
                self.CP("dve", Sbf, Sv, [sk], [k_("Sbf")])
                yield
                for c in range(8 if _dbg != 1 else 0):
                    cs = slice(c * 64, (c + 1) * 64)
                    gci = gc[:, c, h:h + 1]
                    self.TS(dg, self.identf[:64, 0:64], gci, None, ALU.mult, None, ["cst", "gc"], [k_("dg")]); yield
                    self.CP("dve", dgh, dg, [k_("dg")], [k_("dgh")]); yield
                    self.TT(dgt, dg, dgh, ALU.subtract, [k_("dg"), k_("dgh")], [k_("dgt")]); yield
                    self.CP("dve", dgl, dgt, [k_("dgt")], [k_("dgl")]); yield
                    self.MM(P1[:, 0:64], onesb, dgh, True, False, ["cst", k_("dgh")], [K1])
                    self.MM(P1[:, 0:64], onesb, dgl, False, True, ["cst", k_("dgl")], [K1]); yield
                    self.TS(tmpa, P1[:64, 0:64], gci, 0.0, ALU.subtract, ALU.max, [K1, "gc"], [k_("tmpa")]); yield
                    self.ACT(Dm, tmpa, AF.Exp, [k_("tmpa")], [k_("Dm")], scale=-1.0); yield
                    self.TS(DT, P1[:64, 0:64], gci, 0.0, ALU.subtract, ALU.min, [K1, "gc"], [k_("DT")]); yield
                    self.ACT(DT, DT, AF.Exp, [k_("DT")], [k_("DT")]); yield
                    self.ACT(eq, P1[:, 0:64], AF.Exp, [K1], [k_("eq")]); yield
                    self.TT(qdc, qn[:, cs], eq, ALU.mult, ["qn", k_("eq")], [k_("qdc")]); yield
                    Nn, Mm = NN[0], MM_[0]
                    self.TT(Nn, Gs[:, c, :], Dm, ALU.mult, ["Gs", k_("Dm")], [k_("N0")]); yield
                    self.STT(Nn, Nn, beta[:, c, h:h + 1], self.ntri_gt[:64, :], ALU.mult, ALU.mult, [k_("N0"), "beta", "cst"], [k_("N0")]); yield
                    self.MM(P2[:64, 0:64], Nn, self.identb[:64, 0:64], True, True, [k_("N0"), "identb"], [K2]); yield
                    self.CP("act", Mm, P2[:64, 0:64], [K2], [k_("M0")]); yield
                    self.TT(Uu, Mm, self.identf[:64, 0:64], ALU.add, [k_("M0"), "cst"], [k_("Uu")]); yield
                    cur = 0
                    for lvl in range(5):
                        nx = 1 - cur
                        Nc, Mc, Nx, Mx = NN[cur], MM_[cur], NN[nx], MM_[nx]
                        kN, kM, kNx, kMx = k_(f"N{cur}"), k_(f"M{cur}"), k_(f"N{nx}"), k_(f"M{nx}")
                        self.MM(P2[:64, 64:128], Mc, Nc, True, True, [kM, kN], [K2])
                        if lvl < 4:
                            self.MM(P2[:64, 128:192], Nc, Mc, True, True, [kM, kN], [K2])
                        yield
                        self.CP("act", Nx, P2[:64, 64:128], [K2], [kNx]); yield
                        if lvl < 4:
                            self.CP("act", Mx, P2[:64, 128:192], [K2], [kMx]); yield
                        self.MM(P3[:64, 0:64], Nx, Uu, True, True, [kNx, k_("Uu")], [K3]); yield
                        self.TT(Uu, Uu, P3[:64, 0:64], ALU.add, [k_("Uu"), K3], [k_("Uu")]); yield
                        cur = nx
                    self.TS(kb, ktk[:, c, :], bg[:, c, h:h + 1], None, ALU.mult, None, ["ktk", "bg"], [k_("kb")]); yield
                    self.MM(P2[:, 256:320], kb, Uu, True, True, [k_("kb"), k_("Uu")], [K2]); yield
                    self.CP("act", wT, P2[:, 256:320], [K2], [k_("wT")]); yield
                    self.MM(P3[:64, 128:256], Uu, vbt_[:, c, :], True, True, [k_("Uu"), k_("vbt")], [K3]); yield
                    self.CP("act", usb, P3[:64, 128:256], [K3], [k_("usb")]); yield
                    self.MM(P3[:64, 256:384], wT, Sbf, True, True, [k_("wT"), k_("Sbf")], [K3]); yield
                    self.TT(vn, usb, P3[:64, 256:384], ALU.subtract, [k_("usb"), K3], [k_("vn")]); yield
                    self.TT(tmpa, QK[:, c, :], DT, ALU.mult, ["QK", k_("DT")], [k_("tmpa")]); yield
                    self.TT(Aq, tmpa, self.tri_ge[:64, :], ALU.mult, [k_("tmpa"), "cst"], [k_("Aq")]); yield
                    self.MM(o_p[:, cs], Sbf, qdc, True, False, [k_("Sbf"), k_("qdc")], [o_k])
                    self.MM(o_p[:, cs], vn, Aq, False, True, [k_("vn"), k_("Aq")], [o_k]); yield
                    self.TS(ktt, ktk[:, c, :], tailf[:, c, h:h + 1], None, ALU.mult, None, ["ktk", "tailf"], [k_("ktt")]); yield
                    self.MM(P2[:, 384:512], ktt, vn, True, True, [k_("ktt"), k_("vn")], [K2]); yield
                    self.STT(Sv, Sv, dend[:, c, h:h + 1], P2[:, 384:512], ALU.mult, ALU.add, [sk, "dend", K2], [sk]); yield
                    self.CP("act", Sbf, Sv, [sk], [k_("Sbf")]); yield
                s_, sk_ = self.sq()
                self.ACT(s_[:], o_p[:, :], AF.Square, [o_k], [sk_])
                self.MM(P1[:, :], self.ones[:], s_[:], True, True, ["ones", sk_], [K1])
                self.TS(rn, P1[:, :], 1.0 / 128.0, 1e-6, ALU.mult, ALU.add, [K1], ["rn"])
                self.ACT(rn, rn, AF.Sqrt, ["rn"], ["rn"])
                self.P.op("dve", lambda e: e.reciprocal(out=rn, in_=rn), reads=["rn"], writes=["rn"])
                self.TT(t1, o_p[:, :], rn, ALU.mult, [o_k, "rn"], ["t1"])
                self.STT(self.U[:, h, :], szT_, normw, t1, ALU.mult, ALU.mult, [k_("szT"), "gdv", "t1"], [("U", h)])
                yield

            gens = [chain(si, h) for si, h in enumerate(hs)]
            while gens:
                for g_ in list(gens):
                    try:
                        next(g_)
                    except StopIteration:
                        gens.remove(g_)
        self.DMA("sp", "sts_gdn", self.st_gdn, ST, [("ST", h_) for h_ in range(32)], ["st_gdn", "stg"])
        self.linear(Wout, 128, 32, 0, D, 128, lambda k: (self.U[:, k, :], ("U", k)), resid)


_CACHE = {}


def _vecP(v, p=128):
    v = np.asarray(v, np.float32)
    lead = v.shape[:-1]
    c = v.shape[-1] // p
    v = v.reshape(*lead, c, p)
    v = np.moveaxis(v, -1, 0)
    return np.ascontiguousarray(v.reshape(p, -1))


def make_inputs(inputs, b):
    f = lambda a: np.ascontiguousarray(np.asarray(a, np.float32))
    m = {}
    m["xT"] = np.ascontiguousarray(np.asarray(inputs["x"][b], np.float32).T)
    m["cT"] = _vecP(inputs["c"][b])
    m["lng"] = _vecP(inputs["ln_g"])
    m["lnb"] = _vecP(inputs["ln_b"])
    m["bmod"] = _vecP(inputs["b_mod"])
    for k in ("w_mod", "w_ff1", "w_ff2", "rglru_w_in", "rglru_w_out", "rglru_w_rgate", "rglru_w_igate",
              "gla_w_in", "gla_w_out", "gla_w_alpha_up", "gdn_w_in", "gdn_w_out"):
        m[k] = f(inputs[k])
    cst = np.zeros((128, 1024), np.float32)
    cst[:, 0:128] = np.eye(128, dtype=np.float32)
    pp = np.arange(128)[:, None]
    ff = np.arange(64)[None, :]
    cst[:, 128:192] = (ff >= pp).astype(np.float32)
    cst[:, 192:256] = (ff < pp).astype(np.float32)
    cst[:, 256:768] = ((np.arange(512) % 64) != 0).astype(np.float32)[None, :]
    cst[:, 768:896] = 1.0
    cst[:, 896:960] = -(ff < pp).astype(np.float32)
    m["cst"] = cst
    m["glav"] = np.concatenate([_vecP(inputs["gla_b_alpha"][0]), _vecP(inputs["gla_norm_w"][0])], axis=1)
    gd = np.zeros((128, 321), np.float32)
    gd[:, 0:256] = _vecP(inputs["gdn_conv_w"][0])
    gd[:, 256] = np.asarray(inputs["gdn_norm_w"][0], np.float32)
    gd[:, 257:289] = np.asarray(inputs["gdn_a_log"][0], np.float32)[None, :]
    gd[:, 289:321] = np.asarray(inputs["gdn_dt_bias"][0], np.float32)[None, :]
    m["gdnv"] = gd
    cwv = np.asarray(inputs["rglru_conv_w"], np.float32)
    parts = [cwv[:, 0], cwv[:, 1], cwv[:, 2], cwv[:, 3], np.asarray(inputs["rglru_conv_b"], np.float32),
             np.asarray(inputs["rglru_b_rgate"], np.float32).reshape(2, RW), np.asarray(inputs["rglru_b_igate"], np.float32).reshape(2, RW),
             np.asarray(inputs["rglru_lambda"], np.float32)]
    rv = np.stack(parts, axis=1)
    m["rvec"] = _vecP(rv, 80)
    return m


def kernel(**inputs):
    layers = tuple(inputs.pop("_layers", range(DEPTH)))
    n_pass = int(inputs.pop("_n_pass", SEQ // TP))
    ncores = int(inputs.pop("_cores", 8))
    key = (layers, n_pass)
    if key not in _CACHE:
        mk = MK(layers, n_pass)
        _CACHE[key] = (mk.build(), set(mk.din.keys()))
    nc, names = _CACHE[key]
    full = [make_inputs(inputs, b) for b in range(min(4, ncores))]
    in_maps = [{k: v for k, v in full[i % len(full)].items() if k in names} for i in range(ncores)]
    res = run_bass_kernel_spmd(nc, in_maps, core_ids=list(range(ncores)))
    nb = min(4, ncores)
    out = np.stack([np.ascontiguousarray(res.results[b]["yT"].T) for b in range(nb)], axis=0)
    return out.astype(np.float32)
```
